# Optimizing a Trainium2 kernel written in Bass

```python
import math
import jax, jax.numpy as jnp
from jax import lax
import numpy as np

D_MODEL = 1024
BATCH = 8
SEQ = 4096
DEPTH = 1

HEAD_DIM = 64
N_MOBA_HEADS = 6
N_SB_HEADS = 6
N_MEM_HEADS = 4
MOBA_WIDTH = N_MOBA_HEADS * HEAD_DIM
SB_WIDTH = N_SB_HEADS * HEAD_DIM
MEM_WIDTH = N_MEM_HEADS * HEAD_DIM
N_MEM = 256
MOBA_BLOCK = 256
MOBA_TOPK = 3
MOBA_QCHUNK = 64
SB_QBLOCK = 128
N_BUCKETS = 32
MAX_DISTANCE = 128
N_GROUPS = 4
EXPERTS_PER_GROUP = 8
N_EXPERTS = N_GROUPS * EXPERTS_PER_GROUP
EXPERT_TOPK = 2
D_EXPERT = 512
MOE_BLOCK = 256
N_BRANCH = 3
PROJ_WIDTH = 3 * MOBA_WIDTH + 3 * SB_WIDTH + MEM_WIDTH + N_BRANCH * D_MODEL
DEEPNORM_ALPHA = (2.0 * DEPTH) ** 0.25
DEEPNORM_BETA = (8.0 * DEPTH) ** -0.25
LN_EPS = 1e-5
NEG = -1e30

kernel_name = "hybrid_moba_stickbreak_memxattn_hiermoe_deepnorm"


def layer_norm(x, g, b):
    xf = x.astype(jnp.float32)
    mu = xf.mean(-1, keepdims=True)
    var = jnp.square(xf - mu).mean(-1, keepdims=True)
    return ((xf - mu) * lax.rsqrt(var + LN_EPS) * g + b).astype(x.dtype)


def t5_bucket(rel):
    rel = jnp.maximum(rel, 0)
    max_exact = N_BUCKETS // 2
    rel_f = jnp.maximum(rel, 1).astype(jnp.float32)
    large = max_exact + (jnp.log(rel_f / max_exact) / math.log(MAX_DISTANCE / max_exact)
                         * (N_BUCKETS - max_exact)).astype(jnp.int32)
    large = jnp.minimum(large, N_BUCKETS - 1)
    return jnp.where(rel < max_exact, rel, large)


def split_heads(t, n_heads):
    B, S, _ = t.shape
    return t.reshape(B, S, n_heads, HEAD_DIM).transpose(0, 2, 1, 3)


def merge_heads(t):
    B, H, S, dh = t.shape
    return t.transpose(0, 2, 1, 3).reshape(B, S, H * dh)


def moba_attention(q, k, v, rel_bias):
    B, H, S, dh = q.shape
    dtype = q.dtype
    q, k, v = (t.astype(jnp.float32) for t in (q, k, v))
    scale = dh ** -0.5
    nb = -(-S // MOBA_BLOCK)
    pad = nb * MOBA_BLOCK - S
    k = jnp.pad(k, ((0, 0), (0, 0), (0, pad), (0, 0)))
    v = jnp.pad(v, ((0, 0), (0, 0), (0, pad), (0, 0)))
    kb = k.reshape(B, H, nb, MOBA_BLOCK, dh)
    vb = v.reshape(B, H, nb, MOBA_BLOCK, dh)
    k_mean = kb.mean(axis=3)
    topk = min(MOBA_TOPK, nb - 1)
    bias_ht = rel_bias.T.astype(jnp.float32)
    blk_ar = jnp.arange(MOBA_BLOCK)
    b_idx = jnp.arange(B)[:, None, None]
    h_idx = jnp.arange(H)[None, :, None]
    h_idx4 = jnp.arange(H)[None, :, None, None]

    def partial_attn(s, vals, eq):
        m = s.max(-1)
        p = jnp.exp(s - m[..., None])
        return m, p.sum(-1), jnp.einsum(eq, p, vals)

    def chunk(args):
        c, qc = args
        start = c * MOBA_QCHUNK
        q_pos = start + jnp.arange(MOBA_QCHUNK)
        own = start // MOBA_BLOCK
        k_own = lax.dynamic_index_in_dim(kb, own, axis=2, keepdims=False)
        v_own = lax.dynamic_index_in_dim(vb, own, axis=2, keepdims=False)
        rel = q_pos[:, None] - (own * MOBA_BLOCK + blk_ar)[None, :]
        s = jnp.einsum('bhqd,bhkd->bhqk', qc, k_own) * scale + bias_ht[:, t5_bucket(rel)][None]
        s = jnp.where(rel >= 0, s, NEG)
        stats = [partial_attn(s, v_own, 'bhqk,bhkd->bhqd')]
        if topk > 0:
            gate = jnp.einsum('bhqd,bhnd->bhqn', qc, k_mean)
            gate = jnp.where(jnp.arange(nb) < own, gate, NEG)
            _, sel = lax.top_k(gate, topk)
            for r in range(topk):
                idx = sel[..., r]
                k_sel = kb[b_idx, h_idx, idx]
                v_sel = vb[b_idx, h_idx, idx]
                rel = q_pos[:, None] - (idx[..., None] * MOBA_BLOCK + blk_ar)
                s = (jnp.einsum('bhqd,bhqkd->bhqk', qc, k_sel) * scale
                     + bias_ht[h_idx4, t5_bucket(rel)])
                s = jnp.where(r < own, s, NEG)
                stats.append(partial_attn(s, v_sel, 'bhqk,bhqkd->bhqd'))
        m = jnp.stack([st[0] for st in stats])
        l = jnp.stack([st[1] for st in stats])
        o = jnp.stack([st[2] for st in stats])
        w = jnp.exp(m - m.max(0))
        return (w[..., None] * o).sum(0) / (w * l).sum(0)[..., None]

    nc = S // MOBA_QCHUNK
    qcs = q.reshape(B, H, nc, MOBA_QCHUNK, dh).transpose(2, 0, 1, 3, 4)
    out = lax.map(chunk, (jnp.arange(nc), qcs))
    return out.transpose(1, 2, 0, 3, 4).reshape(B, H, S, dh).astype(dtype)


def stick_breaking_attention(q, k, v):
    B, H, S, dh = q.shape
    dtype = q.dtype
    scale = dh ** -0.5
    kf = k.astype(jnp.float32)
    vf = v.astype(jnp.float32)
    nqb = S // SB_QBLOCK
    qbs = q.astype(jnp.float32).reshape(B, H, nqb, SB_QBLOCK, dh).transpose(2, 0, 1, 3, 4)
    k_pos = jnp.arange(S)

    def block(args):
        i, qb = args
        q_pos = i * SB_QBLOCK + jnp.arange(SB_QBLOCK)
        strict = k_pos[None, :] < q_pos[:, None]
        z = jnp.einsum('bhqd,bhkd->bhqk', qb, kf) * scale
        log_keep = jnp.where(strict, jax.nn.log_sigmoid(-z), 0.0)
        log_stick = lax.cumsum(log_keep, axis=3, reverse=True) - log_keep
        a = jnp.where(strict, jnp.exp(jax.nn.log_sigmoid(z) + log_stick), 0.0)
        return jnp.einsum('bhqk,bhkd->bhqd', a, vf)

    out = lax.map(block, (jnp.arange(nqb), qbs))
    return out.transpose(1, 2, 0, 3, 4).reshape(B, H, S, dh).astype(dtype)


def memory_attention(q, k, v):
    scale = q.shape[-1] ** -0.5
    s = jnp.einsum('bhqd,bhmd->bhqm', q.astype(jnp.float32), k.astype(jnp.float32)) * scale
    p = jax.nn.softmax(s, axis=-1)
    return jnp.einsum('bhqm,bhmd->bhqd', p, v.astype(jnp.float32)).astype(q.dtype)


def hybrid_mixer(x, mem, w_in, w_mem_kv, rel_bias, w_br_moba, w_br_sb, w_br_mem, w_out):
    sizes = (MOBA_WIDTH, MOBA_WIDTH, MOBA_WIDTH, SB_WIDTH, SB_WIDTH, SB_WIDTH, MEM_WIDTH)
    offsets = []
    acc = 0
    for sz in sizes:
        acc += sz
        offsets.append(acc)
    proj = x @ w_in
    q_a, k_a, v_a, q_b, k_b, v_b, q_m, gate_logits = jnp.split(proj, offsets, axis=-1)
    y_a = merge_heads(moba_attention(split_heads(q_a, N_MOBA_HEADS), split_heads(k_a, N_MOBA_HEADS),
                                     split_heads(v_a, N_MOBA_HEADS), rel_bias))
    y_b = merge_heads(stick_breaking_attention(split_heads(q_b, N_SB_HEADS), split_heads(k_b, N_SB_HEADS),
                                               split_heads(v_b, N_SB_HEADS)))
    k_m, v_m = jnp.split(mem @ w_mem_kv, 2, axis=-1)
    y_m = merge_heads(memory_attention(split_heads(q_m, N_MEM_HEADS), split_heads(k_m, N_MEM_HEADS),
                                       split_heads(v_m, N_MEM_HEADS)))
    B, S, _ = x.shape
    gates = jax.nn.sigmoid(gate_logits.reshape(B, S, N_BRANCH, D_MODEL))
    merged = (gates[:, :, 0] * (y_a @ w_br_moba)
              + gates[:, :, 1] * (y_b @ w_br_sb)
              + gates[:, :, 2] * (y_m @ w_br_mem))
    return merged @ w_out


def hierarchical_moe(x, w_router_group, b_router_group, w_router_expert, b_router_expert,
                     w_gate, w_up, w_down):
    B, S, D = x.shape
    xt = x.reshape(-1, D)
    N = xt.shape[0]
    g_prob = jax.nn.softmax((xt @ w_router_group + b_router_group).astype(jnp.float32), axis=-1)
    g_p, g_idx = lax.top_k(g_prob, 1)
    e_logits = (xt @ w_router_expert + b_router_expert).astype(jnp.float32)
    e_logits = e_logits.reshape(N, N_GROUPS, EXPERTS_PER_GROUP)
    e_in_group = jnp.take_along_axis(e_logits, g_idx[:, :, None], axis=1)[:, 0]
    top_l, top_i = lax.top_k(e_in_group, EXPERT_TOPK)
    gate = g_p * jax.nn.softmax(top_l, axis=-1)
    expert = g_idx * EXPERTS_PER_GROUP + top_i

    M = N * EXPERT_TOPK
    flat_e = expert.reshape(M)
    flat_tok = jnp.arange(M) // EXPERT_TOPK
    flat_gate = gate.reshape(M)
    order = jnp.argsort(flat_e)
    e_sorted = flat_e[order]
    tok_sorted = flat_tok[order]
    gate_sorted = flat_gate[order]
    counts = jnp.bincount(flat_e, length=N_EXPERTS)
    padded = (counts + MOE_BLOCK - 1) // MOE_BLOCK * MOE_BLOCK
    start = jnp.cumsum(counts) - counts
    pstart = jnp.cumsum(padded) - padded
    dest = pstart[e_sorted] + (jnp.arange(M) - start[e_sorted])
    n_blocks = -(-M // MOE_BLOCK) + N_EXPERTS
    P = n_blocks * MOE_BLOCK
    x_pad = jnp.zeros((P, D), xt.dtype).at[dest].set(xt[tok_sorted])
    block_expert = jnp.clip(jnp.searchsorted(pstart + padded, jnp.arange(n_blocks) * MOE_BLOCK, side='right'),
                            0, N_EXPERTS - 1)

    def expert_block(args):
        xb, e = args
        h = jax.nn.silu(xb @ w_gate[e]) * (xb @ w_up[e])
        return h @ w_down[e]

    y_pad = lax.map(expert_block, (x_pad.reshape(n_blocks, MOE_BLOCK, D), block_expert)).reshape(P, D)
    y_assign = y_pad[dest] * gate_sorted[:, None].astype(y_pad.dtype)
    out = jax.ops.segment_sum(y_assign, tok_sorted, num_segments=N)
    return out.reshape(B, S, D)


def setup_inputs(seed: int = 0) -> dict:
    key = jax.random.key(seed)
    ks = jax.random.split(key, 20)
    f32 = jnp.float32
    L, D = DEPTH, D_MODEL

    def nrm(k, shape, scale):
        return jax.random.normal(k, shape, f32) * scale

    return {
        "x": nrm(ks[0], (BATCH, SEQ, D), 1.0),
        "mem": nrm(ks[1], (BATCH, N_MEM, D), 1.0),
        "w_in": nrm(ks[2], (L, D, PROJ_WIDTH), D ** -0.5),
        "w_mem_kv": nrm(ks[3], (L, D, 2 * MEM_WIDTH), D ** -0.5),
        "rel_bias": nrm(ks[4], (N_BUCKETS, N_MOBA_HEADS), 0.5),
        "w_br_moba": nrm(ks[5], (L, MOBA_WIDTH, D), MOBA_WIDTH ** -0.5),
        "w_br_sb": nrm(ks[6], (L, SB_WIDTH, D), SB_WIDTH ** -0.5),
        "w_br_mem": nrm(ks[7], (L, MEM_WIDTH, D), MEM_WIDTH ** -0.5),
        "w_out": nrm(ks[8], (L, D, D), D ** -0.5 * DEEPNORM_BETA),
        "ln1_g": 1.0 + nrm(ks[9], (L, D), 0.02),
        "ln1_b": nrm(ks[10], (L, D), 0.02),
        "w_router_group": nrm(ks[11], (L, D, N_GROUPS), D ** -0.5),
        "b_router_group": nrm(ks[12], (L, N_GROUPS), 0.01),
        "w_router_expert": nrm(ks[13], (L, D, N_EXPERTS), D ** -0.5),
        "b_router_expert": nrm(ks[14], (L, N_EXPERTS), 0.01),
        "w_gate": nrm(ks[15], (L, N_EXPERTS, D, D_EXPERT), D ** -0.5),
        "w_up": nrm(ks[16], (L, N_EXPERTS, D, D_EXPERT), D ** -0.5),
        "w_down": nrm(ks[17], (L, N_EXPERTS, D_EXPERT, D), D_EXPERT ** -0.5 * DEEPNORM_BETA),
        "ln2_g": 1.0 + nrm(ks[18], (L, D), 0.02),
        "ln2_b": nrm(ks[19], (L, D), 0.02),
    }


def reference(x, mem, w_in, w_mem_kv, rel_bias, w_br_moba, w_br_sb, w_br_mem, w_out, ln1_g, ln1_b,
              w_router_group, b_router_group, w_router_expert, b_router_expert,
              w_gate, w_up, w_down, ln2_g, ln2_b):
    for l in range(DEPTH):
        mix = hybrid_mixer(x, mem, w_in[l], w_mem_kv[l], rel_bias, w_br_moba[l], w_br_sb[l],
                           w_br_mem[l], w_out[l])
        x = layer_norm(DEEPNORM_ALPHA * x + mix, ln1_g[l], ln1_b[l])
        ffn = hierarchical_moe(x, w_router_group[l], b_router_group[l], w_router_expert[l],
                               b_router_expert[l], w_gate[l], w_up[l], w_down[l])
        x = layer_norm(DEEPNORM_ALPHA * x + ffn, ln2_g[l], ln2_b[l])
    return x
```

```python
import math
import numpy as np
import concourse.bass as bass
import concourse.mybir as mybir
from concourse.bass_utils import run_bass_kernel_spmd
from contextlib import ExitStack

F32 = mybir.dt.float32
BF16 = mybir.dt.bfloat16
I32 = mybir.dt.int32
AF = mybir.ActivationFunctionType
ALU = mybir.AluOpType
AX = mybir.AxisListType

S = 4096
D = 1024
NST = 8
CAP = 384
NSLOT = 32 * CAP
NT = CAP // 128
ALPHA = (2.0 * 1) ** 0.25
EPS = 1e-5
NEGM = -30000.0

ENGS = ["sync", "scalar", "vector", "gpsimd", "tensor"]
NPOOL = 28
NHW = 16


class Res:
    __slots__ = ("w", "r")

    def __init__(self):
        self.w = []
        self.r = []


class Op:
    __slots__ = ("eng", "fn", "deps", "dma", "needed", "sem", "val")


def _add(lst, o):
    if not o.dma:
        for i, p in enumerate(lst):
            if (not p.dma) and p.eng == o.eng:
                lst[i] = o
                return
    lst.append(o)


class Sched:
    def __init__(self, nc, es):
        self.nc = nc
        self.esem = {e: es.enter_context(nc.semaphore("s_" + e)) for e in ENGS}
        self.dsem = [es.enter_context(nc.semaphore("d%d" % i)) for i in range(NPOOL)]
        self.ecount = {e: 0 for e in ENGS}
        self.dcount = [0] * NPOOL
        self.dlast = [None] * NPOOL
        self.dnext = {"hw": 0, "sw": 0}
        self.ops = []
        self.all_res = []
        self.cleared = False
        self.nphase = 0

    def res(self):
        r = Res()
        self.all_res.append(r)
        return r

    def op(self, eng, fn, reads=(), writes=(), dma=False, append=False):
        o = Op()
        o.eng = eng
        o.fn = fn
        o.dma = dma
        o.needed = False
        o.deps = []
        o.sem = None
        o.val = 0
        for r in reads:
            o.deps.extend(r.w)
        for w in writes:
            if not append:
                o.deps.extend(w.w)
            o.deps.extend(w.r)
        for r in reads:
            _add(r.r, o)
        for w in writes:
            if append:
                _add(w.w, o)
            else:
                w.w = [o]
            w.r = []
        self.ops.append(o)
        return o

    def _clear_block(self):
        sems = list(self.esem.values()) + self.dsem
        with self.nc.Block() as block:
            @block.sync
            def _(e):
                for s in sems:
                    e.sem_clear(s)
        self.cleared = True

    def emit(self):
        nc = self.nc
        if not self.cleared:
            self._clear_block()
        ops = self.ops
        for o in ops:
            if o.dma:
                if o.eng == "gpsimd":
                    i = NHW + self.dnext["sw"] % (NPOOL - NHW)
                    self.dnext["sw"] += 1
                else:
                    i = self.dnext["hw"] % NHW
                    self.dnext["hw"] += 1
                prev = self.dlast[i]
                if prev is not None:
                    o.deps.append(prev)
                self.dcount[i] += 16
                o.sem = self.dsem[i]
                o.val = self.dcount[i]
                self.dlast[i] = o
        for o in ops:
            for d in o.deps:
                if not d.dma:
                    d.needed = True
        for o in ops:
            if (not o.dma) and o.needed:
                self.ecount[o.eng] += 1
                o.sem = self.esem[o.eng]
                o.val = self.ecount[o.eng]
        per = {e: [o for o in ops if o.eng == e] for e in ENGS}
        waited = {}

        def body(ename):
            def f(eng):
                for o in per[ename]:
                    for d in o.deps:
                        if (not d.dma) and d.eng == ename and (not o.dma) and ename == "tensor":
                            continue
                        key = (ename, id(d.sem))
                        if waited.get(key, 0) >= d.val:
                            continue
                        eng.wait_ge(d.sem, d.val)
                        waited[key] = d.val
                    ins = o.fn(eng)
                    if o.dma:
                        ins.then_inc(o.sem, 16)
                    elif o.needed:
                        ins.then_inc(o.sem, 1)
                for o in per[ename]:
                    if o.dma:
                        key = (ename, id(o.sem))
                        if waited.get(key, 0) >= o.val:
                            continue
                        eng.wait_ge(o.sem, o.val)
                        waited[key] = o.val
            return f

        with nc.Block() as block:
            block.sync(body("sync"))
            block.scalar(body("scalar"))
            block.vector(body("vector"))
            block.gpsimd(body("gpsimd"))
            block.tensor(body("tensor"))
        self.nphase += 1
        self.ops = []
        self.dlast = [None] * NPOOL
        for r in self.all_res:
            r.w = []
            r.r = []
        self.all_res = []


def _bucket(rel):
    rel = np.maximum(rel, 0)
    max_exact = 16
    rel_f = np.maximum(rel, 1).astype(np.float32)
    large = max_exact + (np.log(rel_f / np.float32(max_exact)) / np.float32(math.log(128 / 16))
                         * np.float32(16)).astype(np.int32)
    large = np.minimum(large, 31)
    return np.where(rel < max_exact, rel, large)


def _consts():
    c = {}
    p = np.arange(128)
    c["identf"] = np.eye(128, dtype=np.float32)
    kind = np.zeros((64, S), np.float32)
    for j in range(16):
        kind[j, j * 256:(j + 1) * 256] = 1.0
    c["kind"] = kind
    gm = np.zeros((32, 16), np.float32)
    own = np.ones((32, 16), np.float32)
    for t in range(32):
        gm[t, (t // 2):] = -1e30
        own[t, t // 2] = 0.0
    c["gmadd"] = np.broadcast_to(gm.reshape(1, 512), (128, 512)).copy()
    c["own01"] = np.broadcast_to(own.reshape(1, 512), (128, 512)).copy()
    k = p[:, None]
    q = p[None, :]
    c["neg0"] = np.where(q < k, NEGM, 0.0).astype(np.float32)
    sb = np.zeros((128, 4, 512), np.float32)
    ql = np.arange(512)[None, :]
    for cc in range(4):
        sb[:, cc, :] = np.where((cc * 128 + k) >= ql, NEGM, 0.0)
    c["sbneg"] = sb.reshape(128, 2048)
    u2 = np.zeros((128, 32, 64), np.float32)
    for kt in range(32):
        for m in range(64):
            if (m % 32) < kt:
                u2[:, kt, m] = 1.0
    c["u2"] = u2.reshape(128, 2048)
    sel = np.zeros((128, 32, 128), np.float32)
    for kt in range(32):
        sel[kt, kt, :] = -1.0
        sel[32 + kt, kt, :] = -1.0
    c["sel"] = sel.reshape(128, 4096)
    c["tri"] = np.where(p[:, None] >= p[None, :], -1.0, 0.0).astype(np.float32)
    c["tst"] = np.where(p[:, None] < p[None, :], 1.0, 0.0).astype(np.float32)
    c["ebase"] = np.broadcast_to((np.arange(32, dtype=np.float32) * CAP)[None, :], (128, 32)).copy()
    return c


CONST_SHAPES = {
    "identf": [128, 128], "kind": [64, S], "gmadd": [128, 512], "own01": [128, 512], "neg0": [128, 128],
    "sbneg": [128, 2048], "u2": [128, 2048], "sel": [128, 4096], "tri": [128, 128], "tst": [128, 128],
    "ebase": [128, 32],
}

SHARED_SHAPES = {
    "w_in": [D, 5632], "w_kv": [D, 512], "wbr": [D, D], "w_out": [D, D],
    "ln1g": [128, D], "ln1b": [128, D], "ln2g": [128, D], "ln2b": [128, D],
    "w_r": [D, 36], "b_r": [128, 36], "w_gate": [32, D, 512], "w_up": [32, D, 512], "w_down": [32, 512, D],
    "dg": [128, 6 * 2 * 128], "b31": [128, 6],
}
CORE_SHAPES = {"xT": [D, S], "x": [S, D], "memT": [D, 256]}


def _prep(inputs):
    f = lambda a: np.ascontiguousarray(np.asarray(a, dtype=np.float32))
    sh = {}
    sh["w_in"] = f(inputs["w_in"][0])
    sh["w_kv"] = f(inputs["w_mem_kv"][0])
    wbr = np.concatenate([inputs["w_br_moba"][0], inputs["w_br_sb"][0], inputs["w_br_mem"][0]], axis=0)
    sh["wbr"] = f(wbr)
    sh["w_out"] = f(inputs["w_out"][0])
    for nm, key in (("ln1g", "ln1_g"), ("ln1b", "ln1_b"), ("ln2g", "ln2_g"), ("ln2b", "ln2_b")):
        sh[nm] = f(np.broadcast_to(np.asarray(inputs[key][0])[None, :], (128, D)))
    sh["w_r"] = f(np.concatenate([inputs["w_router_group"][0], inputs["w_router_expert"][0]], axis=1))
    br = np.concatenate([inputs["b_router_group"][0], inputs["b_router_expert"][0]], axis=0)
    sh["b_r"] = f(np.broadcast_to(np.asarray(br)[None, :], (128, 36)))
    sh["w_gate"] = f(inputs["w_gate"][0])
    sh["w_up"] = f(inputs["w_up"][0])
    sh["w_down"] = f(inputs["w_down"][0])
    rb = np.asarray(inputs["rel_bias"], dtype=np.float32)
    p = np.arange(128)
    rel0 = p[None, :] - p[:, None]
    rel1 = rel0 + 128
    idx = np.stack([_bucket(rel0), _bucket(rel1)], axis=1)
    dg = rb[idx]
    sh["dg"] = f(dg.transpose(0, 3, 1, 2).reshape(128, 6 * 2 * 128))
    sh["b31"] = f(np.broadcast_to(rb[31][None, :], (128, 6)))
    sh.update(_consts())
    x = np.asarray(inputs["x"], dtype=np.float32)
    mem = np.asarray(inputs["mem"], dtype=np.float32)
    maps = []
    for b in range(x.shape[0]):
        m = dict(sh)
        m["x"] = np.ascontiguousarray(x[b])
        m["xT"] = np.ascontiguousarray(x[b].T)
        m["memT"] = np.ascontiguousarray(mem[b].T)
        maps.append(m)
    return maps


def build(stage=99, debug=False):
    nc = bass.Bass("TRN2", target_bir_lowering=False)
    I = {}
    for nm, shp in list(CORE_SHAPES.items()) + list(SHARED_SHAPES.items()) + list(CONST_SHAPES.items()):
        I[nm] = nc.dram_tensor(nm, shp, F32, kind="ExternalInput").ap()
    out_d = nc.dram_tensor("out", [S, D], F32, kind="ExternalOutput").ap()
    skind = "ExternalOutput" if debug else "Internal"

    def scratch(nm, shp, dt):
        return nc.dram_tensor(nm, shp, dt, kind=skind).ap()

    QA = scratch("QA", [6, 64, S], BF16)
    KA = scratch("KA", [6, 64, S], BF16)
    VA = scratch("VA", [6, 128, 32 * 64], BF16)
    QB = scratch("QB", [3, 128, S], BF16)
    KB = scratch("KB", [3, 128, S], BF16)
    VB = scratch("VB", [3, 128, 32 * 128], BF16)
    QM = scratch("QM", [2, 128, S], BF16)
    G = scratch("G", [24, 128, S], BF16)
    YT = scratch("YT", [16, 64, S], BF16)
    X1A = scratch("X1A", [S, D], F32)
    XP = nc.dram_tensor("XP", [NSLOT, D], BF16, kind="Internal").ap()
    YP = nc.dram_tensor("YP", [NSLOT, D], BF16, kind="Internal").ap()
    if debug:
        SLOTD = scratch("SLOTD", [128, 64], I32)
        GATED = scratch("GATED", [128, 64], F32)

    with ExitStack() as ges:
        K = Sched(nc, ges)
        slotS = ges.enter_context(nc.sbuf_tensor("slotS", [128, 64], I32))
        gateS = ges.enter_context(nc.sbuf_tensor("gateS", [128, 64], F32))

        def ins(eng, name, reads, writes, append=False, **kw):
            return K.op(eng, lambda e: getattr(e, name)(**kw), reads, writes, append=append)

        def dma(eng, out, in_, reads, writes, append=False):
            return K.op(eng, lambda e: e.dma_start(out=out, in_=in_), reads, writes, dma=True, append=append)

        def mm(out, lhsT, rhs, start, stop, reads, w, first):
            return K.op("tensor", lambda e: e.matmul(out, lhsT=lhsT, rhs=rhs, start=start, stop=stop),
                        reads, [w], append=not first)


        def run_segments(segs, LA=2):
            hoisted = [0] * len(segs)
            for si, seg in enumerate(segs):
                steps = seg["steps"]
                n = len(steps)
                if seg.get("pre") is not None:
                    seg["pre"]()
                for k in range(hoisted[si], min(LA, n)):
                    steps[k][0]()
                for k in range(n):
                    steps[k][1]()
                    steps[k][2]()
                    if k + LA < n:
                        steps[k + LA][0]()
                    elif si + 1 < len(segs) and segs[si + 1].get("hoist", False):
                        j = k + LA - n
                        nxt = segs[si + 1]["steps"]
                        if j < min(LA, len(nxt)) and j == hoisted[si + 1]:
                            nxt[j][0]()
                            hoisted[si + 1] = j + 1
                if seg.get("post") is not None:
                    seg["post"]()

        with ExitStack() as es:
            sb = lambda n, s, d: es.enter_context(nc.sbuf_tensor(n, s, d))
            ps = lambda n: es.enter_context(nc.psum_tensor(n, [128, 512], F32))
            win = sb("p1_win", [128, 8, 5632], BF16)
            r_win = [K.res() for _ in range(11)]
            xt = [sb("p1_xt%d" % i, [128, 8, 512], BF16) for i in range(2)]
            r_xt = [K.res() for _ in range(2)]
            NO = 6
            ost = [sb("p1_o%d" % i, [128, 512], BF16) for i in range(NO)]
            r_ost = [K.res() for _ in range(NO)]
            vst = [sb("p1_v%d" % i, [128, 384], BF16) for i in range(2)]
            r_vst = [K.res() for _ in range(2)]
            psA = [ps("p1_pa%d" % i) for i in range(5)]
            r_psA = [K.res() for _ in range(5)]
            psV = [ps("p1_pv%d" % i) for i in range(2)]
            r_psV = [K.res() for _ in range(2)]
            w_in_v = I["w_in"].rearrange("(c p) n -> p c n", p=128)
            xT_v = I["xT"].rearrange("(c p) t -> p c t", p=128)
            for cb in range(11):
                dma("gpsimd", win[:, :, cb * 512:(cb + 1) * 512], w_in_v[:, :, cb * 512:(cb + 1) * 512], [], [r_win[cb]])
            dma("gpsimd", xt[0][:, :, :], xT_v[:, :, 0:512], [], [r_xt[0]])
            groups = []
            QAf = QA.rearrange("h p t -> (h p) t")
            KAf = KA.rearrange("h p t -> (h p) t")
            for i in range(3):
                groups.append((128 * i, 128, (lambda s, i=i: QAf[128 * i:128 * (i + 1), s * 512:(s + 1) * 512]), "q"))
            for i in range(3):
                groups.append((384 + 128 * i, 128, (lambda s, i=i: KAf[128 * i:128 * (i + 1), s * 512:(s + 1) * 512]), "k"))
            for i in range(3):
                groups.append((1152 + 128 * i, 128, (lambda s, i=i: QB[i, :, s * 512:(s + 1) * 512]), "q"))
            for i in range(3):
                groups.append((1536 + 128 * i, 128, (lambda s, i=i: KB[i, :, s * 512:(s + 1) * 512]), "k"))
            for i in range(2):
                groups.append((2304 + 128 * i, 128, (lambda s, i=i: QM[i, :, s * 512:(s + 1) * 512]), "q"))
            for i in range(24):
                groups.append((2560 + 128 * i, 128, (lambda s, i=i: G[i, :, s * 512:(s + 1) * 512]), "g"))
            VA_v = VA.rearrange("h p (t d) -> p h t d", d=64)
            VB_v = VB.rearrange("i p (t d) -> p i t d", d=128)
            gi = 0
            for s in range(NST):
                xb = xt[s % 2]
                rxb = r_xt[s % 2]
                if s + 1 < NST:
                    dma("gpsimd", xt[(s + 1) % 2][:, :, :], xT_v[:, :, (s + 1) * 512:(s + 2) * 512], [], [r_xt[(s + 1) % 2]])
                for (c0, ncol, dst, kind) in groups:
                    pi = gi % 5
                    oi = gi % NO
                    gi += 1
                    for c in range(8):
                        mm(psA[pi][0:ncol, :], win[:, c, c0:c0 + ncol], xb[:, c, :], c == 0, c == 7,
                           [r_win[c0 // 512], rxb], r_psA[pi], c == 0)
                    if kind == "g":
                        ins("scalar", "activation", [r_psA[pi]], [r_ost[oi]], out=ost[oi][0:ncol, :], in_=psA[pi][0:ncol, :], func=AF.Sigmoid)
                    elif kind == "q":
                        ins("vector", "tensor_scalar", [r_psA[pi]], [r_ost[oi]], out=ost[oi][0:ncol, :], in0=psA[pi][0:ncol, :],
                            scalar1=0.125, scalar2=None, op0=ALU.mult)
                    else:
                        ins("vector", "tensor_copy", [r_psA[pi]], [r_ost[oi]], out=ost[oi][0:ncol, :], in_=psA[pi][0:ncol, :])
                    dma("sync", dst(s), ost[oi][0:ncol, :], [r_ost[oi]], [])
                for tt in range(4):
                    t = 4 * s + tt
                    for vi, (c0, blks) in enumerate(((768, (1, 2)), (1920, (3, 4)))):
                        for c in range(8):
                            mm(psV[vi][:, 0:384], xb[:, c, tt * 128:(tt + 1) * 128], win[:, c, c0:c0 + 384], c == 0, c == 7,
                               [r_win[blks[0]], r_win[blks[1]], rxb], r_psV[vi], c == 0)
                        ins("vector", "tensor_copy", [r_psV[vi]], [r_vst[vi]], out=vst[vi][:, :], in_=psV[vi][:, 0:384])
                        if vi == 0:
                            dma("sync", VA_v[:, :, t, :], vst[vi][:, :].rearrange("p (h d) -> p h d", d=64), [r_vst[vi]], [])
                        else:
                            dma("sync", VB_v[:, :, t, :], vst[vi][:, :].rearrange("p (i d) -> p i d", d=128), [r_vst[vi]], [])
            K.emit()
        if stage <= 1:
            return nc

        with ExitStack() as es:
            sb = lambda n, s, d: es.enter_context(nc.sbuf_tensor(n, s, d))
            kaug = [sb("p2_k%d" % i, [128, S], BF16) for i in range(6)]
            qaug = [sb("p2_q%d" % i, [128, S], BF16) for i in range(6)]
            vaug = [sb("p2_v%d" % i, [128, 32, 64], BF16) for i in range(6)]
            identb = sb("p2_id", [128, 128], BF16)
            ones64 = sb("p2_ones", [128, 64], BF16)
            dfin = sb("p2_dfin", [128, 6, 2, 128], BF16)
            with ExitStack() as es2:
                sb2 = lambda n, s, d: es2.enter_context(nc.sbuf_tensor(n, s, d))
                r_k = [K.res() for _ in range(6)]
                r_kind = [K.res() for _ in range(6)]
                r_q = [K.res() for _ in range(6)]
                r_qm = [[K.res() for _ in range(NST)] for _ in range(6)]
                r_v = [K.res() for _ in range(6)]
                r_id = K.res(); r_ones = K.res(); r_dfin = K.res()
                dgf = sb2("p2_dgf", [128, 1536], F32); r_dgf = K.res()
                b31 = sb2("p2_b31", [128, 6], F32); r_b31 = K.res()
                neg0 = sb2("p2_neg0", [128, 128], F32); r_neg0 = K.res()
                gmadd = sb2("p2_gmadd", [128, 512], F32); r_gmadd = K.res()
                own01 = sb2("p2_own01", [128, 512], F32); r_own = K.res()
                ksum = sb2("p2_ksum", [128, 16], F32); r_ksum = K.res()
                kmb = [sb2("p2_kmb%d" % i, [128, 16], BF16) for i in range(2)]; r_kmb = [K.res() for _ in range(2)]
                gm = [sb2("p2_gm%d" % i, [128, 512], F32) for i in range(2)]; r_gm = [K.res() for _ in range(2)]
                m8 = sb2("p2_m8", [128, 32, 8], F32); r_m8 = K.res()
                mbf = sb2("p2_mbf", [128, 512], F32); r_mbf = K.res()
                mbpad = [sb2("p2_mbpad%d" % i, [128, 32, 80], BF16) for i in range(2)]; r_mbpad = [K.res() for _ in range(2)]
                gps = [es2.enter_context(nc.psum_tensor("p2_g%d" % i, [128, 512], F32)) for i in range(2)]; r_gps = [K.res() for _ in range(2)]
                tps = [es2.enter_context(nc.psum_tensor("p2_t%d" % i, [128, 512], F32)) for i in range(2)]; r_tps = [K.res() for _ in range(2)]
                dma("gpsimd", identb[:, :], I["identf"], [], [r_id])
                dma("sync", dgf[:, :], I["dg"], [], [r_dgf])
                dma("sync", b31[:, :], I["b31"], [], [r_b31])
                dma("sync", neg0[:, :], I["neg0"], [], [r_neg0])
                dma("sync", gmadd[:, :], I["gmadd"], [], [r_gmadd])
                dma("sync", own01[:, :], I["own01"], [], [r_own])
                for h in range(6):
                    dma("sync", kaug[h][0:64, :], KA[h], [], [r_k[h]])
                    dma("scalar", qaug[h][0:64, :], QA[h], [], [r_q[h]])
                    dma("sync", vaug[h][:, :, :], VA[h].rearrange("p (t d) -> p t d", d=64), [], [r_v[h]])
                    dma("gpsimd", kaug[h][64:128, :].rearrange("p (a n) -> p a n", n=2048), I["kind"].rearrange("p (a n) -> p a n", n=2048),
                        [], [r_kind[h]])
                    ins("gpsimd", "memset", [], r_qm[h], ap=qaug[h][64:128, :], constant=0.0)
                ins("gpsimd", "memset", [], [r_ones], ap=ones64[:, :], constant=1.0)
                for i in range(2):
                    ins("gpsimd", "memset", [], [r_mbpad[i]], ap=mbpad[i][:, :, :], constant=0.0)
                    ins("gpsimd", "memset", [], [r_kmb[i]], ap=kmb[i][:, :], constant=0.0)
                for h in range(6):
                    for t in range(2):
                        src = dgf[:, (h * 2 + t) * 128:(h * 2 + t + 1) * 128]
                        if t == 0:
                            ins("vector", "scalar_tensor_tensor", [r_dgf, r_b31, r_neg0], [r_dfin], append=True, out=dfin[:, h, t, :],
                                in0=src, scalar=b31[:, h:h + 1], in1=neg0[:, :], op0=ALU.subtract, op1=ALU.add)
                        else:
                            ins("vector", "tensor_scalar", [r_dgf, r_b31], [r_dfin], append=True, out=dfin[:, h, t, :], in0=src,
                                scalar1=b31[:, h:h + 1], scalar2=None, op0=ALU.subtract)

                def prologue1(h):
                    i = h % 2
                    ins("vector", "tensor_reduce", [r_k[h]], [r_ksum], out=ksum[0:64, :],
                        in_=kaug[h][0:64, :].rearrange("p (j k) -> p j k", k=256), axis=AX.X, op=ALU.add)
                    ins("vector", "tensor_copy", [r_ksum], [r_kmb[i]], out=kmb[i][0:64, :], in_=ksum[0:64, :])
                    for t in range(32):
                        mm(gps[i][:, t * 16:(t + 1) * 16], qaug[h][:, t * 128:(t + 1) * 128], kmb[i][:, :], True, True,
                           [r_q[h], r_kmb[i]] + r_qm[h], r_gps[i], t == 0)
                    ins("vector", "tensor_tensor", [r_gps[i], r_gmadd], [r_gm[i]], out=gm[i][:, :], in0=gps[i][:, :], in1=gmadd[:, :], op=ALU.add)
                    for t in range(32):
                        ins("vector", "max", [r_gm[i]], [r_m8], append=(t > 0), out=m8[:, t, :], in_=gm[i][:, t * 16:(t + 1) * 16])
                    ins("vector", "tensor_tensor", [r_gm[i], r_m8], [r_mbf], out=mbf[:, :].rearrange("p (t j) -> p t j", j=16),
                        in0=gm[i][:, :].rearrange("p (t j) -> p t j", j=16), in1=m8[:, :, 2:3].to_broadcast([128, 32, 16]), op=ALU.is_lt)
                    ins("vector", "scalar_tensor_tensor", [r_mbf, r_own], [r_mbpad[i]], out=mbpad[i][:, :, 64:80],
                        in0=mbf[:, :].rearrange("p (t j) -> p t j", j=16), scalar=NEGM,
                        in1=own01[:, :].rearrange("p (t j) -> p t j", j=16), op0=ALU.mult, op1=ALU.mult)

                def prologue2(h):
                    i = h % 2
                    for s in range(NST):
                        ti = s % 2
                        for c in range(4):
                            t = 4 * s + c
                            mm(tps[ti][0:80, c * 128:(c + 1) * 128], mbpad[i][:, t, :], identb[:, :], True, True, [r_mbpad[i], r_id], r_tps[ti], c == 0)
                        ins("scalar", "copy", [r_tps[ti]], [r_qm[h][s]], out=qaug[h][64:80, s * 512:(s + 1) * 512], in_=tps[ti][64:80, :])

                prologue1(0)
                for h in range(6):
                    if h + 1 < 6:
                        prologue1(h + 1)
                    prologue2(h)
                K.emit()
            with ExitStack() as es2:
                sb2 = lambda n, s, d: es2.enter_context(nc.sbuf_tensor(n, s, d))
                r_all = K.res()
                pt = [sb2("p2_pt%d" % i, [128, 1024], BF16) for i in range(2)]; r_pt = [K.res() for _ in range(2)]
                rden = [sb2("p2_rden%d" % i, [128, 512], F32) for i in range(2)]; r_rden = [K.res() for _ in range(2)]
                yo = [sb2("p2_yo%d" % i, [128, 512], BF16) for i in range(2)]; r_yo = [K.res() for _ in range(2)]
                sps = [es2.enter_context(nc.psum_tensor("p2_s%d" % i, [128, 1024], F32)) for i in range(2)]; r_sps = [K.res() for _ in range(2)]
                nps = [es2.enter_context(nc.psum_tensor("p2_n%d" % i, [128, 512], F32)) for i in range(2)]; r_nps = [K.res() for _ in range(2)]
                dps = [es2.enter_context(nc.psum_tensor("p2_d%d" % i, [128, 512], F32)) for i in range(2)]; r_dps = [K.res() for _ in range(2)]
                cnt2 = {"it": 0}

                def make_seg(h, s):
                    nkt = 4 * s + 4
                    a = (h * NST + s) % 2
                    steps = []
                    for j in range(nkt // 2):
                        st = {}
                        los = [max(0, 2 * j + h2 - 4 * s) * 128 for h2 in range(2)]

                        def A(j=j, st=st, los=los):
                            si = cnt2["it"] % 2
                            cnt2["it"] += 1
                            st["si"] = si
                            for h2 in range(2):
                                kt = 2 * j + h2
                                lo = los[h2]
                                base = h2 * 512
                                cd = kt - 4 * s
                                cp = kt - 4 * s + 1
                                has_d0 = 0 <= cd <= 3
                                has_d1 = 0 <= cp <= 3
                                mm(sps[si][:, base + lo:base + 512], kaug[h][:, kt * 128:(kt + 1) * 128], qaug[h][:, s * 512 + lo:(s + 1) * 512],
                                   True, not (has_d0 or has_d1), [r_all], r_sps[si], h2 == 0)
                                if has_d0:
                                    mm(sps[si][:, base + cd * 128:base + (cd + 1) * 128], identb[:, :], dfin[:, h, 0, :], False, not has_d1, [r_all], r_sps[si], False)
                                if has_d1:
                                    mm(sps[si][:, base + cp * 128:base + (cp + 1) * 128], identb[:, :], dfin[:, h, 1, :], False, True, [r_all], r_sps[si], False)

                        def B(j=j, st=st, los=los):
                            si = st["si"]
                            if los[0] == 0 and los[1] == 0:
                                ins("scalar", "activation", [r_sps[si]], [r_pt[si]], out=pt[si][:, :], in_=sps[si][:, :], func=AF.Exp)
                            else:
                                for h2 in range(2):
                                    lo = h2 * 512 + los[h2]
                                    hi = (h2 + 1) * 512
                                    ins("scalar", "activation", [r_sps[si]], [r_pt[si]], append=(h2 > 0), out=pt[si][:, lo:hi], in_=sps[si][:, lo:hi],
                                        func=AF.Exp)

                        def C(j=j, st=st, los=los):
                            si = st["si"]
                            for h2 in range(2):
                                kt = 2 * j + h2
                                lo = los[h2]
                                base = h2 * 512
                                mm(nps[a][0:64, lo:512], vaug[h][:, kt, :], pt[si][:, base + lo:base + 512], kt == 0, kt == nkt - 1, [r_all, r_pt[si]],
                                   r_nps[a], kt == 0)
                                mm(dps[a][0:64, lo:512], ones64[:, :], pt[si][:, base + lo:base + 512], kt == 0, kt == nkt - 1, [r_all, r_pt[si]],
                                   r_dps[a], kt == 0)

                        steps.append((A, B, C))

                    def post():
                        ins("vector", "reciprocal", [r_dps[a]], [r_rden[a]], out=rden[a][0:64, :], in_=dps[a][0:64, :])
                        ins("vector", "tensor_tensor", [r_nps[a], r_rden[a]], [r_yo[a]], out=yo[a][0:64, :], in0=nps[a][0:64, :],
                            in1=rden[a][0:64, :], op=ALU.mult)
                        dma("sync", YT[h, :, s * 512:(s + 1) * 512], yo[a][0:64, :], [r_yo[a]], [])
                    return {"steps": steps, "pre": None, "post": post, "hoist": True}

                segs = []
                for h in range(6):
                    for s in range(NST):
                        segs.append(make_seg(h, s))
                run_segments(segs)
                K.emit()
        if stage <= 2:
            return nc

        with ExitStack() as es:
            sb = lambda n, s, d: es.enter_context(nc.sbuf_tensor(n, s, d))
            ps = lambda n: es.enter_context(nc.psum_tensor(n, [128, 512], F32))
            kb2 = [sb("p3_k%d" % i, [128, S], BF16) for i in range(2)]
            qz = [[sb("p3_q%d_%d" % (i, j), [128, S], BF16) for j in range(2)] for i in range(2)]
            vb2 = [sb("p3_v%d" % i, [128, 32, 128], BF16) for i in range(2)]
            r_k = [K.res() for _ in range(2)]
            r_q = [K.res() for _ in range(2)]
            r_v = [K.res() for _ in range(2)]
            identb = sb("p3_id", [128, 128], BF16); r_id = K.res()
            sbneg = sb("p3_neg", [128, 4, 512], BF16); r_neg = K.res()
            u2 = sb("p3_u2", [128, 32, 64], BF16); r_u2 = K.res()
            sel = sb("p3_sel", [128, 32, 128], BF16); r_sel = K.res()
            tri = sb("p3_tri", [128, 128], BF16); r_tri = K.res()
            lbuf2 = [[sb("p3_l%d_%d" % (j, i), [128, 1024], BF16) for i in range(16)] for j in range(2)]
            r_l2 = [[K.res() for _ in range(16)] for _ in range(2)]
            ebuf = [sb("p3_e%d" % i, [128, 1024], F32) for i in range(2)]
            r_e = [K.res() for _ in range(2)]
            at = [sb("p3_a%d" % i, [128, 1024], BF16) for i in range(3)]
            r_at = [K.res() for _ in range(3)]
            rhl = [sb("p3_r%d" % i, [128, 512], BF16) for i in range(2)]
            r_rhl = [K.res() for _ in range(2)]
            yo = [sb("p3_yo%d" % i, [128, 512], BF16) for i in range(2)]
            r_yo = [K.res() for _ in range(2)]
            zb = [es.enter_context(nc.psum_tensor("p3_z%d" % i, [128, 1024], F32)) for i in range(3)]; r_zb = [K.res() for _ in range(3)]
            rps = [ps("p3_rm0")] * 2; r_rps = [K.res()] * 2
            ops_ = [ps("p3_o0")] * 2; r_ops = [K.res()] * 2
            dma("gpsimd", identb[:, :], I["identf"], [], [r_id])
            dma("gpsimd", sbneg[:, :, :], I["sbneg"].rearrange("p (c q) -> p c q", q=512), [], [r_neg])
            dma("gpsimd", u2[:, :, :], I["u2"].rearrange("p (k m) -> p k m", m=64), [], [r_u2])
            dma("gpsimd", sel[:, :, :], I["sel"].rearrange("p (k m) -> p k m", m=128), [], [r_sel])
            dma("gpsimd", tri[:, :], I["tri"], [], [r_tri])
            for b_ in range(2):
                ins("gpsimd", "memset", [], [r_rhl[b_]], ap=rhl[b_][:, :], constant=0.0)
                for j_ in range(2):
                    ins("gpsimd", "memset", [], [r_q[b_]], append=(j_ > 0), ap=qz[b_][j_][:, :], constant=0.0)

            def load_pair(i):
                b = i % 2
                dma("sync", kb2[b][:, :], KB[i], [], [r_k[b]])
                for j in range(2):
                    dma("sync", qz[b][j][64 * j:64 * j + 64, :], QB[i, 64 * j:64 * j + 64, :], [], [r_q[b]], append=(j > 0))
                dma("sync", vb2[b][:, :, :], VB[i].rearrange("p (t d) -> p t d", d=128), [], [r_v[b]])

            cnt3 = {"z": 0, "e": 0}

            def make_unit(i, hp, s):
                b = i % 2
                p0 = 64 * hp
                hh = 2 * i + hp
                nkt = 4 * s + 4
                a = (hh * NST + s) % 2
                lbuf = lbuf2[a]
                r_l = r_l2[a]
                qs = qz[b][hp][:, s * 512:(s + 1) * 512]
                steps1 = []
                steps2 = []
                for j in range(nkt // 2):
                    st1 = {}
                    st2 = {}

                    def A1(j=j, st=st1):
                        zi = cnt3["z"] % 3
                        cnt3["z"] += 1
                        st["zi"] = zi
                        for h2 in range(2):
                            kt = 2 * j + h2
                            diag = kt >= 4 * s
                            zo = zb[zi][:, h2 * 512:(h2 + 1) * 512]
                            mm(zo, kb2[b][:, kt * 128:(kt + 1) * 128], qs, True, not diag, [r_k[b], r_q[b]], r_zb[zi], h2 == 0)
                            if diag:
                                mm(zo, identb[:, :], sbneg[:, kt - 4 * s, :], False, True, [r_id, r_neg], r_zb[zi], False)

                    def B1(j=j, st=st1):
                        zi = st["zi"]
                        ei = cnt3["e"] % 2
                        cnt3["e"] += 1
                        ins("scalar", "activation", [r_zb[zi]], [r_e[ei]], out=ebuf[ei][:, :], in_=zb[zi][:, :], func=AF.Exp)
                        ins("scalar", "activation", [r_e[ei]], [r_l[j]], out=lbuf[j][:, :], in_=ebuf[ei][:, :], func=AF.Ln, bias=1.0, scale=1.0)

                    def C1(j=j, st=st1):
                        for h2 in range(2):
                            kt = 2 * j + h2
                            mm(rps[a][0:64, :], u2[:, kt, :], lbuf[j][:, h2 * 512:(h2 + 1) * 512], kt == 0, kt == nkt - 1, [r_u2, r_l[j]],
                               r_rps[a], kt == 0)

                    def A2(j=j, st=st2):
                        zi = cnt3["z"] % 3
                        cnt3["z"] += 1
                        st["zi"] = zi
                        for h2 in range(2):
                            kt = 2 * j + h2
                            diag = kt >= 4 * s
                            zo = zb[zi][:, h2 * 512:(h2 + 1) * 512]
                            mm(zo, kb2[b][:, kt * 128:(kt + 1) * 128], qs, True, False, [r_k[b], r_q[b]], r_zb[zi], h2 == 0)
                            if diag:
                                mm(zo, identb[:, :], sbneg[:, kt - 4 * s, :], False, False, [r_id, r_neg], r_zb[zi], False)
                            mm(zo, tri[:, :], lbuf[j][:, h2 * 512:(h2 + 1) * 512], False, False, [r_tri, r_l[j]], r_zb[zi], False)
                            mm(zo, sel[:, kt, :], rhl[a][:, :], False, True, [r_sel, r_rhl[a]], r_zb[zi], False)

                    def B2(j=j, st=st2):
                        zi = st["zi"]
                        ins("scalar", "activation", [r_zb[zi]], [r_at[zi]], out=at[zi][:, :], in_=zb[zi][:, :], func=AF.Exp)

                    def C2(j=j, st=st2):
                        zi = st["zi"]
                        for h2 in range(2):
                            kt = 2 * j + h2
                            mm(ops_[a][0:64, :], vb2[b][:, kt, p0:p0 + 64], at[zi][:, h2 * 512:(h2 + 1) * 512], kt == 0, kt == nkt - 1,
                               [r_v[b], r_at[zi]], r_ops[a], kt == 0)

                    steps1.append((A1, B1, C1))
                    steps2.append((A2, B2, C2))

                def pre2():
                    ins("vector", "tensor_copy", [r_rps[a]], [r_rhl[a]], out=rhl[a][0:64, :], in_=rps[a][0:64, :])
                    ins("vector", "tensor_tensor", [r_rps[a], r_rhl[a]], [r_rhl[a]], out=rhl[a][32:64, :], in0=rps[a][32:64, :],
                        in1=rhl[a][32:64, :], op=ALU.subtract)

                def post2():
                    ins("vector", "tensor_copy", [r_ops[a]], [r_yo[a]], out=yo[a][0:64, :], in_=ops_[a][0:64, :])
                    dma("sync", YT[6 + hh, :, s * 512:(s + 1) * 512], yo[a][0:64, :], [r_yo[a]], [])
                    if hp == 1 and s == NST - 1 and i + 2 < 3:
                        load_pair(i + 2)

                return (steps1, steps2, pre2, post2)

            def interleave(x, y):
                out = []
                i = j = 0
                while i < len(x) or j < len(y):
                    if j >= len(y) or (i < len(x) and i * len(y) <= j * len(x)):
                        out.append(x[i]); i += 1
                    else:
                        out.append(y[j]); j += 1
                return out

            load_pair(0)
            load_pair(1)
            units = []
            for i in range(3):
                for hp in range(2):
                    for s in range(NST):
                        units.append(make_unit(i, hp, s))
            segs = [{"steps": units[0][0], "pre": None, "post": None, "hoist": False}]
            for u in range(len(units)):
                nxt1 = units[u + 1][0] if u + 1 < len(units) else []
                segs.append({"steps": nxt1[0:2] + interleave(units[u][1], nxt1[2:]), "pre": units[u][2], "post": units[u][3],
                             "hoist": len(nxt1) >= 2})
            run_segments(segs)
            K.emit()
        if stage <= 3:
            return nc

        with ExitStack() as es:
            sb = lambda n, s, d: es.enter_context(nc.sbuf_tensor(n, s, d))
            ps = lambda n: es.enter_context(nc.psum_tensor(n, [128, 512], F32))
            memT = sb("p4_mem", [128, 8, 256], BF16); r_mem = K.res()
            wkv = sb("p4_wkv", [128, 8, 512], BF16); r_wkv = K.res()
            km = sb("p4_km", [128, 2, 256], BF16); r_km = K.res()
            vm = sb("p4_vm", [128, 2, 256], BF16); r_vm = K.res()
            qmz = [sb("p4_qm%d" % i, [128, S], BF16) for i in range(4)]; r_qm = K.res()
            ones64 = sb("p4_ones", [128, 64], BF16); r_ones = K.res()
            pt4 = [sb("p4_pt%d" % i, [128, 1024], BF16) for i in range(2)]
            r_pt = [K.res() for _ in range(2)]
            rden = [sb("p4_rden%d" % i, [128, 512], F32) for i in range(2)]
            r_rden = [K.res() for _ in range(2)]
            yo = [sb("p4_yo%d" % i, [128, 512], BF16) for i in range(2)]
            r_yo = [K.res() for _ in range(2)]
            sps4 = [es.enter_context(nc.psum_tensor("p4_s%d" % i, [128, 1024], F32)) for i in range(2)]; r_sps = [K.res() for _ in range(2)]
            nps = [ps("p4_n%d" % i) for i in range(2)]; r_nps = [K.res() for _ in range(2)]
            dps = [ps("p4_d%d" % i) for i in range(2)]; r_dps = [K.res() for _ in range(2)]
            dma("gpsimd", memT[:, :, :], I["memT"].rearrange("(c p) m -> p c m", p=128), [], [r_mem])
            dma("gpsimd", wkv[:, :, :], I["w_kv"].rearrange("(c p) n -> p c n", p=128), [], [r_wkv])
            r_qmz = [K.res() for _ in range(4)]
            for hm_ in range(4):
                ins("gpsimd" if hm_ % 2 == 0 else "vector", "memset", [], [r_qmz[hm_]], ap=qmz[hm_][:, :], constant=0.0)
            for hm_ in range(4):
                p_ = 64 * (hm_ % 2)
                dma("sync" if hm_ % 2 == 0 else "scalar", qmz[hm_][p_:p_ + 64, :], QM[hm_ // 2, p_:p_ + 64, :], [], [r_qmz[hm_]])
            ins("gpsimd", "memset", [], [r_ones], ap=ones64[:, :], constant=1.0)
            for i in range(2):
                for c in range(8):
                    mm(sps4[i][:, 0:256], wkv[:, c, i * 128:(i + 1) * 128], memT[:, c, :], c == 0, c == 7, [r_wkv, r_mem], r_sps[i], c == 0)
                ins("vector", "tensor_copy", [r_sps[i]], [r_km], append=(i > 0), out=km[:, i, :], in_=sps4[i][:, 0:256])
            for j in range(2):
                for c in range(8):
                    mm(nps[j][:, 0:256], memT[:, c, j * 128:(j + 1) * 128], wkv[:, c, 256:512], c == 0, c == 7, [r_wkv, r_mem], r_nps[j], c == 0)
                ins("vector", "tensor_copy", [r_nps[j]], [r_vm], append=(j > 0), out=vm[:, j, :], in_=nps[j][:, 0:256])
            def make_seg4(hm, s):
                i = hm // 2
                a = (hm * NST + s) % 2
                st = {}

                def A():
                    si = (hm * NST + s) % 2
                    st["si"] = si
                    for j in range(2):
                        mm(sps4[si][:, j * 512:(j + 1) * 512], km[:, i, j * 128:(j + 1) * 128], qmz[hm][:, s * 512:(s + 1) * 512], True, True,
                           [r_km, r_qmz[hm]], r_sps[si], j == 0)

                def B():
                    si = st["si"]
                    ins("scalar", "activation", [r_sps[si]], [r_pt[si]], out=pt4[si][:, :], in_=sps4[si][:, :], func=AF.Exp)

                def C():
                    si = st["si"]
                    for j in range(2):
                        mm(nps[a][0:64, :], vm[:, j, hm * 64:(hm + 1) * 64], pt4[si][:, j * 512:(j + 1) * 512], j == 0, j == 1, [r_vm, r_pt[si]], r_nps[a], j == 0)
                        mm(dps[a][0:64, :], ones64[:, :], pt4[si][:, j * 512:(j + 1) * 512], j == 0, j == 1, [r_ones, r_pt[si]], r_dps[a], j == 0)

                def post():
                    ins("scalar", "activation", [r_dps[a]], [r_rden[a]], out=rden[a][0:64, :], in_=dps[a][0:64, :], func=AF.Ln)
                    ins("scalar", "activation", [r_rden[a]], [r_rden[a]], out=rden[a][0:64, :], in_=rden[a][0:64, :], func=AF.Exp, scale=-1.0)
                    ins("vector", "tensor_tensor", [r_nps[a], r_rden[a]], [r_yo[a]], out=yo[a][0:64, :], in0=nps[a][0:64, :],
                        in1=rden[a][0:64, :], op=ALU.mult)
                    dma("sync", YT[12 + hm, :, s * 512:(s + 1) * 512], yo[a][0:64, :], [r_yo[a]], [])
                return {"steps": [(A, B, C)], "pre": None, "post": post, "hoist": True}

            segs = []
            for hm in range(4):
                for s in range(NST):
                    segs.append(make_seg4(hm, s))
            A0, B0, C0 = segs[0]["steps"][0]
            A0(); B0()
            for u in range(len(segs)):
                if u + 1 < len(segs):
                    An, Bn, Cn = segs[u + 1]["steps"][0]
                    An(); Bn()
                segs[u]["steps"][0][2]()
                segs[u]["post"]()
            K.emit()
        if stage <= 4:
            return nc

        with ExitStack() as es:
            sb = lambda n, s, d: es.enter_context(nc.sbuf_tensor(n, s, d))
            ps = lambda n: es.enter_context(nc.psum_tensor(n, [128, 512], F32))
            wbr = sb("p5_wbr", [128, 8, D], BF16); r_wbr = K.res()
            wout = sb("p5_wout", [128, 8, D], BF16); r_wout = K.res()
            g1 = sb("p5_g1", [128, D], F32); r_g1 = K.res()
            b1 = sb("p5_b1", [128, D], F32); r_b1 = K.res()
            wr = sb("p5_wr", [128, 8, 36], F32); r_wr = K.res()
            brr = sb("p5_br", [128, 36], F32); r_br = K.res()
            identf = sb("p5_idf", [128, 128], F32); r_idf = K.res()
            tst = sb("p5_tst", [128, 128], BF16); r_tst = K.res()
            onesb = sb("p5_ones", [128, 128], BF16); r_onesb = K.res()
            cbe = sb("p5_cbe", [128, 32], F32); r_cbe = K.res()
            ytl = [sb("p5_y%d" % i, [128, 8, 512], BF16) for i in range(2)]; r_y = [K.res() for _ in range(2)]
            gl = sb("p5_gl", [128, 24, 512], BF16); r_gl = [K.res() for _ in range(8)]
            acc = [sb("p5_acc%d" % i, [128, 512], F32) for i in range(2)]; r_acc = [K.res() for _ in range(2)]
            a1 = [sb("p5_a1%d" % i, [128, 512], F32) for i in range(2)]; r_a1 = [K.res() for _ in range(2)]
            c2 = [sb("p5_c2%d" % i, [128, 512], F32) for i in range(2)]; r_c2 = [K.res() for _ in range(2)]
            mgb = [sb("p5_mg%d" % i, [128, 8, 512], BF16) for i in range(2)]; r_mg = [K.res() for _ in range(2)]
            xtok = [sb("p5_xt%d" % i, [128, D], F32) for i in range(4)]; r_x = [K.res() for _ in range(4)]
            x1 = sb("p5_xone", [128, 4, D], F32); r_x1 = [K.res() for _ in range(4)]
            x1a = [sb("p5_x1a%d" % i, [128, D], F32) for i in range(2)]; r_x1a = [K.res() for _ in range(2)]
            x1b = [sb("p5_x1b%d" % i, [128, 4, D], BF16) for i in range(2)]; r_x1b = [[K.res() for _ in range(4)] for _ in range(2)]
            x1T = sb("p5_x1T", [128, 8, 512], F32); r_x1T = [K.res() for _ in range(4)]
            st6 = sb("p5_st6", [128, 4, 12], F32); r_st6 = K.res()
            mv = sb("p5_mv", [128, 4, 2], F32); r_mv = K.res()
            sm = sb("p5_sm", [128, 3, 4], F32); r_sm = K.res()
            lg4 = sb("p5_lg4", [128, 4, 36], F32); r_lg = K.res()
            gmax = sb("p5_gmax", [128, 4], F32); r_gmax = K.res()
            goh = sb("p5_goh", [128, 16], F32); r_goh = K.res()
            dd = sb("p5_dd", [128, 16], F32); r_dd = K.res()
            gex = sb("p5_gex", [128, 16], F32); r_gex = K.res()
            gp = sb("p5_gp", [128, 4], F32); r_gp = K.res()
            lem = sb("p5_lem", [128, 128], F32); r_lem = K.res()
            e8 = sb("p5_e8", [128, 4, 8], F32); r_e8 = K.res()
            oh1 = sb("p5_oh1", [128, 128], F32); r_oh1 = K.res()
            oh2 = sb("p5_oh2", [128, 128], F32); r_oh2 = K.res()
            ohb = sb("p5_ohb", [128, 128], BF16); r_ohb = K.res()
            wt = sb("p5_wt", [128, 4], F32); r_wt = K.res()
            cnt = sb("p5_cnt", [128, 128], F32); r_cnt = K.res()
            t128 = sb("p5_t128", [128, 128], F32); r_t128 = K.res()
            slf = sb("p5_slf", [128, 8], F32); r_slf = K.res()
            r_slot = K.res()
            r_gate = K.res()
            bps = [ps("p5_pb%d" % i) for i in range(3)]; r_bps = [K.res() for _ in range(3)]
            mps = [ps("p5_pm%d" % i) for i in range(2)]; r_mps = [K.res() for _ in range(2)]
            tps2 = [ps("p5_pt%d" % i) for i in range(2)]; r_tps2 = [K.res() for _ in range(2)]
            sps5 = ps("p5_ps"); r_lgps = K.res(); r_cps = K.res()
            dma("gpsimd", wbr[:, :, :], I["wbr"].rearrange("(c p) n -> p c n", p=128), [], [r_wbr])
            dma("gpsimd", wout[:, :, :], I["w_out"].rearrange("(c p) n -> p c n", p=128), [], [r_wout])
            dma("sync", g1[:, :], I["ln1g"], [], [r_g1])
            dma("sync", b1[:, :], I["ln1b"], [], [r_b1])
            dma("sync", wr[:, :, :], I["w_r"].rearrange("(c p) n -> p c n", p=128), [], [r_wr])
            dma("sync", brr[:, :], I["b_r"], [], [r_br])
            dma("sync", identf[:, :], I["identf"], [], [r_idf])
            dma("gpsimd", tst[:, :], I["tst"], [], [r_tst])
            dma("sync", cbe[:, :], I["ebase"], [], [r_cbe])
            ins("gpsimd", "memset", [], [r_onesb], ap=onesb[:, :], constant=1.0)
            YT_v = YT.rearrange("(c two) p t -> c (two p) t", two=2).rearrange("c q t -> q c t")
            G_v = G.rearrange("(b m) p t -> p m b t", b=3)
            gl_v = gl[:, :, :].rearrange("p (m b) t -> p m b t", b=3)
            heads = ((0, 3), (3, 6), (6, 8))
            cnt5 = {"b": 0}

            def load_y(s):
                dma("sync", ytl[s % 2][:, :, :], YT_v[:, :, s * 512:(s + 1) * 512], [], [r_y[s % 2]])

            def load_g(s, m):
                dma("sync", gl_v[:, m, :, :], G_v[:, m, :, s * 512:(s + 1) * 512], [], [r_gl[m]])

            def M(s, pieces=None):
                b = s % 2
                for m in range(8):
                    if pieces is not None and m < len(pieces):
                        pieces[m]()
                    a = m % 2
                    pbs = []
                    for br in range(3):
                        pb = cnt5["b"] % 3
                        cnt5["b"] += 1
                        pbs.append(pb)
                        h0, h1 = heads[br]
                        for hh in range(h0, h1):
                            mm(bps[pb][:, :], wbr[:, hh, m * 128:(m + 1) * 128], ytl[b][:, hh, :], hh == h0, hh == h1 - 1,
                               [r_wbr, r_y[b]], r_bps[pb], hh == h0)
                    ins("vector", "tensor_tensor", [r_bps[pbs[0]], r_gl[m]], [r_acc[a]], out=acc[a][:, :], in0=bps[pbs[0]][:, :],
                        in1=gl[:, 3 * m + 0, :], op=ALU.mult)
                    ins("vector", "tensor_tensor", [r_bps[pbs[1]], r_gl[m]], [r_a1[a]], out=a1[a][:, :], in0=bps[pbs[1]][:, :],
                        in1=gl[:, 3 * m + 1, :], op=ALU.mult)
                    ins("vector", "tensor_tensor", [r_bps[pbs[2]], r_gl[m]], [r_c2[a]], out=c2[a][:, :], in0=bps[pbs[2]][:, :],
                        in1=gl[:, 3 * m + 2, :], op=ALU.mult)
                    ins("gpsimd", "tensor_tensor", [r_acc[a], r_a1[a]], [r_acc[a]], out=acc[a][:, :], in0=acc[a][:, :], in1=a1[a][:, :], op=ALU.add)
                    ins("gpsimd", "tensor_tensor", [r_acc[a], r_c2[a]], [r_mg[b]], append=(m > 0),
                        out=mgb[b][:, m, :], in0=acc[a][:, :], in1=c2[a][:, :], op=ALU.add)
                    if s + 1 < NST:
                        load_g(s + 1, m)
                if s + 2 < NST:
                    load_y(s + 2)

            def load_x(s):
                for tt in range(4):
                    t = 4 * s + tt
                    dma("scalar", xtok[tt][:, :], I["x"][t * 128:(t + 1) * 128, :], [], [r_x[tt]])

            def XA(s):
                b = s % 2
                for tt in range(4):
                    for half in range(2):
                        for c in range(8):
                            mm(mps[half][:, :], mgb[b][:, c, tt * 128:(tt + 1) * 128], wout[:, c, half * 512:(half + 1) * 512], c == 0, c == 7,
                               [r_mg[b], r_wout], r_mps[half], c == 0)
                        ins("vector", "scalar_tensor_tensor", [r_x[tt], r_mps[half]], [r_x[tt]],
                            out=xtok[tt][:, half * 512:(half + 1) * 512], in0=xtok[tt][:, half * 512:(half + 1) * 512], scalar=ALPHA,
                            in1=mps[half][:, :], op0=ALU.mult, op1=ALU.add)
                    for half in range(2):
                        ins("vector", "bn_stats", [r_x[tt]], [r_st6], append=(tt + half > 0), out=st6[:, tt, half * 6:(half + 1) * 6],
                            in_=xtok[tt][:, half * 512:(half + 1) * 512])
                for tt in range(4):
                    ins("vector", "bn_aggr", [r_st6], [r_mv], append=(tt > 0), out=mv[:, tt, :], in_=st6[:, tt, :])
                ins("scalar", "activation", [r_mv], [r_sm], out=sm[:, 0, :], in_=mv[:, :, 1], func=AF.Ln, bias=EPS, scale=1.0)
                ins("scalar", "activation", [r_sm], [r_sm], out=sm[:, 1, :], in_=sm[:, 0, :], func=AF.Exp, scale=-0.5)
                ins("vector", "scalar_tensor_tensor", [r_mv, r_sm], [r_sm], out=sm[:, 2, :], in0=mv[:, :, 0], scalar=-1.0,
                    in1=sm[:, 1, :], op0=ALU.mult, op1=ALU.mult)

            def XB(s):
                b = s % 2
                for tt in range(4):
                    ins("scalar", "activation", [r_x[tt], r_sm], [r_x1[tt]], out=x1[:, tt, :], in_=xtok[tt][:, :], func=AF.Identity,
                        bias=sm[:, 2, tt:tt + 1], scale=sm[:, 1, tt:tt + 1])
                for tt in range(4):
                    ins("vector", "tensor_tensor", [r_x1[tt], r_g1], [r_x1[tt]], out=x1[:, tt, :], in0=x1[:, tt, :], in1=g1[:, :], op=ALU.mult)
                    ins("vector", "tensor_tensor", [r_x1[tt], r_b1], [r_x1[tt]], out=x1[:, tt, :], in0=x1[:, tt, :], in1=b1[:, :], op=ALU.add)
                for tt in range(4):
                    t = 4 * s + tt
                    u = t % 2
                    ins("scalar", "mul", [r_x1[tt]], [r_x1a[u]], out=x1a[u][:, :], in_=x1[:, tt, :], mul=ALPHA)
                    ins("scalar", "copy", [r_x1[tt]], [r_x1b[b][tt]], out=x1b[b][:, tt, :], in_=x1[:, tt, :])
                    dma("sync", X1A[t * 128:(t + 1) * 128, :], x1a[u][:, :], [r_x1a[u]], [])

            def T(s):
                def TR(tt):
                    for c in range(8):
                        hf = c // 4
                        K.op("tensor", (lambda e, c=c, hf=hf, tt=tt: e.transpose(out=tps2[hf][:, (c % 4) * 128:(c % 4 + 1) * 128],
                                                                                 in_=x1[:, tt, c * 128:(c + 1) * 128], identity=identf[:, :])),
                             [r_x1[tt], r_idf], [r_tps2[hf]], append=(c % 4 > 0))

                def CP(tt):
                    for hf in range(2):
                        ins("scalar", "copy", [r_tps2[hf]], [r_x1T[tt]], append=(hf > 0), out=x1T[:, hf * 4:(hf + 1) * 4, tt * 128:(tt + 1) * 128],
                            in_=tps2[hf][:, :].rearrange("p (c t) -> p c t", t=128))

                def MMr(tt):
                    for c in range(8):
                        mm(sps5[:, tt * 36:(tt + 1) * 36], x1T[:, c, tt * 128:(tt + 1) * 128], wr[:, c, :], c == 0, c == 7, [r_x1T[tt], r_wr],
                           r_lgps, tt == 0 and c == 0)

                return [lambda: TR(0), lambda: (CP(0), TR(1)), lambda: MMr(0), lambda: (CP(1), TR(2)), lambda: MMr(1),
                        lambda: (CP(2), TR(3)), lambda: MMr(2), lambda: (CP(3), MMr(3))]

            v4 = lambda ap, n: ap.rearrange("p (t n) -> p t n", n=n)

            def Ra(s):
                ins("vector", "tensor_tensor", [r_lgps, r_br], [r_lg], out=lg4[:, :, :], in0=v4(sps5[:, 0:144], 36),
                    in1=brr[:, :].unsqueeze(1).to_broadcast([128, 4, 36]), op=ALU.add)
                ins("vector", "tensor_reduce", [r_lg], [r_gmax], out=gmax[:, :], in_=lg4[:, :, 0:4], axis=AX.X, op=ALU.max)
                ins("vector", "tensor_tensor", [r_lg, r_gmax], [r_goh], out=v4(goh[:, :], 4), in0=lg4[:, :, 0:4],
                    in1=gmax[:, :].unsqueeze(2).to_broadcast([128, 4, 4]), op=ALU.is_equal)
                ins("vector", "tensor_tensor", [r_lg, r_gmax], [r_dd], out=v4(dd[:, :], 4), in0=lg4[:, :, 0:4],
                    in1=gmax[:, :].unsqueeze(2).to_broadcast([128, 4, 4]), op=ALU.subtract)
                ins("scalar", "activation", [r_dd], [r_gex], out=gex[:, :], in_=dd[:, :], func=AF.Exp)
                ins("vector", "tensor_reduce", [r_gex], [r_gp], out=gp[:, :], in_=v4(gex[:, :], 4), axis=AX.X, op=ALU.add)
                ins("vector", "reciprocal", [r_gp], [r_gp], out=gp[:, :], in_=gp[:, :])
                ins("vector", "tensor_scalar", [r_goh], [r_goh], out=goh[:, :], in0=goh[:, :], scalar1=-1.0, scalar2=1e30,
                    op0=ALU.add, op1=ALU.mult)
                ins("vector", "tensor_tensor", [r_lg, r_goh], [r_lem], out=lem[:, :].rearrange("p (t g e) -> p t g e", g=4, e=8),
                    in0=lg4[:, :, 4:36].rearrange("p t (g e) -> p t g e", e=8),
                    in1=v4(goh[:, :], 4).unsqueeze(3).to_broadcast([128, 4, 4, 8]), op=ALU.add)

            def Rb(s):
                for tt in range(4):
                    ins("vector", "max", [r_lem], [r_e8], append=(tt > 0), out=e8[:, tt, :], in_=lem[:, tt * 32:(tt + 1) * 32])
                ins("vector", "tensor_tensor", [r_lem, r_e8], [r_oh1], out=v4(oh1[:, :], 32), in0=v4(lem[:, :], 32),
                    in1=e8[:, :, 0:1].to_broadcast([128, 4, 32]), op=ALU.is_equal)
                ins("vector", "tensor_tensor", [r_lem, r_e8], [r_oh2], out=v4(oh2[:, :], 32), in0=v4(lem[:, :], 32),
                    in1=e8[:, :, 1:2].to_broadcast([128, 4, 32]), op=ALU.is_equal)
                ins("vector", "tensor_tensor", [r_e8], [r_wt], out=wt[:, :], in0=e8[:, :, 1], in1=e8[:, :, 0], op=ALU.subtract)
                ins("scalar", "activation", [r_wt], [r_wt], out=wt[:, :], in_=wt[:, :], func=AF.Exp)
                ins("vector", "tensor_scalar", [r_wt], [r_wt], out=wt[:, :], in0=wt[:, :], scalar1=1.0, scalar2=None, op0=ALU.add)
                ins("vector", "reciprocal", [r_wt], [r_wt], out=wt[:, :], in_=wt[:, :])
                gs = gateS[:, 8 * s:8 * s + 8].rearrange("p (t k) -> p t k", k=2)
                ins("vector", "tensor_tensor", [r_wt, r_gp], [r_gate], append=True, out=gs[:, :, 0], in0=wt[:, :], in1=gp[:, :], op=ALU.mult)
                ins("vector", "tensor_tensor", [r_gp, r_gate], [r_gate], append=True, out=gs[:, :, 1], in0=gp[:, :], in1=gs[:, :, 0], op=ALU.subtract)
                ins("vector", "tensor_tensor", [r_oh1, r_oh2], [r_ohb], out=ohb[:, :], in0=oh1[:, :], in1=oh2[:, :], op=ALU.add)

            def Rc(s):
                b = s % 2
                for tt in range(4):
                    mm(sps5[:, 256 + tt * 32:256 + (tt + 1) * 32], tst[:, :], ohb[:, tt * 32:(tt + 1) * 32], True, tt == 0,
                       [r_tst, r_ohb], r_cps, tt == 0)
                    for t2 in range(tt):
                        mm(sps5[:, 256 + tt * 32:256 + (tt + 1) * 32], onesb[:, :], ohb[:, t2 * 32:(t2 + 1) * 32], False, t2 == tt - 1,
                           [r_onesb, r_ohb], r_cps, False)
                for tt in range(4):
                    mm(sps5[:, 384:416], onesb[:, :], ohb[:, tt * 32:(tt + 1) * 32], tt == 0, tt == 3, [r_onesb, r_ohb], r_cps, False)
                ins("vector", "tensor_tensor", [r_cps, r_cbe], [r_cnt], out=v4(cnt[:, :], 32), in0=v4(sps5[:, 256:384], 32),
                    in1=cbe[:, :].unsqueeze(1).to_broadcast([128, 4, 32]), op=ALU.add)
                ins("vector", "tensor_tensor", [r_cps, r_cbe], [r_cbe], out=cbe[:, :], in0=sps5[:, 384:416], in1=cbe[:, :], op=ALU.add)
                sl = slf[:, :].rearrange("p (t k) -> p t k", k=2)
                ins("vector", "tensor_tensor", [r_cnt, r_oh1], [r_t128], out=t128[:, :], in0=cnt[:, :], in1=oh1[:, :], op=ALU.mult)
                ins("vector", "tensor_reduce", [r_t128], [r_slf], out=sl[:, :, 0], in_=v4(t128[:, :], 32), axis=AX.X, op=ALU.add)
                ins("vector", "tensor_tensor", [r_cnt, r_oh2], [r_t128], out=t128[:, :], in0=cnt[:, :], in1=oh2[:, :], op=ALU.mult)
                ins("vector", "tensor_reduce", [r_t128, r_slf], [r_slf], out=sl[:, :, 1], in_=v4(t128[:, :], 32), axis=AX.X, op=ALU.add)
                ins("vector", "tensor_copy", [r_slf], [r_slot], append=True, out=slotS[:, 8 * s:8 * s + 8], in_=slf[:, :])
                for tt in range(4):
                    for k in range(2):
                        col = 8 * s + 2 * tt + k
                        K.op("gpsimd", (lambda e, col=col, tt=tt, b=b: e.indirect_dma_start(
                            out=XP, out_offset=bass.IndirectOffsetOnAxis(ap=slotS[:, col:col + 1], axis=0),
                            in_=x1b[b][:, tt, :], in_offset=None)), [r_x1b[b][tt], r_slot], [], dma=True)

            load_y(0)
            load_y(1)
            for m in range(8):
                load_g(0, m)

            load_x(0)
            M(0)
            XA(0)
            XB(0)
            for s in range(NST):
                tp_ = T(s)
                if s + 1 < NST:
                    load_x(s + 1)
                    M(s + 1, tp_)
                else:
                    for f in tp_:
                        f()
                Ra(s)
                if s + 1 < NST:
                    XA(s + 1)
                Rb(s)
                if s + 1 < NST:
                    XB(s + 1)
                Rc(s)
            if debug:
                dma("sync", SLOTD, slotS[:, :], [r_slot], [])
                dma("sync", GATED, gateS[:, :], [r_gate], [])
            K.emit()
        if stage <= 5:
            return nc

        with ExitStack() as es:
            sb = lambda n, s, d: es.enter_context(nc.sbuf_tensor(n, s, d))
            ps = lambda n, dt=F32, w=512: es.enter_context(nc.psum_tensor(n, [128, w], dt))
            wg = [sb("p6_wg%d" % i, [128, 8, 512], BF16) for i in range(2)]; r_wg = [K.res() for _ in range(2)]
            wu = [sb("p6_wu%d" % i, [128, 8, 512], BF16) for i in range(2)]; r_wu = [K.res() for _ in range(2)]
            wd = [sb("p6_wd%d" % i, [128, 4, D], BF16) for i in range(2)]; r_wd = [K.res() for _ in range(2)]
            xp = [sb("p6_xp%d" % i, [128, NT, D], BF16) for i in range(2)]; r_xp = [K.res() for _ in range(2)]
            xpT = [sb("p6_xpT%d" % i, [128, 8, CAP], BF16) for i in range(2)]; r_xpT = [K.res() for _ in range(2)]
            identb = sb("p6_id", [128, 128], BF16); r_id = K.res()
            sg = [sb("p6_sg%d" % i, [128, 512], F32) for i in range(2)]; r_sg = [K.res() for _ in range(2)]
            hT = [sb("p6_hT%d" % i, [128, 4, CAP], BF16) for i in range(2)]; r_hT = [K.res() for _ in range(2)]
            yb = [sb("p6_y%d" % i, [128, D], BF16) for i in range(4)]; r_yb = [K.res() for _ in range(4)]
            tp = [ps("p6_pt%d" % i, BF16, 1024) for i in range(2)]; r_tp = [K.res() for _ in range(2)]
            gp = [ps("p6_pg%d" % i) for i in range(2)]; r_gp = [K.res() for _ in range(2)]
            up = [ps("p6_pu%d" % i) for i in range(2)]; r_up = [K.res() for _ in range(2)]
            yp = [ps("p6_py%d" % i) for i in range(2)]; r_yp = [K.res() for _ in range(2)]
            dma("gpsimd", identb[:, :], I["identf"], [], [r_id])
            XP_v = XP.rearrange("(e i p) d -> e p i d", p=128, i=NT)
            YP_v = YP.rearrange("(e i p) d -> e i p d", p=128, i=NT)

            def load_e(e):
                b = e % 2
                dma("gpsimd", wg[b][:, :, :], I["w_gate"][e].rearrange("(c p) n -> p c n", p=128), [], [r_wg[b]])
                dma("gpsimd", wu[b][:, :, :], I["w_up"][e].rearrange("(c p) n -> p c n", p=128), [], [r_wu[b]])
                for hf in range(2):
                    dma("gpsimd", wd[b][:, :, hf * 512:(hf + 1) * 512], I["w_down"][e].rearrange("(c p) n -> p c n", p=128)[:, :, hf * 512:(hf + 1) * 512],
                        [], [r_wd[b]], append=(hf > 0))
                dma("sync", xp[b][:, :, :], XP_v[e], [], [r_xp[b]])

            cnt6 = {"t": 0, "j": 0, "y": 0}

            def TRP(e):
                b = e % 2
                for i in range(NT):
                    tb = cnt6["t"] % 2
                    cnt6["t"] += 1
                    for c in range(8):
                        K.op("tensor", (lambda en, i=i, c=c, b=b, tb=tb: en.transpose(out=tp[tb][:, c * 128:(c + 1) * 128],
                                                                                      in_=xp[b][:, i, c * 128:(c + 1) * 128], identity=identb[:, :])),
                             [r_xp[b], r_id], [r_tp[tb]], append=(c > 0))
                    ins("vector" if i % 2 == 0 else "scalar", "tensor_copy" if i % 2 == 0 else "copy", [r_tp[tb]], [r_xpT[b]], append=(i > 0),
                        out=xpT[b][:, :, i * 128:(i + 1) * 128], in_=tp[tb][:, :].rearrange("p (c t) -> p c t", t=128))

            def GU(e):
                b = e % 2
                for j in range(4):
                    jb = cnt6["j"] % 2
                    cnt6["j"] += 1
                    for c in range(8):
                        mm(gp[jb][:, 0:CAP], wg[b][:, c, j * 128:(j + 1) * 128], xpT[b][:, c, :], c == 0, c == 7, [r_wg[b], r_xpT[b]], r_gp[jb], c == 0)
                    for c in range(8):
                        mm(up[jb][:, 0:CAP], wu[b][:, c, j * 128:(j + 1) * 128], xpT[b][:, c, :], c == 0, c == 7, [r_wu[b], r_xpT[b]], r_up[jb], c == 0)
                    ins("scalar", "activation", [r_gp[jb]], [r_sg[jb]], out=sg[jb][:, 0:CAP], in_=gp[jb][:, 0:CAP], func=AF.Silu)
                    ins("vector", "tensor_tensor", [r_sg[jb], r_up[jb]], [r_hT[b]], append=(j > 0), out=hT[b][:, j, :], in0=sg[jb][:, 0:CAP],
                        in1=up[jb][:, 0:CAP], op=ALU.mult)

            def DN(e):
                b = e % 2
                for i in range(NT):
                    ob = cnt6["y"] % 4
                    cnt6["y"] += 1
                    for hf in range(2):
                        for j in range(4):
                            mm(yp[hf][:, :], hT[b][:, j, i * 128:(i + 1) * 128], wd[b][:, j, hf * 512:(hf + 1) * 512], j == 0, j == 3,
                               [r_hT[b], r_wd[b]], r_yp[hf], j == 0)
                        if hf == 0:
                            ins("scalar", "copy", [r_yp[hf]], [r_yb[ob]], out=yb[ob][:, hf * 512:(hf + 1) * 512], in_=yp[hf][:, :])
                        else:
                            ins("vector", "tensor_copy", [r_yp[hf]], [r_yb[ob]], append=True, out=yb[ob][:, hf * 512:(hf + 1) * 512], in_=yp[hf][:, :])
                    dma("sync", YP_v[e, i], yb[ob][:, :], [r_yb[ob]], [])

            load_e(0)
            TRP(0)
            for e in range(32):
                if e + 1 < 32:
                    load_e(e + 1)
                GU(e)
                if e + 1 < 32:
                    TRP(e + 1)
                DN(e)
            K.emit()
        if stage <= 6:
            return nc

        with ExitStack() as es:
            sb = lambda n, s, d: es.enter_context(nc.sbuf_tensor(n, s, d))
            NB = 3
            g2 = sb("p7_g2", [128, D], F32); r_g2 = K.res()
            b2 = sb("p7_b2", [128, D], F32); r_b2 = K.res()
            xa = [sb("p7_xa%d" % i, [128, D], F32) for i in range(NB)]; r_xa = [K.res() for _ in range(NB)]
            y1 = [sb("p7_y1%d" % i, [128, D], BF16) for i in range(NB)]; r_y1 = [K.res() for _ in range(NB)]
            y2 = [sb("p7_y2%d" % i, [128, D], BF16) for i in range(NB)]; r_y2 = [K.res() for _ in range(NB)]
            h2 = [sb("p7_h%d" % i, [128, D], F32) for i in range(NB)]; r_h2 = [K.res() for _ in range(NB)]
            t7 = [sb("p7_t7_%d" % i, [128, D], F32) for i in range(2)]; r_t7 = [K.res() for _ in range(2)]
            o2 = [sb("p7_o%d" % i, [128, D], F32) for i in range(2)]; r_o2 = [K.res() for _ in range(2)]
            junk = sb("p7_junk", [128, D], BF16); r_junk = K.res()
            st6 = [sb("p7_st6_%d" % i, [128, 4], F32) for i in range(NB)]; r_st6 = [K.res() for _ in range(NB)]
            mv = [sb("p7_mv_%d" % i, [128, 2], F32) for i in range(NB)]; r_mv = [K.res() for _ in range(NB)]
            sm = [sb("p7_sm_%d" % i, [128, 4], F32) for i in range(NB)]; r_sm = [K.res() for _ in range(NB)]
            dma("sync", g2[:, :], I["ln2g"], [], [r_g2])
            dma("sync", b2[:, :], I["ln2b"], [], [r_b2])

            def load_t(t):
                u = t % NB
                dma("sync", xa[u][:, :], X1A[t * 128:(t + 1) * 128, :], [], [r_xa[u]])
                K.op("gpsimd", (lambda e, t=t, u=u: e.indirect_dma_start(
                    out=y1[u][:, :], out_offset=None, in_=YP,
                    in_offset=bass.IndirectOffsetOnAxis(ap=slotS[:, 2 * t:2 * t + 1], axis=0))), [], [r_y1[u]], dma=True)
                K.op("gpsimd", (lambda e, t=t, u=u: e.indirect_dma_start(
                    out=y2[u][:, :], out_offset=None, in_=YP,
                    in_offset=bass.IndirectOffsetOnAxis(ap=slotS[:, 2 * t + 1:2 * t + 2], axis=0))), [], [r_y2[u]], dma=True)

            def Y1a(t):
                u = t % NB
                w = t % 2
                ins("vector", "scalar_tensor_tensor", [r_y1[u], r_xa[u]], [r_t7[w]], out=t7[w][:, :], in0=y1[u][:, :],
                    scalar=gateS[:, 2 * t:2 * t + 1], in1=xa[u][:, :], op0=ALU.mult, op1=ALU.add)
                ins("vector", "scalar_tensor_tensor", [r_y2[u], r_t7[w]], [r_h2[u]], out=h2[u][:, :], in0=y2[u][:, :],
                    scalar=gateS[:, 2 * t + 1:2 * t + 2], in1=t7[w][:, :], op0=ALU.mult, op1=ALU.add)
                if t + NB < 32:
                    load_t(t + NB)

            def YS(t):
                u = t % NB
                ins("scalar", "activation", [r_h2[u]], [r_junk, r_st6[u]], out=junk[:, :], in_=h2[u][:, :], func=AF.Identity,
                    accum_out=st6[u][:, 0:1])
                ins("scalar", "activation", [r_h2[u]], [r_junk, r_st6[u]], out=junk[:, :], in_=h2[u][:, :], func=AF.Square,
                    accum_out=st6[u][:, 1:2])

            def Y1b(t):
                u = t % NB
                ins("vector", "tensor_scalar", [r_st6[u]], [r_mv[u]], out=mv[u][:, 0:1], in0=st6[u][:, 0:1], scalar1=1.0 / D, scalar2=None,
                    op0=ALU.mult)
                ins("vector", "tensor_tensor", [r_mv[u]], [r_st6[u]], out=st6[u][:, 2:3], in0=mv[u][:, 0:1], in1=mv[u][:, 0:1], op=ALU.mult)
                ins("vector", "scalar_tensor_tensor", [r_st6[u], r_mv[u]], [r_mv[u]], out=mv[u][:, 1:2], in0=st6[u][:, 1:2], scalar=1.0 / D,
                    in1=st6[u][:, 2:3], op0=ALU.mult, op1=ALU.subtract)
                ins("scalar", "activation", [r_mv[u]], [r_sm[u]], out=sm[u][:, 0:1], in_=mv[u][:, 1:2], func=AF.Ln, bias=EPS, scale=1.0)
                ins("scalar", "activation", [r_sm[u]], [r_sm[u]], out=sm[u][:, 1:2], in_=sm[u][:, 0:1], func=AF.Exp, scale=-0.5)
                ins("vector", "scalar_tensor_tensor", [r_mv[u], r_sm[u]], [r_sm[u]], out=sm[u][:, 2:3], in0=mv[u][:, 0:1], scalar=-1.0,
                    in1=sm[u][:, 1:2], op0=ALU.mult, op1=ALU.mult)

            def Y2a(t):
                u = t % NB
                w = t % 2
                ins("scalar", "activation", [r_h2[u], r_sm[u]], [r_o2[w]], out=o2[w][:, :], in_=h2[u][:, :], func=AF.Identity,
                    bias=sm[u][:, 2:3], scale=sm[u][:, 1:2])

            def Y2b(t):
                w = t % 2
                ins("vector", "tensor_tensor", [r_o2[w], r_g2], [r_o2[w]], out=o2[w][:, :], in0=o2[w][:, :], in1=g2[:, :], op=ALU.mult)
                ins("vector", "tensor_tensor", [r_o2[w], r_b2], [r_o2[w]], out=o2[w][:, :], in0=o2[w][:, :], in1=b2[:, :], op=ALU.add)
                dma("sync", out_d[t * 128:(t + 1) * 128, :], o2[w][:, :], [r_o2[w]], [])

            for t in range(NB):
                load_t(t)
            Y1a(0); YS(0); Y1b(0)
            Y1a(1); YS(1)
            for t in range(32):
                Y2a(t)
                if t + 2 < 32:
                    Y1a(t + 2)
                    YS(t + 2)
                Y2b(t)
                if t + 1 < 32:
                    Y1b(t + 1)
            K.emit()
    return nc


_NC_CACHE = {}


def kernel(**inputs):
    maps = _prep(inputs)
    if "nc" not in _NC_CACHE:
        _NC_CACHE["nc"] = build()
    nc = _NC_CACHE["nc"]
    res = run_bass_kernel_spmd(nc, maps, core_ids=list(range(len(maps))))
    out = np.stack([np.asarray(r["out"], dtype=np.float32) for r in res.results], axis=0)
    return out
```

```python
import math
import numpy as np
import concourse.bass as bass
import concourse.mybir as mybir
from concourse.bass_utils import run_bass_kernel_spmd
from contextlib import ExitStack

F32 = mybir.dt.float32
BF16 = mybir.dt.bfloat16
I32 = mybir.dt.int32
AF = mybir.ActivationFunctionType
ALU = mybir.AluOpType
AX = mybir.AxisListType

S = 4096
D = 1024
NST = 8
CAP = 384
NSLOT = 32 * CAP
NT = CAP // 128
ALPHA = (2.0 * 1) ** 0.25
EPS = 1e-5
NEGM = -30000.0

ENGS = ["sync", "scalar", "vector", "gpsimd", "tensor"]
NPOOL = 28
NHW = 16


class Res:
    __slots__ = ("w", "r")

    def __init__(self):
        self.w = []
        self.r = []


class Op:
    __slots__ = ("eng", "fn", "deps", "dma", "needed", "sem", "val")


def _add(lst, o):
    if not o.dma:
        for i, p in enumerate(lst):
            if (not p.dma) and p.eng == o.eng:
                lst[i] = o
                return
    lst.append(o)


class Sched:
    def __init__(self, nc, es):
        self.nc = nc
        self.esem = {e: es.enter_context(nc.semaphore("s_" + e)) for e in ENGS}
        self.dsem = [es.enter_context(nc.semaphore("d%d" % i)) for i in range(NPOOL)]
        self.ecount = {e: 0 for e in ENGS}
        self.dcount = [0] * NPOOL
        self.dlast = [None] * NPOOL
        self.dnext = {"hw": 0, "sw": 0}
        self.ops = []
        self.all_res = []
        self.cleared = False
        self.nphase = 0

    def res(self):
        r = Res()
        self.all_res.append(r)
        return r

    def op(self, eng, fn, reads=(), writes=(), dma=False, append=False):
        o = Op()
        o.eng = eng
        o.fn = fn
        o.dma = dma
        o.needed = False
        o.deps = []
        o.sem = None
        o.val = 0
        for r in reads:
            o.deps.extend(r.w)
        for w in writes:
            if not append:
                o.deps.extend(w.w)
            o.deps.extend(w.r)
        for r in reads:
            _add(r.r, o)
        for w in writes:
            if append:
                _add(w.w, o)
            else:
                w.w = [o]
            w.r = []
        self.ops.append(o)
        return o

    def _clear_block(self):
        sems = list(self.esem.values()) + self.dsem
        with self.nc.Block() as block:
            @block.sync
            def _(e):
                for s in sems:
                    e.sem_clear(s)
        self.cleared = True

    def emit(self):
        nc = self.nc
        if not self.cleared:
            self._clear_block()
        ops = self.ops
        for o in ops:
            if o.dma:
                if o.eng == "gpsimd":
                    i = NHW + self.dnext["sw"] % (NPOOL - NHW)
                    self.dnext["sw"] += 1
                else:
                    i = self.dnext["hw"] % NHW
                    self.dnext["hw"] += 1
                prev = self.dlast[i]
                if prev is not None:
                    o.deps.append(prev)
                self.dcount[i] += 16
                o.sem = self.dsem[i]
                o.val = self.dcount[i]
                self.dlast[i] = o
        for o in ops:
            for d in o.deps:
                if not d.dma:
                    d.needed = True
        for o in ops:
            if (not o.dma) and o.needed:
                self.ecount[o.eng] += 1
                o.sem = self.esem[o.eng]
                o.val = self.ecount[o.eng]
        per = {e: [o for o in ops if o.eng == e] for e in ENGS}
        waited = {}

        def body(ename):
            def f(eng):
                for o in per[ename]:
                    for d in o.deps:
                        if (not d.dma) and d.eng == ename and (not o.dma) and ename == "tensor":
                            continue
                        key = (ename, id(d.sem))
                        if waited.get(key, 0) >= d.val:
                            continue
                        eng.wait_ge(d.sem, d.val)
                        waited[key] = d.val
                    ins = o.fn(eng)
                    if o.dma:
                        ins.then_inc(o.sem, 16)
                    elif o.needed:
                        ins.then_inc(o.sem, 1)
                for o in per[ename]:
                    if o.dma:
                        key = (ename, id(o.sem))
                        if waited.get(key, 0) >= o.val:
                            continue
                        eng.wait_ge(o.sem, o.val)
                        waited[key] = o.val
            return f

        with nc.Block() as block:
            block.sync(body("sync"))
            block.scalar(body("scalar"))
            block.vector(body("vector"))
            block.gpsimd(body("gpsimd"))
            block.tensor(body("tensor"))
        self.nphase += 1
        self.ops = []
        self.dlast = [None] * NPOOL
        for r in self.all_res:
            r.w = []
            r.r = []
        self.all_res = []


def _bucket(rel):
    rel = np.maximum(rel, 0)
    max_exact = 16
    rel_f = np.maximum(rel, 1).astype(np.float32)
    large = max_exact + (np.log(rel_f / np.float32(max_exact)) / np.float32(math.log(128 / 16))
                         * np.float32(16)).astype(np.int32)
    large = np.minimum(large, 31)
    return np.where(rel < max_exact, rel, large)


def _consts():
    c = {}
    p = np.arange(128)
    c["identf"] = np.eye(128, dtype=np.float32)
    kind = np.zeros((64, S), np.float32)
    for j in range(16):
        kind[j, j * 256:(j + 1) * 256] = 1.0
    c["kind"] = kind
    gm = np.zeros((32, 16), np.float32)
    own = np.ones((32, 16), np.float32)
    for t in range(32):
        gm[t, (t // 2):] = -1e30
        own[t, t // 2] = 0.0
    c["gmadd"] = np.broadcast_to(gm.reshape(1, 512), (128, 512)).copy()
    c["own01"] = np.broadcast_to(own.reshape(1, 512), (128, 512)).copy()
    k = p[:, None]
    q = p[None, :]
    c["neg0"] = np.where(q < k, NEGM, 0.0).astype(np.float32)
    sb = np.zeros((128, 4, 512), np.float32)
    ql = np.arange(512)[None, :]
    for cc in range(4):
        sb[:, cc, :] = np.where((cc * 128 + k) >= ql, NEGM, 0.0)
    c["sbneg"] = sb.reshape(128, 2048)
    u2 = np.zeros((128, 32, 64), np.float32)
    for kt in range(32):
        for m in range(64):
            if (m % 32) < kt:
                u2[:, kt, m] = 1.0
    c["u2"] = u2.reshape(128, 2048)
    sel = np.zeros((128, 32, 128), np.float32)
    for kt in range(32):
        sel[kt, kt, :] = -1.0
        sel[32 + kt, kt, :] = -1.0
    c["sel"] = sel.reshape(128, 4096)
    c["tri"] = np.where(p[:, None] >= p[None, :], -1.0, 0.0).astype(np.float32)
    c["tst"] = np.where(p[:, None] < p[None, :], 1.0, 0.0).astype(np.float32)
    c["ebase"] = np.broadcast_to((np.arange(32, dtype=np.float32) * CAP)[None, :], (128, 32)).copy()
    return c


CONST_SHAPES = {
    "identf": [128, 128], "kind": [64, S], "gmadd": [128, 512], "own01": [128, 512], "neg0": [128, 128],
    "sbneg": [128, 2048], "u2": [128, 2048], "sel": [128, 4096], "tri": [128, 128], "tst": [128, 128],
    "ebase": [128, 32],
}

SHARED_SHAPES = {
    "w_in": [D, 5632], "w_kv": [D, 512], "wbr": [D, D], "w_out": [D, D],
    "ln1g": [128, D], "ln1b": [128, D], "ln2g": [128, D], "ln2b": [128, D],
    "w_r": [D, 36], "b_r": [128, 36], "w_gate": [32, D, 512], "w_up": [32, D, 512], "w_down": [32, 512, D],
    "dg": [128, 6 * 2 * 128], "b31": [128, 6],
}
CORE_SHAPES = {"xT": [D, S], "x": [S, D], "memT": [D, 256]}


def _prep(inputs):
    f = lambda a: np.ascontiguousarray(np.asarray(a, dtype=np.float32))
    sh = {}
    sh["w_in"] = f(inputs["w_in"][0])
    sh["w_kv"] = f(inputs["w_mem_kv"][0])
    wbr = np.concatenate([inputs["w_br_moba"][0], inputs["w_br_sb"][0], inputs["w_br_mem"][0]], axis=0)
    sh["wbr"] = f(wbr)
    sh["w_out"] = f(inputs["w_out"][0])
    for nm, key in (("ln1g", "ln1_g"), ("ln1b", "ln1_b"), ("ln2g", "ln2_g"), ("ln2b", "ln2_b")):
        sh[nm] = f(np.broadcast_to(np.asarray(inputs[key][0])[None, :], (128, D)))
    sh["w_r"] = f(np.concatenate([inputs["w_router_group"][0], inputs["w_router_expert"][0]], axis=1))
    br = np.concatenate([inputs["b_router_group"][0], inputs["b_router_expert"][0]], axis=0)
    sh["b_r"] = f(np.broadcast_to(np.asarray(br)[None, :], (128, 36)))
    sh["w_gate"] = f(inputs["w_gate"][0])
    sh["w_up"] = f(inputs["w_up"][0])
    sh["w_down"] = f(inputs["w_down"][0])
    rb = np.asarray(inputs["rel_bias"], dtype=np.float32)
    p = np.arange(128)
    rel0 = p[None, :] - p[:, None]
    rel1 = rel0 + 128
    idx = np.stack([_bucket(rel0), _bucket(rel1)], axis=1)
    dg = rb[idx]
    sh["dg"] = f(dg.transpose(0, 3, 1, 2).reshape(128, 6 * 2 * 128))
    sh["b31"] = f(np.broadcast_to(rb[31][None, :], (128, 6)))
    sh.update(_consts())
    x = np.asarray(inputs["x"], dtype=np.float32)
    mem = np.asarray(inputs["mem"], dtype=np.float32)
    maps = []
    for b in range(x.shape[0]):
        m = dict(sh)
        m["x"] = np.ascontiguousarray(x[b])
        m["xT"] = np.ascontiguousarray(x[b].T)
        m["memT"] = np.ascontiguousarray(mem[b].T)
        maps.append(m)
    return maps


def build(stage=99, debug=False):
    nc = bass.Bass("TRN2", target_bir_lowering=False)
    I = {}
    for nm, shp in list(CORE_SHAPES.items()) + list(SHARED_SHAPES.items()) + list(CONST_SHAPES.items()):
        I[nm] = nc.dram_tensor(nm, shp, F32, kind="ExternalInput").ap()
    out_d = nc.dram_tensor("out", [S, D], F32, kind="ExternalOutput").ap()
    skind = "ExternalOutput" if debug else "Internal"

    def scratch(nm, shp, dt):
        return nc.dram_tensor(nm, shp, dt, kind=skind).ap()

    QA = scratch("QA", [6, 64, S], BF16)
    KA = scratch("KA", [6, 64, S], BF16)
    VA = scratch("VA", [6, 128, 32 * 64], BF16)
    QB = scratch("QB", [3, 128, S], BF16)
    KB = scratch("KB", [3, 128, S], BF16)
    VB = scratch("VB", [3, 128, 32 * 128], BF16)
    QM = scratch("QM", [2, 128, S], BF16)
    G = scratch("G", [24, 128, S], BF16)
    YT = scratch("YT", [16, 64, S], BF16)
    X1A = scratch("X1A", [S, D], F32)
    XP = nc.dram_tensor("XP", [NSLOT, D], BF16, kind="Internal").ap()
    YP = nc.dram_tensor("YP", [NSLOT, D], BF16, kind="Internal").ap()
    if debug:
        SLOTD = scratch("SLOTD", [128, 64], I32)
        GATED = scratch("GATED", [128, 64], F32)

    with ExitStack() as ges:
        K = Sched(nc, ges)
        slotS = ges.enter_context(nc.sbuf_tensor("slotS", [128, 64], I32))
        gateS = ges.enter_context(nc.sbuf_tensor("gateS", [128, 64], F32))

        def ins(eng, name, reads, writes, append=False, **kw):
            return K.op(eng, lambda e: getattr(e, name)(**kw), reads, writes, append=append)

        def dma(eng, out, in_, reads, writes, append=False):
            return K.op(eng, lambda e: e.dma_start(out=out, in_=in_), reads, writes, dma=True, append=append)

        def mm(out, lhsT, rhs, start, stop, reads, w, first):
            return K.op("tensor", lambda e: e.matmul(out, lhsT=lhsT, rhs=rhs, start=start, stop=stop),
                        reads, [w], append=not first)


        def run_segments(segs, LA=2):
            hoisted = [0] * len(segs)
            for si, seg in enumerate(segs):
                steps = seg["steps"]
                n = len(steps)
                if seg.get("pre") is not None:
                    seg["pre"]()
                for k in range(hoisted[si], min(LA, n)):
                    steps[k][0]()
                for k in range(n):
                    steps[k][1]()
                    steps[k][2]()
                    if k + LA < n:
                        steps[k + LA][0]()
                    elif si + 1 < len(segs) and segs[si + 1].get("hoist", False):
                        j = k + LA - n
                        nxt = segs[si + 1]["steps"]
                        if j < min(LA, len(nxt)) and j == hoisted[si + 1]:
                            nxt[j][0]()
                            hoisted[si + 1] = j + 1
                if seg.get("post") is not None:
                    seg["post"]()

        with ExitStack() as es:
            sb = lambda n, s, d: es.enter_context(nc.sbuf_tensor(n, s, d))
            ps = lambda n: es.enter_context(nc.psum_tensor(n, [128, 512], F32))
            win = sb("p1_win", [128, 8, 5632], BF16)
            r_win = [K.res() for _ in range(11)]
            xt = [sb("p1_xt%d" % i, [128, 8, 512], BF16) for i in range(2)]
            r_xt = [K.res() for _ in range(2)]
            NO = 6
            ost = [sb("p1_o%d" % i, [128, 512], BF16) for i in range(NO)]
            r_ost = [K.res() for _ in range(NO)]
            vst = [sb("p1_v%d" % i, [128, 384], BF16) for i in range(2)]
            r_vst = [K.res() for _ in range(2)]
            psA = [ps("p1_pa%d" % i) for i in range(5)]
            r_psA = [K.res() for _ in range(5)]
            psV = [ps("p1_pv%d" % i) for i in range(2)]
            r_psV = [K.res() for _ in range(2)]
            w_in_v = I["w_in"].rearrange("(c p) n -> p c n", p=128)
            xT_v = I["xT"].rearrange("(c p) t -> p c t", p=128)
            dma("gpsimd", xt[0][:, :, :], xT_v[:, :, 0:512], [], [r_xt[0]])
            for cb in range(11):
                dma("gpsimd", win[:, :, cb * 512:(cb + 1) * 512], w_in_v[:, :, cb * 512:(cb + 1) * 512], [], [r_win[cb]])
            groups = []
            QAf = QA.rearrange("h p t -> (h p) t")
            KAf = KA.rearrange("h p t -> (h p) t")
            for i in range(3):
                groups.append((128 * i, 128, (lambda s, i=i: QAf[128 * i:128 * (i + 1), s * 512:(s + 1) * 512]), "q"))
            for i in range(3):
                groups.append((384 + 128 * i, 128, (lambda s, i=i: KAf[128 * i:128 * (i + 1), s * 512:(s + 1) * 512]), "k"))
            for i in range(3):
                groups.append((1152 + 128 * i, 128, (lambda s, i=i: QB[i, :, s * 512:(s + 1) * 512]), "q"))
            for i in range(3):
                groups.append((1536 + 128 * i, 128, (lambda s, i=i: KB[i, :, s * 512:(s + 1) * 512]), "k"))
            for i in range(2):
                groups.append((2304 + 128 * i, 128, (lambda s, i=i: QM[i, :, s * 512:(s + 1) * 512]), "q"))
            for i in range(24):
                groups.append((2560 + 128 * i, 128, (lambda s, i=i: G[i, :, s * 512:(s + 1) * 512]), "g"))
            VA_v = VA.rearrange("h p (t d) -> p h t d", d=64)
            VB_v = VB.rearrange("i p (t d) -> p i t d", d=128)
            gi = 0
            for s in range(NST):
                xb = xt[s % 2]
                rxb = r_xt[s % 2]
                if s + 1 < NST:
                    dma("gpsimd", xt[(s + 1) % 2][:, :, :], xT_v[:, :, (s + 1) * 512:(s + 2) * 512], [], [r_xt[(s + 1) % 2]])
                for (c0, ncol, dst, kind) in groups:
                    pi = gi % 5
                    oi = gi % NO
                    gi += 1
                    for c in range(8):
                        mm(psA[pi][0:ncol, :], win[:, c, c0:c0 + ncol], xb[:, c, :], c == 0, c == 7,
                           [r_win[c0 // 512], rxb], r_psA[pi], c == 0)
                    if kind == "g":
                        ins("scalar", "activation", [r_psA[pi]], [r_ost[oi]], out=ost[oi][0:ncol, :], in_=psA[pi][0:ncol, :], func=AF.Sigmoid)
                    elif kind == "q":
                        ins("vector", "tensor_scalar", [r_psA[pi]], [r_ost[oi]], out=ost[oi][0:ncol, :], in0=psA[pi][0:ncol, :],
                            scalar1=0.125, scalar2=None, op0=ALU.mult)
                    else:
                        ins("vector", "tensor_copy", [r_psA[pi]], [r_ost[oi]], out=ost[oi][0:ncol, :], in_=psA[pi][0:ncol, :])
                    dma("sync", dst(s), ost[oi][0:ncol, :], [r_ost[oi]], [])
                for tt in range(4):
                    t = 4 * s + tt
                    for vi, (c0, blks) in enumerate(((768, (1, 2)), (1920, (3, 4)))):
                        for c in range(8):
                            mm(psV[vi][:, 0:384], xb[:, c, tt * 128:(tt + 1) * 128], win[:, c, c0:c0 + 384], c == 0, c == 7,
                               [r_win[blks[0]], r_win[blks[1]], rxb], r_psV[vi], c == 0)
                        ins("vector", "tensor_copy", [r_psV[vi]], [r_vst[vi]], out=vst[vi][:, :], in_=psV[vi][:, 0:384])
                        if vi == 0:
                            dma("sync", VA_v[:, :, t, :], vst[vi][:, :].rearrange("p (h d) -> p h d", d=64), [r_vst[vi]], [])
                        else:
                            dma("sync", VB_v[:, :, t, :], vst[vi][:, :].rearrange("p (i d) -> p i d", d=128), [r_vst[vi]], [])
            K.emit()
        if stage <= 1:
            return nc

        with ExitStack() as es:
            sb = lambda n, s, d: es.enter_context(nc.sbuf_tensor(n, s, d))
            kaug = [sb("p2_k%d" % i, [128, S], BF16) for i in range(6)]
            qaug = [sb("p2_q%d" % i, [128, S], BF16) for i in range(6)]
            vaug = [sb("p2_v%d" % i, [128, 32, 64], BF16) for i in range(6)]
            identb = sb("p2_id", [128, 128], BF16)
            ones64 = sb("p2_ones", [128, 64], BF16)
            dfin = sb("p2_dfin", [128, 6, 2, 128], BF16)
            with ExitStack() as es2:
                sb2 = lambda n, s, d: es2.enter_context(nc.sbuf_tensor(n, s, d))
                r_k = [K.res() for _ in range(6)]
                r_kind = [K.res() for _ in range(6)]
                r_q = [K.res() for _ in range(6)]
                r_qm = [[K.res() for _ in range(NST)] for _ in range(6)]
                r_v = [K.res() for _ in range(6)]
                r_id = K.res(); r_ones = K.res(); r_dfin = K.res()
                dgf = sb2("p2_dgf", [128, 1536], F32); r_dgf = K.res()
                b31 = sb2("p2_b31", [128, 6], F32); r_b31 = K.res()
                neg0 = sb2("p2_neg0", [128, 128], F32); r_neg0 = K.res()
                gmadd = sb2("p2_gmadd", [128, 512], F32); r_gmadd = K.res()
                own01 = sb2("p2_own01", [128, 512], F32); r_own = K.res()
                ksum = sb2("p2_ksum", [128, 16], F32); r_ksum = K.res()
                kmb = [sb2("p2_kmb%d" % i, [128, 16], BF16) for i in range(2)]; r_kmb = [K.res() for _ in range(2)]
                gm = [sb2("p2_gm%d" % i, [128, 512], F32) for i in range(2)]; r_gm = [K.res() for _ in range(2)]
                m8 = sb2("p2_m8", [128, 32, 8], F32); r_m8 = K.res()
                mbf = sb2("p2_mbf", [128, 512], F32); r_mbf = K.res()
                mbpad = [sb2("p2_mbpad%d" % i, [128, 32, 80], BF16) for i in range(2)]; r_mbpad = [K.res() for _ in range(2)]
                gps = [es2.enter_context(nc.psum_tensor("p2_g%d" % i, [128, 512], F32)) for i in range(2)]; r_gps = [K.res() for _ in range(2)]
                tps = [es2.enter_context(nc.psum_tensor("p2_t%d" % i, [128, 512], F32)) for i in range(2)]; r_tps = [K.res() for _ in range(2)]
                dma("gpsimd", identb[:, :], I["identf"], [], [r_id])
                dma("sync", dgf[:, :], I["dg"], [], [r_dgf])
                dma("sync", b31[:, :], I["b31"], [], [r_b31])
                dma("sync", neg0[:, :], I["neg0"], [], [r_neg0])
                dma("sync", gmadd[:, :], I["gmadd"], [], [r_gmadd])
                dma("sync", own01[:, :], I["own01"], [], [r_own])
                for h in range(6):
                    dma("sync", kaug[h][0:64, :], KA[h], [], [r_k[h]])
                    dma("scalar", qaug[h][0:64, :], QA[h], [], [r_q[h]])
                    dma("sync", vaug[h][:, :, :], VA[h].rearrange("p (t d) -> p t d", d=64), [], [r_v[h]])
                    dma("gpsimd", kaug[h][64:128, :].rearrange("p (a n) -> p a n", n=2048), I["kind"].rearrange("p (a n) -> p a n", n=2048),
                        [], [r_kind[h]])
                    ins("gpsimd", "memset", [], r_qm[h], ap=qaug[h][64:128, :], constant=0.0)
                ins("gpsimd", "memset", [], [r_ones], ap=ones64[:, :], constant=1.0)
                for i in range(2):
                    ins("gpsimd", "memset", [], [r_mbpad[i]], ap=mbpad[i][:, :, :], constant=0.0)
                    ins("gpsimd", "memset", [], [r_kmb[i]], ap=kmb[i][:, :], constant=0.0)
                for h in range(6):
                    for t in range(2):
                        src = dgf[:, (h * 2 + t) * 128:(h * 2 + t + 1) * 128]
                        if t == 0:
                            ins("vector", "scalar_tensor_tensor", [r_dgf, r_b31, r_neg0], [r_dfin], append=True, out=dfin[:, h, t, :],
                                in0=src, scalar=b31[:, h:h + 1], in1=neg0[:, :], op0=ALU.subtract, op1=ALU.add)
                        else:
                            ins("vector", "tensor_scalar", [r_dgf, r_b31], [r_dfin], append=True, out=dfin[:, h, t, :], in0=src,
                                scalar1=b31[:, h:h + 1], scalar2=None, op0=ALU.subtract)

                def prologue1(h):
                    i = h % 2
                    ins("vector", "tensor_reduce", [r_k[h]], [r_ksum], out=ksum[0:64, :],
                        in_=kaug[h][0:64, :].rearrange("p (j k) -> p j k", k=256), axis=AX.X, op=ALU.add)
                    ins("vector", "tensor_copy", [r_ksum], [r_kmb[i]], out=kmb[i][0:64, :], in_=ksum[0:64, :])
                    for t in range(32):
                        mm(gps[i][:, t * 16:(t + 1) * 16], qaug[h][:, t * 128:(t + 1) * 128], kmb[i][:, :], True, True,
                           [r_q[h], r_kmb[i]] + r_qm[h], r_gps[i], t == 0)
                    ins("vector", "tensor_tensor", [r_gps[i], r_gmadd], [r_gm[i]], out=gm[i][:, :], in0=gps[i][:, :], in1=gmadd[:, :], op=ALU.add)
                    for t in range(32):
                        ins("vector", "max", [r_gm[i]], [r_m8], append=(t > 0), out=m8[:, t, :], in_=gm[i][:, t * 16:(t + 1) * 16])
                    ins("vector", "tensor_tensor", [r_gm[i], r_m8], [r_mbf], out=mbf[:, :].rearrange("p (t j) -> p t j", j=16),
                        in0=gm[i][:, :].rearrange("p (t j) -> p t j", j=16), in1=m8[:, :, 2:3].to_broadcast([128, 32, 16]), op=ALU.is_lt)
                    ins("vector", "scalar_tensor_tensor", [r_mbf, r_own], [r_mbpad[i]], out=mbpad[i][:, :, 64:80],
                        in0=mbf[:, :].rearrange("p (t j) -> p t j", j=16), scalar=NEGM,
                        in1=own01[:, :].rearrange("p (t j) -> p t j", j=16), op0=ALU.mult, op1=ALU.mult)

                def prologue2(h):
                    i = h % 2
                    for s in range(NST):
                        ti = s % 2
                        for c in range(4):
                            t = 4 * s + c
                            mm(tps[ti][0:80, c * 128:(c + 1) * 128], mbpad[i][:, t, :], identb[:, :], True, True, [r_mbpad[i], r_id], r_tps[ti], c == 0)
                        ins("scalar", "copy", [r_tps[ti]], [r_qm[h][s]], out=qaug[h][64:80, s * 512:(s + 1) * 512], in_=tps[ti][64:80, :])

                prologue1(0)
                for h in range(6):
                    if h + 1 < 6:
                        prologue1(h + 1)
                    prologue2(h)
                K.emit()
            with ExitStack() as es2:
                sb2 = lambda n, s, d: es2.enter_context(nc.sbuf_tensor(n, s, d))
                r_all = K.res()
                pt = [sb2("p2_pt%d" % i, [128, 1024], BF16) for i in range(2)]; r_pt = [K.res() for _ in range(2)]
                rden = [sb2("p2_rden%d" % i, [128, 512], F32) for i in range(2)]; r_rden = [K.res() for _ in range(2)]
                yo = [sb2("p2_yo%d" % i, [128, 512], BF16) for i in range(2)]; r_yo = [K.res() for _ in range(2)]
                sps = [es2.enter_context(nc.psum_tensor("p2_s%d" % i, [128, 1024], F32)) for i in range(2)]; r_sps = [K.res() for _ in range(2)]
                nps = [es2.enter_context(nc.psum_tensor("p2_n%d" % i, [128, 512], F32)) for i in range(2)]; r_nps = [K.res() for _ in range(2)]
                dps = [es2.enter_context(nc.psum_tensor("p2_d%d" % i, [128, 512], F32)) for i in range(2)]; r_dps = [K.res() for _ in range(2)]
                cnt2 = {"it": 0}

                def make_seg(h, s):
                    nkt = 4 * s + 4
                    a = (h * NST + s) % 2
                    steps = []
                    for j in range(nkt // 2):
                        st = {}
                        los = [max(0, 2 * j + h2 - 4 * s) * 128 for h2 in range(2)]

                        def A(j=j, st=st, los=los):
                            si = cnt2["it"] % 2
                            cnt2["it"] += 1
                            st["si"] = si
                            for h2 in range(2):
                                kt = 2 * j + h2
                                lo = los[h2]
                                base = h2 * 512
                                cd = kt - 4 * s
                                cp = kt - 4 * s + 1
                                has_d0 = 0 <= cd <= 3
                                has_d1 = 0 <= cp <= 3
                                mm(sps[si][:, base + lo:base + 512], kaug[h][:, kt * 128:(kt + 1) * 128], qaug[h][:, s * 512 + lo:(s + 1) * 512],
                                   True, not (has_d0 or has_d1), [r_all], r_sps[si], h2 == 0)
                                if has_d0:
                                    mm(sps[si][:, base + cd * 128:base + (cd + 1) * 128], identb[:, :], dfin[:, h, 0, :], False, not has_d1, [r_all], r_sps[si], False)
                                if has_d1:
                                    mm(sps[si][:, base + cp * 128:base + (cp + 1) * 128], identb[:, :], dfin[:, h, 1, :], False, True, [r_all], r_sps[si], False)

                        def B(j=j, st=st, los=los):
                            si = st["si"]
                            if los[0] == 0 and los[1] == 0:
                                ins("scalar", "activation", [r_sps[si]], [r_pt[si]], out=pt[si][:, :], in_=sps[si][:, :], func=AF.Exp)
                            else:
                                for h2 in range(2):
                                    lo = h2 * 512 + los[h2]
                                    hi = (h2 + 1) * 512
                                    ins("scalar", "activation", [r_sps[si]], [r_pt[si]], append=(h2 > 0), out=pt[si][:, lo:hi], in_=sps[si][:, lo:hi],
                                        func=AF.Exp)

                        def C(j=j, st=st, los=los):
                            si = st["si"]
                            for h2 in range(2):
                                kt = 2 * j + h2
                                lo = los[h2]
                                base = h2 * 512
                                mm(nps[a][0:64, lo:512], vaug[h][:, kt, :], pt[si][:, base + lo:base + 512], kt == 0, kt == nkt - 1, [r_all, r_pt[si]],
                                   r_nps[a], kt == 0)
                                mm(dps[a][0:64, lo:512], ones64[:, :], pt[si][:, base + lo:base + 512], kt == 0, kt == nkt - 1, [r_all, r_pt[si]],
                                   r_dps[a], kt == 0)

                        steps.append((A, B, C))

                    def post():
                        ins("vector", "reciprocal", [r_dps[a]], [r_rden[a]], out=rden[a][0:64, :], in_=dps[a][0:64, :])
                        ins("vector", "tensor_tensor", [r_nps[a], r_rden[a]], [r_yo[a]], out=yo[a][0:64, :], in0=nps[a][0:64, :],
                            in1=rden[a][0:64, :], op=ALU.mult)
                        dma("sync", YT[h, :, s * 512:(s + 1) * 512], yo[a][0:64, :], [r_yo[a]], [])
                    return {"steps": steps, "pre": None, "post": post, "hoist": True}

                segs = []
                for h in range(6):
                    for s in range(NST):
                        segs.append(make_seg(h, s))
                run_segments(segs)
                K.emit()
        if stage <= 2:
            return nc

        with ExitStack() as es:
            sb = lambda n, s, d: es.enter_context(nc.sbuf_tensor(n, s, d))
            ps = lambda n: es.enter_context(nc.psum_tensor(n, [128, 512], F32))
            kb2 = [sb("p3_k%d" % i, [128, S], BF16) for i in range(2)]
            qz = [[sb("p3_q%d_%d" % (i, j), [128, S], BF16) for j in range(2)] for i in range(2)]
            vb2 = [sb("p3_v%d" % i, [128, 32, 128], BF16) for i in range(2)]
            r_k = [K.res() for _ in range(2)]
            r_q = [K.res() for _ in range(2)]
            r_v = [K.res() for _ in range(2)]
            identb = sb("p3_id", [128, 128], BF16); r_id = K.res()
            sbneg = sb("p3_neg", [128, 4, 512], BF16); r_neg = K.res()
            u2 = sb("p3_u2", [128, 32, 64], BF16); r_u2 = K.res()
            sel = sb("p3_sel", [128, 32, 128], BF16); r_sel = K.res()
            tri = sb("p3_tri", [128, 128], BF16); r_tri = K.res()
            lbuf2 = [[sb("p3_l%d_%d" % (j, i), [128, 1024], BF16) for i in range(16)] for j in range(2)]
            r_l2 = [[K.res() for _ in range(16)] for _ in range(2)]
            ebuf = [sb("p3_e%d" % i, [128, 1024], F32) for i in range(2)]
            r_e = [K.res() for _ in range(2)]
            at = [sb("p3_a%d" % i, [128, 1024], BF16) for i in range(3)]
            r_at = [K.res() for _ in range(3)]
            rhl = [sb("p3_r%d" % i, [128, 512], BF16) for i in range(2)]
            r_rhl = [K.res() for _ in range(2)]
            yo = [sb("p3_yo%d" % i, [128, 512], BF16) for i in range(2)]
            r_yo = [K.res() for _ in range(2)]
            zb = [es.enter_context(nc.psum_tensor("p3_z%d" % i, [128, 1024], F32)) for i in range(3)]; r_zb = [K.res() for _ in range(3)]
            rps = [ps("p3_rm0")] * 2; r_rps = [K.res()] * 2
            ops_ = [ps("p3_o0")] * 2; r_ops = [K.res()] * 2
            dma("gpsimd", identb[:, :], I["identf"], [], [r_id])
            dma("gpsimd", sbneg[:, :, :], I["sbneg"].rearrange("p (c q) -> p c q", q=512), [], [r_neg])
            dma("gpsimd", u2[:, :, :], I["u2"].rearrange("p (k m) -> p k m", m=64), [], [r_u2])
            dma("gpsimd", sel[:, :, :], I["sel"].rearrange("p (k m) -> p k m", m=128), [], [r_sel])
            dma("gpsimd", tri[:, :], I["tri"], [], [r_tri])
            for b_ in range(2):
                ins("gpsimd", "memset", [], [r_rhl[b_]], ap=rhl[b_][:, :], constant=0.0)
                for j_ in range(2):
                    ins("gpsimd", "memset", [], [r_q[b_]], append=(j_ > 0), ap=qz[b_][j_][:, :], constant=0.0)

            def load_pair(i):
                b = i % 2
                dma("sync", kb2[b][:, :], KB[i], [], [r_k[b]])
                for j in range(2):
                    dma("sync", qz[b][j][64 * j:64 * j + 64, :], QB[i, 64 * j:64 * j + 64, :], [], [r_q[b]], append=(j > 0))
                dma("sync", vb2[b][:, :, :], VB[i].rearrange("p (t d) -> p t d", d=128), [], [r_v[b]])

            cnt3 = {"z": 0, "e": 0}

            def make_unit(i, hp, s):
                b = i % 2
                p0 = 64 * hp
                hh = 2 * i + hp
                nkt = 4 * s + 4
                a = (hh * NST + s) % 2
                lbuf = lbuf2[a]
                r_l = r_l2[a]
                qs = qz[b][hp][:, s * 512:(s + 1) * 512]
                steps1 = []
                steps2 = []
                for j in range(nkt // 2):
                    st1 = {}
                    st2 = {}

                    los = [max(0, 2 * j + h2 - 4 * s) * 128 for h2 in range(2)]

                    def A1(j=j, st=st1, los=los):
                        zi = cnt3["z"] % 3
                        cnt3["z"] += 1
                        st["zi"] = zi
                        for h2 in range(2):
                            kt = 2 * j + h2
                            diag = kt >= 4 * s
                            lo = los[h2]
                            base = h2 * 512
                            mm(zb[zi][:, base + lo:base + 512], kb2[b][:, kt * 128:(kt + 1) * 128], qs[:, lo:512], True, not diag,
                               [r_k[b], r_q[b]], r_zb[zi], h2 == 0)
                            if diag:
                                mm(zb[zi][:, base + lo:base + lo + 128], identb[:, :], sbneg[:, kt - 4 * s, lo:lo + 128], False, True,
                                   [r_id, r_neg], r_zb[zi], False)

                    def B1(j=j, st=st1, los=los):
                        zi = st["zi"]
                        ei = cnt3["e"] % 2
                        cnt3["e"] += 1
                        if los[0] == 0 and los[1] == 0:
                            ins("scalar", "activation", [r_zb[zi]], [r_e[ei]], out=ebuf[ei][:, :], in_=zb[zi][:, :], func=AF.Exp)
                            ins("scalar", "activation", [r_e[ei]], [r_l[j]], out=lbuf[j][:, :], in_=ebuf[ei][:, :], func=AF.Ln, bias=1.0, scale=1.0)
                        else:
                            for h2 in range(2):
                                c0_, c1_ = h2 * 512 + los[h2], (h2 + 1) * 512
                                ins("scalar", "activation", [r_zb[zi]], [r_e[ei]], append=(h2 > 0), out=ebuf[ei][:, c0_:c1_], in_=zb[zi][:, c0_:c1_],
                                    func=AF.Exp)
                                ins("scalar", "activation", [r_e[ei]], [r_l[j]], append=(h2 > 0), out=lbuf[j][:, c0_:c1_], in_=ebuf[ei][:, c0_:c1_],
                                    func=AF.Ln, bias=1.0, scale=1.0)

                    def C1(j=j, st=st1, los=los):
                        for h2 in range(2):
                            kt = 2 * j + h2
                            lo = los[h2]
                            mm(rps[a][0:64, lo:512], u2[:, kt, :], lbuf[j][:, h2 * 512 + lo:(h2 + 1) * 512], kt == 0, kt == nkt - 1, [r_u2, r_l[j]],
                               r_rps[a], kt == 0)

                    def A2(j=j, st=st2, los=los):
                        zi = cnt3["z"] % 3
                        cnt3["z"] += 1
                        st["zi"] = zi
                        for h2 in range(2):
                            kt = 2 * j + h2
                            diag = kt >= 4 * s
                            lo = los[h2]
                            base = h2 * 512
                            zo = zb[zi][:, base + lo:base + 512]
                            mm(zo, kb2[b][:, kt * 128:(kt + 1) * 128], qs[:, lo:512], True, False, [r_k[b], r_q[b]], r_zb[zi], h2 == 0)
                            if diag:
                                mm(zb[zi][:, base + lo:base + lo + 128], identb[:, :], sbneg[:, kt - 4 * s, lo:lo + 128], False, False,
                                   [r_id, r_neg], r_zb[zi], False)
                            mm(zo, tri[:, :], lbuf[j][:, base + lo:base + 512], False, False, [r_tri, r_l[j]], r_zb[zi], False)
                            mm(zo, sel[:, kt, :], rhl[a][:, lo:512], False, True, [r_sel, r_rhl[a]], r_zb[zi], False)

                    def B2(j=j, st=st2, los=los):
                        zi = st["zi"]
                        if los[0] == 0 and los[1] == 0:
                            ins("scalar", "activation", [r_zb[zi]], [r_at[zi]], out=at[zi][:, :], in_=zb[zi][:, :], func=AF.Exp)
                        else:
                            for h2 in range(2):
                                c0_, c1_ = h2 * 512 + los[h2], (h2 + 1) * 512
                                ins("scalar", "activation", [r_zb[zi]], [r_at[zi]], append=(h2 > 0), out=at[zi][:, c0_:c1_], in_=zb[zi][:, c0_:c1_],
                                    func=AF.Exp)

                    def C2(j=j, st=st2, los=los):
                        zi = st["zi"]
                        for h2 in range(2):
                            kt = 2 * j + h2
                            lo = los[h2]
                            mm(ops_[a][0:64, lo:512], vb2[b][:, kt, p0:p0 + 64], at[zi][:, h2 * 512 + lo:(h2 + 1) * 512], kt == 0, kt == nkt - 1,
                               [r_v[b], r_at[zi]], r_ops[a], kt == 0)

                    steps1.append((A1, B1, C1))
                    steps2.append((A2, B2, C2))

                def pre2():
                    ins("vector", "tensor_copy", [r_rps[a]], [r_rhl[a]], out=rhl[a][0:64, :], in_=rps[a][0:64, :])
                    ins("vector", "tensor_tensor", [r_rps[a], r_rhl[a]], [r_rhl[a]], out=rhl[a][32:64, :], in0=rps[a][32:64, :],
                        in1=rhl[a][32:64, :], op=ALU.subtract)

                def post2():
                    ins("vector", "tensor_copy", [r_ops[a]], [r_yo[a]], out=yo[a][0:64, :], in_=ops_[a][0:64, :])
                    dma("sync", YT[6 + hh, :, s * 512:(s + 1) * 512], yo[a][0:64, :], [r_yo[a]], [])
                    if hp == 1 and s == NST - 1 and i + 2 < 3:
                        load_pair(i + 2)

                return (steps1, steps2, pre2, post2)

            def interleave(x, y):
                out = []
                i = j = 0
                while i < len(x) or j < len(y):
                    if j >= len(y) or (i < len(x) and i * len(y) <= j * len(x)):
                        out.append(x[i]); i += 1
                    else:
                        out.append(y[j]); j += 1
                return out

            load_pair(0)
            load_pair(1)
            units = []
            for i in range(3):
                for hp in range(2):
                    for s in range(NST):
                        units.append(make_unit(i, hp, s))
            segs = [{"steps": units[0][0], "pre": None, "post": None, "hoist": False}]
            for u in range(len(units)):
                nxt1 = units[u + 1][0] if u + 1 < len(units) else []
                segs.append({"steps": nxt1[0:2] + interleave(units[u][1], nxt1[2:]), "pre": units[u][2], "post": units[u][3],
                             "hoist": len(nxt1) >= 2})
            run_segments(segs)
            K.emit()
        if stage <= 3:
            return nc

        with ExitStack() as es:
            sb = lambda n, s, d: es.enter_context(nc.sbuf_tensor(n, s, d))
            ps = lambda n: es.enter_context(nc.psum_tensor(n, [128, 512], F32))
            memT = sb("p4_mem", [128, 8, 256], BF16); r_mem = K.res()
            wkv = sb("p4_wkv", [128, 8, 512], BF16); r_wkv = K.res()
            km = sb("p4_km", [128, 2, 256], BF16); r_km = K.res()
            vm = sb("p4_vm", [128, 2, 256], BF16); r_vm = K.res()
            qmz = [sb("p4_qm%d" % i, [128, S], BF16) for i in range(4)]; r_qm = K.res()
            ones64 = sb("p4_ones", [128, 64], BF16); r_ones = K.res()
            pt4 = [sb("p4_pt%d" % i, [128, 1024], BF16) for i in range(2)]
            r_pt = [K.res() for _ in range(2)]
            rden = [sb("p4_rden%d" % i, [128, 512], F32) for i in range(2)]
            r_rden = [K.res() for _ in range(2)]
            yo = [sb("p4_yo%d" % i, [128, 512], BF16) for i in range(2)]
            r_yo = [K.res() for _ in range(2)]
            sps4 = [es.enter_context(nc.psum_tensor("p4_s%d" % i, [128, 1024], F32)) for i in range(2)]; r_sps = [K.res() for _ in range(2)]
            nps = [ps("p4_n%d" % i) for i in range(2)]; r_nps = [K.res() for _ in range(2)]
            dps = [ps("p4_d%d" % i) for i in range(2)]; r_dps = [K.res() for _ in range(2)]
            dma("gpsimd", memT[:, :, :], I["memT"].rearrange("(c p) m -> p c m", p=128), [], [r_mem])
            dma("gpsimd", wkv[:, :, :], I["w_kv"].rearrange("(c p) n -> p c n", p=128), [], [r_wkv])
            r_qmz = [K.res() for _ in range(4)]
            for hm_ in range(4):
                ins("gpsimd" if hm_ % 2 == 0 else "vector", "memset", [], [r_qmz[hm_]], ap=qmz[hm_][:, :], constant=0.0)
            for hm_ in range(4):
                p_ = 64 * (hm_ % 2)
                dma("sync" if hm_ % 2 == 0 else "scalar", qmz[hm_][p_:p_ + 64, :], QM[hm_ // 2, p_:p_ + 64, :], [], [r_qmz[hm_]])
            ins("gpsimd", "memset", [], [r_ones], ap=ones64[:, :], constant=1.0)
            for i in range(2):
                for c in range(8):
                    mm(sps4[i][:, 0:256], wkv[:, c, i * 128:(i + 1) * 128], memT[:, c, :], c == 0, c == 7, [r_wkv, r_mem], r_sps[i], c == 0)
                ins("vector", "tensor_copy", [r_sps[i]], [r_km], append=(i > 0), out=km[:, i, :], in_=sps4[i][:, 0:256])
            for j in range(2):
                for c in range(8):
                    mm(nps[j][:, 0:256], memT[:, c, j * 128:(j + 1) * 128], wkv[:, c, 256:512], c == 0, c == 7, [r_wkv, r_mem], r_nps[j], c == 0)
                ins("vector", "tensor_copy", [r_nps[j]], [r_vm], append=(j > 0), out=vm[:, j, :], in_=nps[j][:, 0:256])
            def make_seg4(hm, s):
                i = hm // 2
                a = (hm * NST + s) % 2
                st = {}

                def A():
                    si = (hm * NST + s) % 2
                    st["si"] = si
                    for j in range(2):
                        mm(sps4[si][:, j * 512:(j + 1) * 512], km[:, i, j * 128:(j + 1) * 128], qmz[hm][:, s * 512:(s + 1) * 512], True, True,
                           [r_km, r_qmz[hm]], r_sps[si], j == 0)

                def B():
                    si = st["si"]
                    ins("scalar", "activation", [r_sps[si]], [r_pt[si]], out=pt4[si][:, :], in_=sps4[si][:, :], func=AF.Exp)

                def C():
                    si = st["si"]
                    for j in range(2):
                        mm(nps[a][0:64, :], vm[:, j, hm * 64:(hm + 1) * 64], pt4[si][:, j * 512:(j + 1) * 512], j == 0, j == 1, [r_vm, r_pt[si]], r_nps[a], j == 0)
                        mm(dps[a][0:64, :], ones64[:, :], pt4[si][:, j * 512:(j + 1) * 512], j == 0, j == 1, [r_ones, r_pt[si]], r_dps[a], j == 0)

                def post():
                    ins("scalar", "activation", [r_dps[a]], [r_rden[a]], out=rden[a][0:64, :], in_=dps[a][0:64, :], func=AF.Ln)
                    ins("scalar", "activation", [r_rden[a]], [r_rden[a]], out=rden[a][0:64, :], in_=rden[a][0:64, :], func=AF.Exp, scale=-1.0)
                    ins("vector", "tensor_tensor", [r_nps[a], r_rden[a]], [r_yo[a]], out=yo[a][0:64, :], in0=nps[a][0:64, :],
                        in1=rden[a][0:64, :], op=ALU.mult)
                    dma("sync", YT[12 + hm, :, s * 512:(s + 1) * 512], yo[a][0:64, :], [r_yo[a]], [])
                return {"steps": [(A, B, C)], "pre": None, "post": post, "hoist": True}

            segs = []
            for hm in range(4):
                for s in range(NST):
                    segs.append(make_seg4(hm, s))
            A0, B0, C0 = segs[0]["steps"][0]
            A0(); B0()
            for u in range(len(segs)):
                if u + 1 < len(segs):
                    An, Bn, Cn = segs[u + 1]["steps"][0]
                    An(); Bn()
                segs[u]["steps"][0][2]()
                segs[u]["post"]()
            K.emit()
        if stage <= 4:
            return nc

        with ExitStack() as es:
            sb = lambda n, s, d: es.enter_context(nc.sbuf_tensor(n, s, d))
            ps = lambda n: es.enter_context(nc.psum_tensor(n, [128, 512], F32))
            wbr = sb("p5_wbr", [128, 8, D], BF16); r_wbr = K.res()
            wout = sb("p5_wout", [128, 8, D], BF16); r_wout = K.res()
            g1 = sb("p5_g1", [128, D], F32); r_g1 = K.res()
            b1 = sb("p5_b1", [128, D], F32); r_b1 = K.res()
            wr = sb("p5_wr", [128, 8, 36], F32); r_wr = K.res()
            brr = sb("p5_br", [128, 36], F32); r_br = K.res()
            identf = sb("p5_idf", [128, 128], F32); r_idf = K.res()
            tst = sb("p5_tst", [128, 128], BF16); r_tst = K.res()
            onesb = sb("p5_ones", [128, 128], BF16); r_onesb = K.res()
            cbe = sb("p5_cbe", [128, 32], F32); r_cbe = K.res()
            ytl = [sb("p5_y%d" % i, [128, 8, 512], BF16) for i in range(2)]; r_y = [K.res() for _ in range(2)]
            gl = sb("p5_gl", [128, 24, 512], BF16); r_gl = [K.res() for _ in range(8)]
            acc = [sb("p5_acc%d" % i, [128, 512], F32) for i in range(2)]; r_acc = [K.res() for _ in range(2)]
            a1 = [sb("p5_a1%d" % i, [128, 512], F32) for i in range(2)]; r_a1 = [K.res() for _ in range(2)]
            c2 = [sb("p5_c2%d" % i, [128, 512], F32) for i in range(2)]; r_c2 = [K.res() for _ in range(2)]
            mgb = [sb("p5_mg%d" % i, [128, 8, 512], BF16) for i in range(2)]; r_mg = [K.res() for _ in range(2)]
            xtok = [sb("p5_xt%d" % i, [128, D], F32) for i in range(4)]; r_x = [K.res() for _ in range(4)]
            x1 = sb("p5_xone", [128, 4, D], F32); r_x1 = [K.res() for _ in range(4)]
            x1a = [sb("p5_x1a%d" % i, [128, D], F32) for i in range(2)]; r_x1a = [K.res() for _ in range(2)]
            x1b = [sb("p5_x1b%d" % i, [128, 4, D], BF16) for i in range(2)]; r_x1b = [[K.res() for _ in range(4)] for _ in range(2)]
            x1T = sb("p5_x1T", [128, 8, 512], F32); r_x1T = [K.res() for _ in range(4)]
            st6 = sb("p5_st6", [128, 4, 12], F32); r_st6 = K.res()
            mv = sb("p5_mv", [128, 4, 2], F32); r_mv = K.res()
            sm = sb("p5_sm", [128, 3, 4], F32); r_sm = K.res()
            lg4 = sb("p5_lg4", [128, 4, 36], F32); r_lg = K.res()
            gmax = sb("p5_gmax", [128, 4], F32); r_gmax = K.res()
            goh = sb("p5_goh", [128, 16], F32); r_goh = K.res()
            dd = sb("p5_dd", [128, 16], F32); r_dd = K.res()
            gex = sb("p5_gex", [128, 16], F32); r_gex = K.res()
            gp = sb("p5_gp", [128, 4], F32); r_gp = K.res()
            lem = sb("p5_lem", [128, 128], F32); r_lem = K.res()
            e8 = sb("p5_e8", [128, 4, 8], F32); r_e8 = K.res()
            oh1 = sb("p5_oh1", [128, 128], F32); r_oh1 = K.res()
            oh2 = sb("p5_oh2", [128, 128], F32); r_oh2 = K.res()
            ohb = sb("p5_ohb", [128, 128], BF16); r_ohb = K.res()
            wt = sb("p5_wt", [128, 4], F32); r_wt = K.res()
            cnt = sb("p5_cnt", [128, 128], F32); r_cnt = K.res()
            t128 = sb("p5_t128", [128, 128], F32); r_t128 = K.res()
            slf = sb("p5_slf", [128, 8], F32); r_slf = K.res()
            r_slot = K.res()
            r_gate = K.res()
            bps = [ps("p5_pb%d" % i) for i in range(3)]; r_bps = [K.res() for _ in range(3)]
            mps = [ps("p5_pm%d" % i) for i in range(2)]; r_mps = [K.res() for _ in range(2)]
            tps2 = [ps("p5_pt%d" % i) for i in range(2)]; r_tps2 = [K.res() for _ in range(2)]
            sps5 = ps("p5_ps"); r_lgps = K.res(); r_cps = K.res()
            dma("gpsimd", wbr[:, :, :], I["wbr"].rearrange("(c p) n -> p c n", p=128), [], [r_wbr])
            dma("gpsimd", wout[:, :, :], I["w_out"].rearrange("(c p) n -> p c n", p=128), [], [r_wout])
            dma("sync", g1[:, :], I["ln1g"], [], [r_g1])
            dma("sync", b1[:, :], I["ln1b"], [], [r_b1])
            dma("sync", wr[:, :, :], I["w_r"].rearrange("(c p) n -> p c n", p=128), [], [r_wr])
            dma("sync", brr[:, :], I["b_r"], [], [r_br])
            dma("sync", identf[:, :], I["identf"], [], [r_idf])
            dma("gpsimd", tst[:, :], I["tst"], [], [r_tst])
            dma("sync", cbe[:, :], I["ebase"], [], [r_cbe])
            ins("gpsimd", "memset", [], [r_onesb], ap=onesb[:, :], constant=1.0)
            YT_v = YT.rearrange("(c two) p t -> c (two p) t", two=2).rearrange("c q t -> q c t")
            G_v = G.rearrange("(b m) p t -> p m b t", b=3)
            gl_v = gl[:, :, :].rearrange("p (m b) t -> p m b t", b=3)
            heads = ((0, 3), (3, 6), (6, 8))
            cnt5 = {"b": 0}

            def load_y(s):
                dma("sync", ytl[s % 2][:, :, :], YT_v[:, :, s * 512:(s + 1) * 512], [], [r_y[s % 2]])

            def load_g(s, m):
                dma("sync", gl_v[:, m, :, :], G_v[:, m, :, s * 512:(s + 1) * 512], [], [r_gl[m]])

            def M(s, pieces=None):
                b = s % 2
                for m in range(8):
                    if pieces is not None and m < len(pieces):
                        pieces[m]()
                    a = m % 2
                    pbs = []
                    for br in range(3):
                        pb = cnt5["b"] % 3
                        cnt5["b"] += 1
                        pbs.append(pb)
                        h0, h1 = heads[br]
                        for hh in range(h0, h1):
                            mm(bps[pb][:, :], wbr[:, hh, m * 128:(m + 1) * 128], ytl[b][:, hh, :], hh == h0, hh == h1 - 1,
                               [r_wbr, r_y[b]], r_bps[pb], hh == h0)
                    ins("vector", "tensor_tensor", [r_bps[pbs[0]], r_gl[m]], [r_acc[a]], out=acc[a][:, :], in0=bps[pbs[0]][:, :],
                        in1=gl[:, 3 * m + 0, :], op=ALU.mult)
                    ins("vector", "tensor_tensor", [r_bps[pbs[1]], r_gl[m]], [r_a1[a]], out=a1[a][:, :], in0=bps[pbs[1]][:, :],
                        in1=gl[:, 3 * m + 1, :], op=ALU.mult)
                    ins("vector", "tensor_tensor", [r_bps[pbs[2]], r_gl[m]], [r_c2[a]], out=c2[a][:, :], in0=bps[pbs[2]][:, :],
                        in1=gl[:, 3 * m + 2, :], op=ALU.mult)
                    ins("gpsimd", "tensor_tensor", [r_acc[a], r_a1[a]], [r_acc[a]], out=acc[a][:, :], in0=acc[a][:, :], in1=a1[a][:, :], op=ALU.add)
                    ins("gpsimd", "tensor_tensor", [r_acc[a], r_c2[a]], [r_mg[b]], append=(m > 0),
                        out=mgb[b][:, m, :], in0=acc[a][:, :], in1=c2[a][:, :], op=ALU.add)
                    if s + 1 < NST:
                        load_g(s + 1, m)
                if s + 2 < NST:
                    load_y(s + 2)

            def load_x(s):
                for tt in range(4):
                    t = 4 * s + tt
                    dma("scalar", xtok[tt][:, :], I["x"][t * 128:(t + 1) * 128, :], [], [r_x[tt]])

            def XA(s):
                b = s % 2
                for tt in range(4):
                    for half in range(2):
                        for c in range(8):
                            mm(mps[half][:, :], mgb[b][:, c, tt * 128:(tt + 1) * 128], wout[:, c, half * 512:(half + 1) * 512], c == 0, c == 7,
                               [r_mg[b], r_wout], r_mps[half], c == 0)
                        ins("vector", "scalar_tensor_tensor", [r_x[tt], r_mps[half]], [r_x[tt]],
                            out=xtok[tt][:, half * 512:(half + 1) * 512], in0=xtok[tt][:, half * 512:(half + 1) * 512], scalar=ALPHA,
                            in1=mps[half][:, :], op0=ALU.mult, op1=ALU.add)
                    for half in range(2):
                        ins("vector", "bn_stats", [r_x[tt]], [r_st6], append=(tt + half > 0), out=st6[:, tt, half * 6:(half + 1) * 6],
                            in_=xtok[tt][:, half * 512:(half + 1) * 512])
                for tt in range(4):
                    ins("vector", "bn_aggr", [r_st6], [r_mv], append=(tt > 0), out=mv[:, tt, :], in_=st6[:, tt, :])
                ins("scalar", "activation", [r_mv], [r_sm], out=sm[:, 0, :], in_=mv[:, :, 1], func=AF.Ln, bias=EPS, scale=1.0)
                ins("scalar", "activation", [r_sm], [r_sm], out=sm[:, 1, :], in_=sm[:, 0, :], func=AF.Exp, scale=-0.5)
                ins("vector", "scalar_tensor_tensor", [r_mv, r_sm], [r_sm], out=sm[:, 2, :], in0=mv[:, :, 0], scalar=-1.0,
                    in1=sm[:, 1, :], op0=ALU.mult, op1=ALU.mult)

            def XB(s):
                b = s % 2
                for tt in range(4):
                    ins("scalar", "activation", [r_x[tt], r_sm], [r_x1[tt]], out=x1[:, tt, :], in_=xtok[tt][:, :], func=AF.Identity,
                        bias=sm[:, 2, tt:tt + 1], scale=sm[:, 1, tt:tt + 1])
                for tt in range(4):
                    ins("vector", "tensor_tensor", [r_x1[tt], r_g1], [r_x1[tt]], out=x1[:, tt, :], in0=x1[:, tt, :], in1=g1[:, :], op=ALU.mult)
                    ins("vector", "tensor_tensor", [r_x1[tt], r_b1], [r_x1[tt]], out=x1[:, tt, :], in0=x1[:, tt, :], in1=b1[:, :], op=ALU.add)
                for tt in range(4):
                    t = 4 * s + tt
                    u = t % 2
                    ins("scalar", "mul", [r_x1[tt]], [r_x1a[u]], out=x1a[u][:, :], in_=x1[:, tt, :], mul=ALPHA)
                    ins("scalar", "copy", [r_x1[tt]], [r_x1b[b][tt]], out=x1b[b][:, tt, :], in_=x1[:, tt, :])
                    dma("sync", X1A[t * 128:(t + 1) * 128, :], x1a[u][:, :], [r_x1a[u]], [])

            def T(s):
                def TR(tt):
                    for c in range(8):
                        hf = c // 4
                        K.op("tensor", (lambda e, c=c, hf=hf, tt=tt: e.transpose(out=tps2[hf][:, (c % 4) * 128:(c % 4 + 1) * 128],
                                                                                 in_=x1[:, tt, c * 128:(c + 1) * 128], identity=identf[:, :])),
                             [r_x1[tt], r_idf], [r_tps2[hf]], append=(c % 4 > 0))

                def CP(tt):
                    for hf in range(2):
                        ins("scalar", "copy", [r_tps2[hf]], [r_x1T[tt]], append=(hf > 0), out=x1T[:, hf * 4:(hf + 1) * 4, tt * 128:(tt + 1) * 128],
                            in_=tps2[hf][:, :].rearrange("p (c t) -> p c t", t=128))

                def MMr(tt):
                    for c in range(8):
                        mm(sps5[:, tt * 36:(tt + 1) * 36], x1T[:, c, tt * 128:(tt + 1) * 128], wr[:, c, :], c == 0, c == 7, [r_x1T[tt], r_wr],
                           r_lgps, tt == 0 and c == 0)

                return [lambda: TR(0), lambda: (CP(0), TR(1)), lambda: MMr(0), lambda: (CP(1), TR(2)), lambda: MMr(1),
                        lambda: (CP(2), TR(3)), lambda: MMr(2), lambda: (CP(3), MMr(3))]

            v4 = lambda ap, n: ap.rearrange("p (t n) -> p t n", n=n)

            def Ra(s):
                ins("vector", "tensor_tensor", [r_lgps, r_br], [r_lg], out=lg4[:, :, :], in0=v4(sps5[:, 0:144], 36),
                    in1=brr[:, :].unsqueeze(1).to_broadcast([128, 4, 36]), op=ALU.add)
                ins("vector", "tensor_reduce", [r_lg], [r_gmax], out=gmax[:, :], in_=lg4[:, :, 0:4], axis=AX.X, op=ALU.max)
                ins("vector", "tensor_tensor", [r_lg, r_gmax], [r_goh], out=v4(goh[:, :], 4), in0=lg4[:, :, 0:4],
                    in1=gmax[:, :].unsqueeze(2).to_broadcast([128, 4, 4]), op=ALU.is_equal)
                ins("vector", "tensor_tensor", [r_lg, r_gmax], [r_dd], out=v4(dd[:, :], 4), in0=lg4[:, :, 0:4],
                    in1=gmax[:, :].unsqueeze(2).to_broadcast([128, 4, 4]), op=ALU.subtract)
                ins("scalar", "activation", [r_dd], [r_gex], out=gex[:, :], in_=dd[:, :], func=AF.Exp)
                ins("vector", "tensor_reduce", [r_gex], [r_gp], out=gp[:, :], in_=v4(gex[:, :], 4), axis=AX.X, op=ALU.add)
                ins("vector", "reciprocal", [r_gp], [r_gp], out=gp[:, :], in_=gp[:, :])
                ins("vector", "tensor_scalar", [r_goh], [r_goh], out=goh[:, :], in0=goh[:, :], scalar1=-1.0, scalar2=1e30,
                    op0=ALU.add, op1=ALU.mult)
                ins("vector", "tensor_tensor", [r_lg, r_goh], [r_lem], out=lem[:, :].rearrange("p (t g e) -> p t g e", g=4, e=8),
                    in0=lg4[:, :, 4:36].rearrange("p t (g e) -> p t g e", e=8),
                    in1=v4(goh[:, :], 4).unsqueeze(3).to_broadcast([128, 4, 4, 8]), op=ALU.add)

            def Rb(s):
                for tt in range(4):
                    ins("vector", "max", [r_lem], [r_e8], append=(tt > 0), out=e8[:, tt, :], in_=lem[:, tt * 32:(tt + 1) * 32])
                ins("vector", "tensor_tensor", [r_lem, r_e8], [r_oh1], out=v4(oh1[:, :], 32), in0=v4(lem[:, :], 32),
                    in1=e8[:, :, 0:1].to_broadcast([128, 4, 32]), op=ALU.is_equal)
                ins("vector", "tensor_tensor", [r_lem, r_e8], [r_oh2], out=v4(oh2[:, :], 32), in0=v4(lem[:, :], 32),
                    in1=e8[:, :, 1:2].to_broadcast([128, 4, 32]), op=ALU.is_equal)
                ins("vector", "tensor_tensor", [r_e8], [r_wt], out=wt[:, :], in0=e8[:, :, 1], in1=e8[:, :, 0], op=ALU.subtract)
                ins("scalar", "activation", [r_wt], [r_wt], out=wt[:, :], in_=wt[:, :], func=AF.Exp)
                ins("vector", "tensor_scalar", [r_wt], [r_wt], out=wt[:, :], in0=wt[:, :], scalar1=1.0, scalar2=None, op0=ALU.add)
                ins("vector", "reciprocal", [r_wt], [r_wt], out=wt[:, :], in_=wt[:, :])
                gs = gateS[:, 8 * s:8 * s + 8].rearrange("p (t k) -> p t k", k=2)
                ins("vector", "tensor_tensor", [r_wt, r_gp], [r_gate], append=True, out=gs[:, :, 0], in0=wt[:, :], in1=gp[:, :], op=ALU.mult)
                ins("vector", "tensor_tensor", [r_gp, r_gate], [r_gate], append=True, out=gs[:, :, 1], in0=gp[:, :], in1=gs[:, :, 0], op=ALU.subtract)
                ins("vector", "tensor_tensor", [r_oh1, r_oh2], [r_ohb], out=ohb[:, :], in0=oh1[:, :], in1=oh2[:, :], op=ALU.add)

            def Rc(s):
                b = s % 2
                for tt in range(4):
                    mm(sps5[:, 256 + tt * 32:256 + (tt + 1) * 32], tst[:, :], ohb[:, tt * 32:(tt + 1) * 32], True, tt == 0,
                       [r_tst, r_ohb], r_cps, tt == 0)
                    for t2 in range(tt):
                        mm(sps5[:, 256 + tt * 32:256 + (tt + 1) * 32], onesb[:, :], ohb[:, t2 * 32:(t2 + 1) * 32], False, t2 == tt - 1,
                           [r_onesb, r_ohb], r_cps, False)
                for tt in range(4):
                    mm(sps5[:, 384:416], onesb[:, :], ohb[:, tt * 32:(tt + 1) * 32], tt == 0, tt == 3, [r_onesb, r_ohb], r_cps, False)
                ins("vector", "tensor_tensor", [r_cps, r_cbe], [r_cnt], out=v4(cnt[:, :], 32), in0=v4(sps5[:, 256:384], 32),
                    in1=cbe[:, :].unsqueeze(1).to_broadcast([128, 4, 32]), op=ALU.add)
                ins("vector", "tensor_tensor", [r_cps, r_cbe], [r_cbe], out=cbe[:, :], in0=sps5[:, 384:416], in1=cbe[:, :], op=ALU.add)
                sl = slf[:, :].rearrange("p (t k) -> p t k", k=2)
                ins("vector", "tensor_tensor", [r_cnt, r_oh1], [r_t128], out=t128[:, :], in0=cnt[:, :], in1=oh1[:, :], op=ALU.mult)
                ins("vector", "tensor_reduce", [r_t128], [r_slf], out=sl[:, :, 0], in_=v4(t128[:, :], 32), axis=AX.X, op=ALU.add)
                ins("vector", "tensor_tensor", [r_cnt, r_oh2], [r_t128], out=t128[:, :], in0=cnt[:, :], in1=oh2[:, :], op=ALU.mult)
                ins("vector", "tensor_reduce", [r_t128, r_slf], [r_slf], out=sl[:, :, 1], in_=v4(t128[:, :], 32), axis=AX.X, op=ALU.add)
                ins("vector", "tensor_copy", [r_slf], [r_slot], append=True, out=slotS[:, 8 * s:8 * s + 8], in_=slf[:, :])
                for tt in range(4):
                    for k in range(2):
                        col = 8 * s + 2 * tt + k
                        K.op("gpsimd", (lambda e, col=col, tt=tt, b=b: e.indirect_dma_start(
                            out=XP, out_offset=bass.IndirectOffsetOnAxis(ap=slotS[:, col:col + 1], axis=0),
                            in_=x1b[b][:, tt, :], in_offset=None)), [r_x1b[b][tt], r_slot], [], dma=True)

            load_y(0)
            load_y(1)
            for m in range(8):
                load_g(0, m)

            load_x(0)
            M(0)
            XA(0)
            XB(0)
            for s in range(NST):
                tp_ = T(s)
                if s + 1 < NST:
                    load_x(s + 1)
                    M(s + 1, tp_)
                else:
                    for f in tp_:
                        f()
                Ra(s)
                if s + 1 < NST:
                    XA(s + 1)
                Rb(s)
                if s + 1 < NST:
                    XB(s + 1)
                Rc(s)
            if debug:
                dma("sync", SLOTD, slotS[:, :], [r_slot], [])
                dma("sync", GATED, gateS[:, :], [r_gate], [])
            K.emit()
        if stage <= 5:
            return nc

        with ExitStack() as es:
            sb = lambda n, s, d: es.enter_context(nc.sbuf_tensor(n, s, d))
            ps = lambda n, dt=F32, w=512: es.enter_context(nc.psum_tensor(n, [128, w], dt))
            wg = [sb("p6_wg%d" % i, [128, 8, 512], BF16) for i in range(2)]; r_wg = [K.res() for _ in range(2)]
            wu = [sb("p6_wu%d" % i, [128, 8, 512], BF16) for i in range(2)]; r_wu = [K.res() for _ in range(2)]
            wd = [sb("p6_wd%d" % i, [128, 4, D], BF16) for i in range(2)]; r_wd = [K.res() for _ in range(2)]
            xp = [sb("p6_xp%d" % i, [128, NT, D], BF16) for i in range(2)]; r_xp = [K.res() for _ in range(2)]
            xpT = [sb("p6_xpT%d" % i, [128, 8, CAP], BF16) for i in range(2)]; r_xpT = [K.res() for _ in range(2)]
            identb = sb("p6_id", [128, 128], BF16); r_id = K.res()
            sg = [sb("p6_sg%d" % i, [128, 512], F32) for i in range(2)]; r_sg = [K.res() for _ in range(2)]
            hT = [sb("p6_hT%d" % i, [128, 4, CAP], BF16) for i in range(2)]; r_hT = [K.res() for _ in range(2)]
            yb = [sb("p6_y%d" % i, [128, D], BF16) for i in range(4)]; r_yb = [K.res() for _ in range(4)]
            tp = [ps("p6_pt%d" % i, BF16, 1024) for i in range(2)]; r_tp = [K.res() for _ in range(2)]
            gp = [ps("p6_pg%d" % i) for i in range(2)]; r_gp = [K.res() for _ in range(2)]
            up = [ps("p6_pu%d" % i) for i in range(2)]; r_up = [K.res() for _ in range(2)]
            yp = [ps("p6_py%d" % i) for i in range(2)]; r_yp = [K.res() for _ in range(2)]
            dma("gpsimd", identb[:, :], I["identf"], [], [r_id])
            XP_v = XP.rearrange("(e i p) d -> e p i d", p=128, i=NT)
            YP_v = YP.rearrange("(e i p) d -> e i p d", p=128, i=NT)

            def load_e(e):
                b = e % 2
                dma("gpsimd", wg[b][:, :, :], I["w_gate"][e].rearrange("(c p) n -> p c n", p=128), [], [r_wg[b]])
                dma("gpsimd", wu[b][:, :, :], I["w_up"][e].rearrange("(c p) n -> p c n", p=128), [], [r_wu[b]])
                for hf in range(2):
                    dma("gpsimd", wd[b][:, :, hf * 512:(hf + 1) * 512], I["w_down"][e].rearrange("(c p) n -> p c n", p=128)[:, :, hf * 512:(hf + 1) * 512],
                        [], [r_wd[b]], append=(hf > 0))
                dma("sync", xp[b][:, :, :], XP_v[e], [], [r_xp[b]])

            cnt6 = {"t": 0, "j": 0, "y": 0}

            def TRP(e):
                b = e % 2
                for i in range(NT):
                    tb = cnt6["t"] % 2
                    cnt6["t"] += 1
                    for c in range(8):
                        K.op("tensor", (lambda en, i=i, c=c, b=b, tb=tb: en.transpose(out=tp[tb][:, c * 128:(c + 1) * 128],
                                                                                      in_=xp[b][:, i, c * 128:(c + 1) * 128], identity=identb[:, :])),
                             [r_xp[b], r_id], [r_tp[tb]], append=(c > 0))
                    ins("vector" if i % 2 == 0 else "scalar", "tensor_copy" if i % 2 == 0 else "copy", [r_tp[tb]], [r_xpT[b]], append=(i > 0),
                        out=xpT[b][:, :, i * 128:(i + 1) * 128], in_=tp[tb][:, :].rearrange("p (c t) -> p c t", t=128))

            def GU(e):
                b = e % 2
                for j in range(4):
                    jb = cnt6["j"] % 2
                    cnt6["j"] += 1
                    for c in range(8):
                        mm(gp[jb][:, 0:CAP], wg[b][:, c, j * 128:(j + 1) * 128], xpT[b][:, c, :], c == 0, c == 7, [r_wg[b], r_xpT[b]], r_gp[jb], c == 0)
                    for c in range(8):
                        mm(up[jb][:, 0:CAP], wu[b][:, c, j * 128:(j + 1) * 128], xpT[b][:, c, :], c == 0, c == 7, [r_wu[b], r_xpT[b]], r_up[jb], c == 0)
                    ins("scalar", "activation", [r_gp[jb]], [r_sg[jb]], out=sg[jb][:, 0:CAP], in_=gp[jb][:, 0:CAP], func=AF.Silu)
                    ins("vector", "tensor_tensor", [r_sg[jb], r_up[jb]], [r_hT[b]], append=(j > 0), out=hT[b][:, j, :], in0=sg[jb][:, 0:CAP],
                        in1=up[jb][:, 0:CAP], op=ALU.mult)

            def DN(e):
                b = e % 2
                for i in range(NT):
                    ob = cnt6["y"] % 4
                    cnt6["y"] += 1
                    for hf in range(2):
                        for j in range(4):
                            mm(yp[hf][:, :], hT[b][:, j, i * 128:(i + 1) * 128], wd[b][:, j, hf * 512:(hf + 1) * 512], j == 0, j == 3,
                               [r_hT[b], r_wd[b]], r_yp[hf], j == 0)
                        if hf == 0:
                            ins("scalar", "copy", [r_yp[hf]], [r_yb[ob]], out=yb[ob][:, hf * 512:(hf + 1) * 512], in_=yp[hf][:, :])
                        else:
                            ins("vector", "tensor_copy", [r_yp[hf]], [r_yb[ob]], append=True, out=yb[ob][:, hf * 512:(hf + 1) * 512], in_=yp[hf][:, :])
                    dma("sync", YP_v[e, i], yb[ob][:, :], [r_yb[ob]], [])

            load_e(0)
            TRP(0)
            for e in range(32):
                if e + 1 < 32:
                    load_e(e + 1)
                GU(e)
                if e + 1 < 32:
                    TRP(e + 1)
                DN(e)
            K.emit()
        if stage <= 6:
            return nc

        with ExitStack() as es:
            sb = lambda n, s, d: es.enter_context(nc.sbuf_tensor(n, s, d))
            NB = 3
            g2 = sb("p7_g2", [128, D], F32); r_g2 = K.res()
            b2 = sb("p7_b2", [128, D], F32); r_b2 = K.res()
            xa = [sb("p7_xa%d" % i, [128, D], F32) for i in range(NB)]; r_xa = [K.res() for _ in range(NB)]
            y1 = [sb("p7_y1%d" % i, [128, D], BF16) for i in range(NB)]; r_y1 = [K.res() for _ in range(NB)]
            y2 = [sb("p7_y2%d" % i, [128, D], BF16) for i in range(NB)]; r_y2 = [K.res() for _ in range(NB)]
            h2 = [sb("p7_h%d" % i, [128, D], F32) for i in range(NB)]; r_h2 = [K.res() for _ in range(NB)]
            t7 = [sb("p7_t7_%d" % i, [128, D], F32) for i in range(2)]; r_t7 = [K.res() for _ in range(2)]
            o2 = [sb("p7_o%d" % i, [128, D], F32) for i in range(2)]; r_o2 = [K.res() for _ in range(2)]
            junk = sb("p7_junk", [128, D], BF16); r_junk = K.res()
            st6 = [sb("p7_st6_%d" % i, [128, 4], F32) for i in range(NB)]; r_st6 = [K.res() for _ in range(NB)]
            mv = [sb("p7_mv_%d" % i, [128, 2], F32) for i in range(NB)]; r_mv = [K.res() for _ in range(NB)]
            sm = [sb("p7_sm_%d" % i, [128, 4], F32) for i in range(NB)]; r_sm = [K.res() for _ in range(NB)]
            dma("sync", g2[:, :], I["ln2g"], [], [r_g2])
            dma("sync", b2[:, :], I["ln2b"], [], [r_b2])

            def load_t(t):
                u = t % NB
                dma("sync", xa[u][:, :], X1A[t * 128:(t + 1) * 128, :], [], [r_xa[u]])
                K.op("gpsimd", (lambda e, t=t, u=u: e.indirect_dma_start(
                    out=y1[u][:, :], out_offset=None, in_=YP,
                    in_offset=bass.IndirectOffsetOnAxis(ap=slotS[:, 2 * t:2 * t + 1], axis=0))), [], [r_y1[u]], dma=True)
                K.op("gpsimd", (lambda e, t=t, u=u: e.indirect_dma_start(
                    out=y2[u][:, :], out_offset=None, in_=YP,
                    in_offset=bass.IndirectOffsetOnAxis(ap=slotS[:, 2 * t + 1:2 * t + 2], axis=0))), [], [r_y2[u]], dma=True)

            def Y1a(t):
                u = t % NB
                w = t % 2
                ins("vector", "scalar_tensor_tensor", [r_y1[u], r_xa[u]], [r_t7[w]], out=t7[w][:, :], in0=y1[u][:, :],
                    scalar=gateS[:, 2 * t:2 * t + 1], in1=xa[u][:, :], op0=ALU.mult, op1=ALU.add)
                ins("vector", "scalar_tensor_tensor", [r_y2[u], r_t7[w]], [r_h2[u]], out=h2[u][:, :], in0=y2[u][:, :],
                    scalar=gateS[:, 2 * t + 1:2 * t + 2], in1=t7[w][:, :], op0=ALU.mult, op1=ALU.add)
                if t + NB < 32:
                    load_t(t + NB)

            def YS(t):
                u = t % NB
                ins("scalar", "activation", [r_h2[u]], [r_junk, r_st6[u]], out=junk[:, :], in_=h2[u][:, :], func=AF.Identity,
                    accum_out=st6[u][:, 0:1])
                ins("scalar", "activation", [r_h2[u]], [r_junk, r_st6[u]], out=junk[:, :], in_=h2[u][:, :], func=AF.Square,
                    accum_out=st6[u][:, 1:2])

            def Y1b(t):
                u = t % NB
                ins("vector", "tensor_scalar", [r_st6[u]], [r_mv[u]], out=mv[u][:, 0:1], in0=st6[u][:, 0:1], scalar1=1.0 / D, scalar2=None,
                    op0=ALU.mult)
                ins("vector", "tensor_tensor", [r_mv[u]], [r_st6[u]], out=st6[u][:, 2:3], in0=mv[u][:, 0:1], in1=mv[u][:, 0:1], op=ALU.mult)
                ins("vector", "scalar_tensor_tensor", [r_st6[u], r_mv[u]], [r_mv[u]], out=mv[u][:, 1:2], in0=st6[u][:, 1:2], scalar=1.0 / D,
                    in1=st6[u][:, 2:3], op0=ALU.mult, op1=ALU.subtract)
                ins("scalar", "activation", [r_mv[u]], [r_sm[u]], out=sm[u][:, 0:1], in_=mv[u][:, 1:2], func=AF.Ln, bias=EPS, scale=1.0)
                ins("scalar", "activation", [r_sm[u]], [r_sm[u]], out=sm[u][:, 1:2], in_=sm[u][:, 0:1], func=AF.Exp, scale=-0.5)
                ins("vector", "scalar_tensor_tensor", [r_mv[u], r_sm[u]], [r_sm[u]], out=sm[u][:, 2:3], in0=mv[u][:, 0:1], scalar=-1.0,
                    in1=sm[u][:, 1:2], op0=ALU.mult, op1=ALU.mult)

            def Y2a(t):
                u = t % NB
                w = t % 2
                ins("scalar", "activation", [r_h2[u], r_sm[u]], [r_o2[w]], out=o2[w][:, :], in_=h2[u][:, :], func=AF.Identity,
                    bias=sm[u][:, 2:3], scale=sm[u][:, 1:2])

            def Y2b(t):
                w = t % 2
                ins("vector", "tensor_tensor", [r_o2[w], r_g2], [r_o2[w]], out=o2[w][:, :], in0=o2[w][:, :], in1=g2[:, :], op=ALU.mult)
                ins("vector", "tensor_tensor", [r_o2[w], r_b2], [r_o2[w]], out=o2[w][:, :], in0=o2[w][:, :], in1=b2[:, :], op=ALU.add)
                dma("sync", out_d[t * 128:(t + 1) * 128, :], o2[w][:, :], [r_o2[w]], [])

            for t in range(NB):
                load_t(t)
            Y1a(0); YS(0); Y1b(0)
            Y1a(1); YS(1)
            for t in range(32):
                Y2a(t)
                if t + 2 < 32:
                    Y1a(t + 2)
                    YS(t + 2)
                Y2b(t)
                if t + 1 < 32:
                    Y1b(t + 1)
            K.emit()
    return nc


_NC_CACHE = {}


def kernel(**inputs):
    maps = _prep(inputs)
    if "nc" not in _NC_CACHE:
        _NC_CACHE["nc"] = build()
    nc = _NC_CACHE["nc"]
    res = run_bass_kernel_spmd(nc, maps, core_ids=list(range(len(maps))))
    out = np.stack([np.asarray(r["out"], dtype=np.float32) for r in res.results], axis=0)
    return out
```

```python
import math
import numpy as np
import concourse.bass as bass
import concourse.mybir as mybir
from concourse.bass_utils import run_bass_kernel_spmd
from contextlib import ExitStack

F32 = mybir.dt.float32
BF16 = mybir.dt.bfloat16
I32 = mybir.dt.int32
AF = mybir.ActivationFunctionType
ALU = mybir.AluOpType
AX = mybir.AxisListType

S = 4096
D = 1024
NST = 8
CAP = 384
NSLOT = 32 * CAP
NT = CAP // 128
ALPHA = (2.0 * 1) ** 0.25
EPS = 1e-5
NEGM = -30000.0

ENGS = ["sync", "scalar", "vector", "gpsimd", "tensor"]
NPOOL = 28
NHW = 16


class Res:
    __slots__ = ("w", "r")

    def __init__(self):
        self.w = []
        self.r = []


class Op:
    __slots__ = ("eng", "fn", "deps", "dma", "needed", "sem", "val")


def _add(lst, o):
    if not o.dma:
        for i, p in enumerate(lst):
            if (not p.dma) and p.eng == o.eng:
                lst[i] = o
                return
    lst.append(o)


class Sched:
    def __init__(self, nc, es):
        self.nc = nc
        self.esem = {e: es.enter_context(nc.semaphore("s_" + e)) for e in ENGS}
        self.dsem = [es.enter_context(nc.semaphore("d%d" % i)) for i in range(NPOOL)]
        self.ecount = {e: 0 for e in ENGS}
        self.dcount = [0] * NPOOL
        self.dlast = [None] * NPOOL
        self.dnext = {"hw": 0, "sw": 0}
        self.ops = []
        self.all_res = []
        self.cleared = False
        self.nphase = 0

    def res(self):
        r = Res()
        self.all_res.append(r)
        return r

    def op(self, eng, fn, reads=(), writes=(), dma=False, append=False):
        o = Op()
        o.eng = eng
        o.fn = fn
        o.dma = dma
        o.needed = False
        o.deps = []
        o.sem = None
        o.val = 0
        for r in reads:
            o.deps.extend(r.w)
        for w in writes:
            if not append:
                o.deps.extend(w.w)
            o.deps.extend(w.r)
        for r in reads:
            _add(r.r, o)
        for w in writes:
            if append:
                _add(w.w, o)
            else:
                w.w = [o]
            w.r = []
        self.ops.append(o)
        return o

    def _clear_block(self):
        sems = list(self.esem.values()) + self.dsem
        with self.nc.Block() as block:
            @block.sync
            def _(e):
                for s in sems:
                    e.sem_clear(s)
        self.cleared = True

    def emit(self):
        nc = self.nc
        if not self.cleared:
            self._clear_block()
        ops = self.ops
        for o in ops:
            if o.dma:
                if o.eng == "gpsimd":
                    i = NHW + self.dnext["sw"] % (NPOOL - NHW)
                    self.dnext["sw"] += 1
                else:
                    i = self.dnext["hw"] % NHW
                    self.dnext["hw"] += 1
                prev = self.dlast[i]
                if prev is not None:
                    o.deps.append(prev)
                self.dcount[i] += 16
                o.sem = self.dsem[i]
                o.val = self.dcount[i]
                self.dlast[i] = o
        for o in ops:
            for d in o.deps:
                if not d.dma:
                    d.needed = True
        for o in ops:
            if (not o.dma) and o.needed:
                self.ecount[o.eng] += 1
                o.sem = self.esem[o.eng]
                o.val = self.ecount[o.eng]
        per = {e: [o for o in ops if o.eng == e] for e in ENGS}
        waited = {}

        def body(ename):
            def f(eng):
                for o in per[ename]:
                    for d in o.deps:
                        if (not d.dma) and d.eng == ename and (not o.dma) and ename == "tensor":
                            continue
                        key = (ename, id(d.sem))
                        if waited.get(key, 0) >= d.val:
                            continue
                        eng.wait_ge(d.sem, d.val)
                        waited[key] = d.val
                    ins = o.fn(eng)
                    if o.dma:
                        ins.then_inc(o.sem, 16)
                    elif o.needed:
                        ins.then_inc(o.sem, 1)
                for o in per[ename]:
                    if o.dma:
                        key = (ename, id(o.sem))
                        if waited.get(key, 0) >= o.val:
                            continue
                        eng.wait_ge(o.sem, o.val)
                        waited[key] = o.val
            return f

        with nc.Block() as block:
            block.sync(body("sync"))
            block.scalar(body("scalar"))
            block.vector(body("vector"))
            block.gpsimd(body("gpsimd"))
            block.tensor(body("tensor"))
        self.nphase += 1
        self.ops = []
        self.dlast = [None] * NPOOL
        for r in self.all_res:
            r.w = []
            r.r = []
        self.all_res = []


def _bucket(rel):
    rel = np.maximum(rel, 0)
    max_exact = 16
    rel_f = np.maximum(rel, 1).astype(np.float32)
    large = max_exact + (np.log(rel_f / np.float32(max_exact)) / np.float32(math.log(128 / 16))
                         * np.float32(16)).astype(np.int32)
    large = np.minimum(large, 31)
    return np.where(rel < max_exact, rel, large)


def _consts():
    c = {}
    p = np.arange(128)
    c["identf"] = np.eye(128, dtype=np.float32)
    kind = np.zeros((64, S), np.float32)
    for j in range(16):
        kind[j, j * 256:(j + 1) * 256] = 1.0
    c["kind"] = kind
    gm = np.zeros((32, 16), np.float32)
    own = np.ones((32, 16), np.float32)
    for t in range(32):
        gm[t, (t // 2):] = -1e30
        own[t, t // 2] = 0.0
    c["gmadd"] = np.broadcast_to(gm.reshape(1, 512), (128, 512)).copy()
    c["own01"] = np.broadcast_to(own.reshape(1, 512), (128, 512)).copy()
    k = p[:, None]
    q = p[None, :]
    c["neg0"] = np.where(q < k, NEGM, 0.0).astype(np.float32)
    sb = np.zeros((128, 4, 512), np.float32)
    ql = np.arange(512)[None, :]
    for cc in range(4):
        sb[:, cc, :] = np.where((cc * 128 + k) >= ql, NEGM, 0.0)
    c["sbneg"] = sb.reshape(128, 2048)
    u2 = np.zeros((128, 32, 128), np.float32)
    for kt in range(32):
        for m in range(64):
            if (m % 32) < kt:
                u2[:, kt, m] = 1.0
    c["u2"] = u2.reshape(128, 4096)
    sel = np.zeros((128, 32, 128), np.float32)
    for kt in range(32):
        sel[kt, kt, :] = -1.0
        sel[32 + kt, kt, :] = -1.0
    c["sel"] = sel.reshape(128, 4096)
    c["tri"] = np.where(p[:, None] >= p[None, :], -1.0, 0.0).astype(np.float32)
    c["tst"] = np.where(p[:, None] < p[None, :], 1.0, 0.0).astype(np.float32)
    c["ebase"] = np.broadcast_to((np.arange(32, dtype=np.float32) * CAP)[None, :], (128, 32)).copy()
    return c


CONST_SHAPES = {
    "identf": [128, 128], "kind": [64, S], "gmadd": [128, 512], "own01": [128, 512], "neg0": [128, 128],
    "sbneg": [128, 2048], "u2": [128, 4096], "sel": [128, 4096], "tri": [128, 128], "tst": [128, 128],
    "ebase": [128, 32],
}

SHARED_SHAPES = {
    "w_in": [D, 5632], "w_kv": [D, 512], "wbr": [D, D], "w_out": [D, D],
    "ln1g": [128, D], "ln1b": [128, D], "ln2g": [128, D], "ln2b": [128, D],
    "w_r": [D, 36], "b_r": [128, 36], "w_gate": [32, D, 512], "w_up": [32, D, 512], "w_down": [32, 512, D],
    "dg": [128, 6 * 2 * 128], "b31": [128, 6],
}
CORE_SHAPES = {"xT": [D, S], "x": [S, D], "memT": [D, 256]}


def _prep(inputs):
    f = lambda a: np.ascontiguousarray(np.asarray(a, dtype=np.float32))
    sh = {}
    sh["w_in"] = f(inputs["w_in"][0])
    sh["w_kv"] = f(inputs["w_mem_kv"][0])
    wbr = np.concatenate([inputs["w_br_moba"][0], inputs["w_br_sb"][0], inputs["w_br_mem"][0]], axis=0)
    sh["wbr"] = f(wbr)
    sh["w_out"] = f(inputs["w_out"][0])
    for nm, key in (("ln1g", "ln1_g"), ("ln1b", "ln1_b"), ("ln2g", "ln2_g"), ("ln2b", "ln2_b")):
        sh[nm] = f(np.broadcast_to(np.asarray(inputs[key][0])[None, :], (128, D)))
    sh["w_r"] = f(np.concatenate([inputs["w_router_group"][0], inputs["w_router_expert"][0]], axis=1))
    br = np.concatenate([inputs["b_router_group"][0], inputs["b_router_expert"][0]], axis=0)
    sh["b_r"] = f(np.broadcast_to(np.asarray(br)[None, :], (128, 36)))
    sh["w_gate"] = f(inputs["w_gate"][0])
    sh["w_up"] = f(inputs["w_up"][0])
    sh["w_down"] = f(inputs["w_down"][0])
    rb = np.asarray(inputs["rel_bias"], dtype=np.float32)
    p = np.arange(128)
    rel0 = p[None, :] - p[:, None]
    rel1 = rel0 + 128
    idx = np.stack([_bucket(rel0), _bucket(rel1)], axis=1)
    dg = rb[idx]
    sh["dg"] = f(dg.transpose(0, 3, 1, 2).reshape(128, 6 * 2 * 128))
    sh["b31"] = f(np.broadcast_to(rb[31][None, :], (128, 6)))
    sh.update(_consts())
    x = np.asarray(inputs["x"], dtype=np.float32)
    mem = np.asarray(inputs["mem"], dtype=np.float32)
    maps = []
    for b in range(x.shape[0]):
        m = dict(sh)
        m["x"] = np.ascontiguousarray(x[b])
        m["xT"] = np.ascontiguousarray(x[b].T)
        m["memT"] = np.ascontiguousarray(mem[b].T)
        maps.append(m)
    return maps


def build(stage=99, debug=False):
    nc = bass.Bass("TRN2", target_bir_lowering=False)
    I = {}
    for nm, shp in list(CORE_SHAPES.items()) + list(SHARED_SHAPES.items()) + list(CONST_SHAPES.items()):
        I[nm] = nc.dram_tensor(nm, shp, F32, kind="ExternalInput").ap()
    out_d = nc.dram_tensor("out", [S, D], F32, kind="ExternalOutput").ap()
    skind = "ExternalOutput" if debug else "Internal"

    def scratch(nm, shp, dt):
        return nc.dram_tensor(nm, shp, dt, kind=skind).ap()

    QA = scratch("QA", [6, 64, S], BF16)
    KA = scratch("KA", [6, 64, S], BF16)
    VA = scratch("VA", [6, 128, 32 * 64], BF16)
    QB = scratch("QB", [3, 128, S], BF16)
    KB = scratch("KB", [3, 128, S], BF16)
    VB = scratch("VB", [3, 128, 32 * 128], BF16)
    QM = scratch("QM", [2, 128, S], BF16)
    G = scratch("G", [24, 128, S], BF16)
    YT = scratch("YT", [16, 64, S], BF16)
    X1A = scratch("X1A", [S, D], F32)
    XP = nc.dram_tensor("XP", [NSLOT, D], BF16, kind="Internal").ap()
    YP = nc.dram_tensor("YP", [NSLOT, D], BF16, kind="Internal").ap()
    if debug:
        SLOTD = scratch("SLOTD", [128, 64], I32)
        GATED = scratch("GATED", [128, 64], F32)

    with ExitStack() as ges:
        K = Sched(nc, ges)
        slotS = ges.enter_context(nc.sbuf_tensor("slotS", [128, 64], I32))
        gateS = ges.enter_context(nc.sbuf_tensor("gateS", [128, 64], F32))

        def ins(eng, name, reads, writes, append=False, **kw):
            return K.op(eng, lambda e: getattr(e, name)(**kw), reads, writes, append=append)

        def dma(eng, out, in_, reads, writes, append=False):
            return K.op(eng, lambda e: e.dma_start(out=out, in_=in_), reads, writes, dma=True, append=append)

        def mm(out, lhsT, rhs, start, stop, reads, w, first):
            return K.op("tensor", lambda e: e.matmul(out, lhsT=lhsT, rhs=rhs, start=start, stop=stop),
                        reads, [w], append=not first)


        def run_segments(segs, LA=2):
            hoisted = [0] * len(segs)
            for si, seg in enumerate(segs):
                steps = seg["steps"]
                n = len(steps)
                if seg.get("pre") is not None:
                    seg["pre"]()
                for k in range(hoisted[si], min(LA, n)):
                    steps[k][0]()
                for k in range(n):
                    steps[k][1]()
                    steps[k][2]()
                    if k + LA < n:
                        steps[k + LA][0]()
                    elif si + 1 < len(segs) and segs[si + 1].get("hoist", False):
                        j = k + LA - n
                        nxt = segs[si + 1]["steps"]
                        if j < min(LA, len(nxt)) and j == hoisted[si + 1]:
                            nxt[j][0]()
                            hoisted[si + 1] = j + 1
                if seg.get("post") is not None:
                    seg["post"]()

        with ExitStack() as es:
            sb = lambda n, s, d: es.enter_context(nc.sbuf_tensor(n, s, d))
            ps = lambda n: es.enter_context(nc.psum_tensor(n, [128, 512], F32))
            win = sb("p1_win", [128, 8, 5632], BF16)
            r_win = [K.res() for _ in range(11)]
            xt = [sb("p1_xt%d" % i, [128, 8, 512], BF16) for i in range(2)]
            r_xt = [K.res() for _ in range(2)]
            NO = 6
            ost = [sb("p1_o%d" % i, [128, 512], BF16) for i in range(NO)]
            r_ost = [K.res() for _ in range(NO)]
            vst = [sb("p1_v%d" % i, [128, 384], BF16) for i in range(2)]
            r_vst = [K.res() for _ in range(2)]
            psA = [ps("p1_pa%d" % i) for i in range(5)]
            r_psA = [K.res() for _ in range(5)]
            psV = [ps("p1_pv%d" % i) for i in range(2)]
            r_psV = [K.res() for _ in range(2)]
            w_in_v = I["w_in"].rearrange("(c p) n -> p c n", p=128)
            xT_v = I["xT"].rearrange("(c p) t -> p c t", p=128)
            dma("gpsimd", xt[0][:, :, :], xT_v[:, :, 0:512], [], [r_xt[0]])
            for cb in range(11):
                dma("gpsimd", win[:, :, cb * 512:(cb + 1) * 512], w_in_v[:, :, cb * 512:(cb + 1) * 512], [], [r_win[cb]])
            groups = []
            QAf = QA.rearrange("h p t -> (h p) t")
            KAf = KA.rearrange("h p t -> (h p) t")
            for i in range(3):
                groups.append((128 * i, 128, (lambda s, i=i: QAf[128 * i:128 * (i + 1), s * 512:(s + 1) * 512]), "q"))
            for i in range(3):
                groups.append((384 + 128 * i, 128, (lambda s, i=i: KAf[128 * i:128 * (i + 1), s * 512:(s + 1) * 512]), "k"))
            for i in range(3):
                groups.append((1152 + 128 * i, 128, (lambda s, i=i: QB[i, :, s * 512:(s + 1) * 512]), "q"))
            for i in range(3):
                groups.append((1536 + 128 * i, 128, (lambda s, i=i: KB[i, :, s * 512:(s + 1) * 512]), "k"))
            for i in range(2):
                groups.append((2304 + 128 * i, 128, (lambda s, i=i: QM[i, :, s * 512:(s + 1) * 512]), "q"))
            for i in range(24):
                groups.append((2560 + 128 * i, 128, (lambda s, i=i: G[i, :, s * 512:(s + 1) * 512]), "g"))
            VA_v = VA.rearrange("h p (t d) -> p h t d", d=64)
            VB_v = VB.rearrange("i p (t d) -> p i t d", d=128)
            gi = 0
            for s in range(NST):
                xb = xt[s % 2]
                rxb = r_xt[s % 2]
                if s + 1 < NST:
                    dma("gpsimd", xt[(s + 1) % 2][:, :, :], xT_v[:, :, (s + 1) * 512:(s + 2) * 512], [], [r_xt[(s + 1) % 2]])
                for (c0, ncol, dst, kind) in groups:
                    pi = gi % 5
                    oi = gi % NO
                    gi += 1
                    for c in range(8):
                        mm(psA[pi][0:ncol, :], win[:, c, c0:c0 + ncol], xb[:, c, :], c == 0, c == 7,
                           [r_win[c0 // 512], rxb], r_psA[pi], c == 0)
                    if kind == "g":
                        ins("scalar", "activation", [r_psA[pi]], [r_ost[oi]], out=ost[oi][0:ncol, :], in_=psA[pi][0:ncol, :], func=AF.Sigmoid)
                    elif kind == "q":
                        ins("vector", "tensor_scalar", [r_psA[pi]], [r_ost[oi]], out=ost[oi][0:ncol, :], in0=psA[pi][0:ncol, :],
                            scalar1=0.125, scalar2=None, op0=ALU.mult)
                    else:
                        ins("vector", "tensor_copy", [r_psA[pi]], [r_ost[oi]], out=ost[oi][0:ncol, :], in_=psA[pi][0:ncol, :])
                    dma("sync", dst(s), ost[oi][0:ncol, :], [r_ost[oi]], [])
                for tt in range(4):
                    t = 4 * s + tt
                    for vi, (c0, blks) in enumerate(((768, (1, 2)), (1920, (3, 4)))):
                        for c in range(8):
                            mm(psV[vi][:, 0:384], xb[:, c, tt * 128:(tt + 1) * 128], win[:, c, c0:c0 + 384], c == 0, c == 7,
                               [r_win[blks[0]], r_win[blks[1]], rxb], r_psV[vi], c == 0)
                        ins("vector", "tensor_copy", [r_psV[vi]], [r_vst[vi]], out=vst[vi][:, :], in_=psV[vi][:, 0:384])
                        if vi == 0:
                            dma("sync", VA_v[:, :, t, :], vst[vi][:, :].rearrange("p (h d) -> p h d", d=64), [r_vst[vi]], [])
                        else:
                            dma("sync", VB_v[:, :, t, :], vst[vi][:, :].rearrange("p (i d) -> p i d", d=128), [r_vst[vi]], [])
            K.emit()
        if stage <= 1:
            return nc

        with ExitStack() as es:
            sb = lambda n, s, d: es.enter_context(nc.sbuf_tensor(n, s, d))
            kaug = [sb("p2_k%d" % i, [128, S], BF16) for i in range(6)]
            qaug = [sb("p2_q%d" % i, [128, S], BF16) for i in range(6)]
            vaug = [sb("p2_v%d" % i, [128, 32, 128], BF16) for i in range(6)]
            esel = sb("p2_esel", [128, 64], F32)
            identb = sb("p2_id", [128, 128], BF16)
            ones64 = sb("p2_ones", [128, 64], BF16)
            dfin = sb("p2_dfin", [128, 6, 2, 128], BF16)
            with ExitStack() as es2:
                sb2 = lambda n, s, d: es2.enter_context(nc.sbuf_tensor(n, s, d))
                r_k = [K.res() for _ in range(6)]
                r_kind = [K.res() for _ in range(6)]
                r_q = [K.res() for _ in range(6)]
                r_qm = [[K.res() for _ in range(NST)] for _ in range(6)]
                r_v = [K.res() for _ in range(6)]
                r_id = K.res(); r_ones = K.res(); r_dfin = K.res()
                dgf = sb2("p2_dgf", [128, 1536], F32); r_dgf = K.res()
                b31 = sb2("p2_b31", [128, 6], F32); r_b31 = K.res()
                neg0 = sb2("p2_neg0", [128, 128], F32); r_neg0 = K.res()
                gmadd = sb2("p2_gmadd", [128, 512], F32); r_gmadd = K.res()
                own01 = sb2("p2_own01", [128, 512], F32); r_own = K.res()
                ksum = sb2("p2_ksum", [128, 16], F32); r_ksum = K.res()
                kmb = [sb2("p2_kmb%d" % i, [128, 16], BF16) for i in range(2)]; r_kmb = [K.res() for _ in range(2)]
                gm = [sb2("p2_gm%d" % i, [128, 512], F32) for i in range(2)]; r_gm = [K.res() for _ in range(2)]
                m8 = sb2("p2_m8", [128, 32, 8], F32); r_m8 = K.res()
                mbf = sb2("p2_mbf", [128, 512], F32); r_mbf = K.res()
                mbpad = [sb2("p2_mbpad%d" % i, [128, 32, 80], BF16) for i in range(2)]; r_mbpad = [K.res() for _ in range(2)]
                gps = [es2.enter_context(nc.psum_tensor("p2_g%d" % i, [128, 512], F32)) for i in range(2)]; r_gps = [K.res() for _ in range(2)]
                tps = [es2.enter_context(nc.psum_tensor("p2_t%d" % i, [128, 512], F32)) for i in range(2)]; r_tps = [K.res() for _ in range(2)]
                dma("gpsimd", identb[:, :], I["identf"], [], [r_id])
                dma("sync", dgf[:, :], I["dg"], [], [r_dgf])
                dma("sync", b31[:, :], I["b31"], [], [r_b31])
                dma("sync", neg0[:, :], I["neg0"], [], [r_neg0])
                dma("sync", gmadd[:, :], I["gmadd"], [], [r_gmadd])
                dma("sync", own01[:, :], I["own01"], [], [r_own])
                for h in range(6):
                    dma("sync", kaug[h][0:64, :], KA[h], [], [r_k[h]])
                    dma("scalar", qaug[h][0:64, :], QA[h], [], [r_q[h]])
                    dma("sync", vaug[h][:, :, 0:64], VA[h].rearrange("p (t d) -> p t d", d=64), [], [r_v[h]])
                    ins("gpsimd", "memset", [], [r_v[h]], append=True, ap=vaug[h][:, :, 64:128], constant=1.0)
                    dma("gpsimd", kaug[h][64:128, :].rearrange("p (a n) -> p a n", n=2048), I["kind"].rearrange("p (a n) -> p a n", n=2048),
                        [], [r_kind[h]])
                    ins("gpsimd", "memset", [], r_qm[h], ap=qaug[h][64:128, :], constant=0.0)
                ins("gpsimd", "memset", [], [r_ones], ap=ones64[:, :], constant=1.0)
                ins("gpsimd", "memset", [], [r_ones], ap=esel[:, :], constant=0.0)
                ins("gpsimd", "memset", [r_ones], [r_ones], ap=esel[64:65, :], constant=1.0)
                for i in range(2):
                    ins("gpsimd", "memset", [], [r_mbpad[i]], ap=mbpad[i][:, :, :], constant=0.0)
                    ins("gpsimd", "memset", [], [r_kmb[i]], ap=kmb[i][:, :], constant=0.0)
                for h in range(6):
                    for t in range(2):
                        src = dgf[:, (h * 2 + t) * 128:(h * 2 + t + 1) * 128]
                        if t == 0:
                            ins("vector", "scalar_tensor_tensor", [r_dgf, r_b31, r_neg0], [r_dfin], append=True, out=dfin[:, h, t, :],
                                in0=src, scalar=b31[:, h:h + 1], in1=neg0[:, :], op0=ALU.subtract, op1=ALU.add)
                        else:
                            ins("vector", "tensor_scalar", [r_dgf, r_b31], [r_dfin], append=True, out=dfin[:, h, t, :], in0=src,
                                scalar1=b31[:, h:h + 1], scalar2=None, op0=ALU.subtract)

                def prologue1(h):
                    i = h % 2
                    ins("vector", "tensor_reduce", [r_k[h]], [r_ksum], out=ksum[0:64, :],
                        in_=kaug[h][0:64, :].rearrange("p (j k) -> p j k", k=256), axis=AX.X, op=ALU.add)
                    ins("vector", "tensor_copy", [r_ksum], [r_kmb[i]], out=kmb[i][0:64, :], in_=ksum[0:64, :])
                    for t in range(32):
                        mm(gps[i][:, t * 16:(t + 1) * 16], qaug[h][:, t * 128:(t + 1) * 128], kmb[i][:, :], True, True,
                           [r_q[h], r_kmb[i]] + r_qm[h], r_gps[i], t == 0)
                    ins("vector", "tensor_tensor", [r_gps[i], r_gmadd], [r_gm[i]], out=gm[i][:, :], in0=gps[i][:, :], in1=gmadd[:, :], op=ALU.add)
                    for t in range(32):
                        ins("vector", "max", [r_gm[i]], [r_m8], append=(t > 0), out=m8[:, t, :], in_=gm[i][:, t * 16:(t + 1) * 16])
                    ins("vector", "tensor_tensor", [r_gm[i], r_m8], [r_mbf], out=mbf[:, :].rearrange("p (t j) -> p t j", j=16),
                        in0=gm[i][:, :].rearrange("p (t j) -> p t j", j=16), in1=m8[:, :, 2:3].to_broadcast([128, 32, 16]), op=ALU.is_lt)
                    ins("vector", "scalar_tensor_tensor", [r_mbf, r_own], [r_mbpad[i]], out=mbpad[i][:, :, 64:80],
                        in0=mbf[:, :].rearrange("p (t j) -> p t j", j=16), scalar=NEGM,
                        in1=own01[:, :].rearrange("p (t j) -> p t j", j=16), op0=ALU.mult, op1=ALU.mult)

                def prologue2(h):
                    i = h % 2
                    for s in range(NST):
                        ti = s % 2
                        for c in range(4):
                            t = 4 * s + c
                            mm(tps[ti][0:80, c * 128:(c + 1) * 128], mbpad[i][:, t, :], identb[:, :], True, True, [r_mbpad[i], r_id], r_tps[ti], c == 0)
                        ins("scalar", "copy", [r_tps[ti]], [r_qm[h][s]], out=qaug[h][64:80, s * 512:(s + 1) * 512], in_=tps[ti][64:80, :])

                prologue1(0)
                for h in range(6):
                    if h + 1 < 6:
                        prologue1(h + 1)
                    prologue2(h)
                K.emit()
            with ExitStack() as es2:
                sb2 = lambda n, s, d: es2.enter_context(nc.sbuf_tensor(n, s, d))
                r_all = K.res()
                pt = [sb2("p2_pt%d" % i, [128, 1024], BF16) for i in range(2)]; r_pt = [K.res() for _ in range(2)]
                rden = [sb2("p2_rden%d" % i, [128, 512], F32) for i in range(2)]; r_rden = [K.res() for _ in range(2)]
                densb = [sb2("p2_densb%d" % i, [128, 512], F32) for i in range(2)]; r_densb = [K.res() for _ in range(2)]
                for i in range(2):
                    ins("gpsimd", "memset", [], [r_densb[i]], ap=densb[i][:, :], constant=0.0)
                yo = [sb2("p2_yo%d" % i, [128, 512], BF16) for i in range(2)]; r_yo = [K.res() for _ in range(2)]
                sps = [es2.enter_context(nc.psum_tensor("p2_s%d" % i, [128, 1024], F32)) for i in range(2)]; r_sps = [K.res() for _ in range(2)]
                nps = [es2.enter_context(nc.psum_tensor("p2_n%d" % i, [128, 512], F32)) for i in range(2)]; r_nps = [K.res() for _ in range(2)]
                dps = [es2.enter_context(nc.psum_tensor("p2_d%d" % i, [128, 512], F32)) for i in range(2)]; r_dps = [K.res() for _ in range(2)]
                cnt2 = {"it": 0}

                def make_seg(h, s):
                    nkt = 4 * s + 4
                    a = (h * NST + s) % 2
                    steps = []
                    for j in range(nkt // 2):
                        st = {}
                        los = [max(0, 2 * j + h2 - 4 * s) * 128 for h2 in range(2)]

                        def A(j=j, st=st, los=los):
                            si = cnt2["it"] % 2
                            cnt2["it"] += 1
                            st["si"] = si
                            for h2 in range(2):
                                kt = 2 * j + h2
                                lo = los[h2]
                                base = h2 * 512
                                cd = kt - 4 * s
                                cp = kt - 4 * s + 1
                                has_d0 = 0 <= cd <= 3
                                has_d1 = 0 <= cp <= 3
                                mm(sps[si][:, base + lo:base + 512], kaug[h][:, kt * 128:(kt + 1) * 128], qaug[h][:, s * 512 + lo:(s + 1) * 512],
                                   True, not (has_d0 or has_d1), [r_all], r_sps[si], h2 == 0)
                                if has_d0:
                                    mm(sps[si][:, base + cd * 128:base + (cd + 1) * 128], identb[:, :], dfin[:, h, 0, :], False, not has_d1, [r_all], r_sps[si], False)
                                if has_d1:
                                    mm(sps[si][:, base + cp * 128:base + (cp + 1) * 128], identb[:, :], dfin[:, h, 1, :], False, True, [r_all], r_sps[si], False)

                        def B(j=j, st=st, los=los):
                            si = st["si"]
                            if los[0] == 0 and los[1] == 0:
                                ins("scalar", "activation", [r_sps[si]], [r_pt[si]], out=pt[si][:, :], in_=sps[si][:, :], func=AF.Exp)
                            else:
                                for h2 in range(2):
                                    lo = h2 * 512 + los[h2]
                                    hi = (h2 + 1) * 512
                                    ins("scalar", "activation", [r_sps[si]], [r_pt[si]], append=(h2 > 0), out=pt[si][:, lo:hi], in_=sps[si][:, lo:hi],
                                        func=AF.Exp)

                        def C(j=j, st=st, los=los):
                            si = st["si"]
                            for h2 in range(2):
                                kt = 2 * j + h2
                                lo = los[h2]
                                base = h2 * 512
                                mm(nps[a][:, lo:512], vaug[h][:, kt, :], pt[si][:, base + lo:base + 512], kt == 0, kt == nkt - 1, [r_all, r_pt[si]],
                                   r_nps[a], kt == 0)

                        steps.append((A, B, C))

                    def post():
                        ins("scalar", "copy", [r_nps[a]], [r_densb[a]], out=densb[a][64:128, :], in_=nps[a][64:128, :])
                        mm(dps[a][0:64, :], esel[:, :], densb[a][:, :], True, True, [r_all, r_densb[a]], r_dps[a], True)
                        ins("vector", "reciprocal", [r_dps[a]], [r_rden[a]], out=rden[a][0:64, :], in_=dps[a][0:64, :])
                        ins("vector", "tensor_tensor", [r_nps[a], r_rden[a]], [r_yo[a]], out=yo[a][0:64, :], in0=nps[a][0:64, :],
                            in1=rden[a][0:64, :], op=ALU.mult)
                        dma("sync", YT[h, :, s * 512:(s + 1) * 512], yo[a][0:64, :], [r_yo[a]], [])
                    return {"steps": steps, "pre": None, "post": post, "hoist": True}

                segs = []
                for h in range(6):
                    for s in range(NST):
                        segs.append(make_seg(h, s))
                run_segments(segs)
                K.emit()
        if stage <= 2:
            return nc

        with ExitStack() as es:
            sb = lambda n, s, d: es.enter_context(nc.sbuf_tensor(n, s, d))
            ps = lambda n: es.enter_context(nc.psum_tensor(n, [128, 512], F32))
            kb2 = [sb("p3_k%d" % i, [128, S], BF16) for i in range(2)]
            qz = [[sb("p3_q%d_%d" % (i, j), [128, S], BF16) for j in range(2)] for i in range(2)]
            vb2 = [sb("p3_v%d" % i, [128, 32, 128], BF16) for i in range(2)]
            r_k = [K.res() for _ in range(2)]
            r_q = [K.res() for _ in range(2)]
            r_v = [K.res() for _ in range(2)]
            identb = sb("p3_id", [128, 128], BF16); r_id = K.res()
            sbneg = sb("p3_neg", [128, 4, 512], BF16); r_neg = K.res()
            u2 = sb("p3_u2", [128, 32, 128], BF16); r_u2 = K.res()
            sel = sb("p3_sel", [128, 32, 128], BF16); r_sel = K.res()
            tri = sb("p3_tri", [128, 128], BF16); r_tri = K.res()
            lbuf2 = [[sb("p3_l%d_%d" % (j, i), [128, 1024], BF16) for i in range(16)] for j in range(2)]
            r_l2 = [[K.res() for _ in range(16)] for _ in range(2)]
            ebuf = [sb("p3_e%d" % i, [128, 1024], F32) for i in range(2)]
            r_e = [K.res() for _ in range(2)]
            at = [sb("p3_a%d" % i, [128, 1024], BF16) for i in range(3)]
            r_at = [K.res() for _ in range(3)]
            rhl = [sb("p3_r%d" % i, [128, 512], BF16) for i in range(2)]
            r_rhl = [K.res() for _ in range(2)]
            yo = [sb("p3_yo%d" % i, [128, 512], BF16) for i in range(2)]
            r_yo = [K.res() for _ in range(2)]
            zb = [es.enter_context(nc.psum_tensor("p3_z%d" % i, [128, 1024], F32)) for i in range(3)]; r_zb = [K.res() for _ in range(3)]
            rps = [ps("p3_rm0")] * 2; r_rps = [K.res()] * 2
            ops_ = [ps("p3_o0")] * 2; r_ops = [K.res()] * 2
            dma("gpsimd", identb[:, :], I["identf"], [], [r_id])
            dma("gpsimd", sbneg[:, :, :], I["sbneg"].rearrange("p (c q) -> p c q", q=512), [], [r_neg])
            dma("gpsimd", u2[:, :, :], I["u2"].rearrange("p (k m) -> p k m", m=128), [], [r_u2])
            dma("gpsimd", sel[:, :, :], I["sel"].rearrange("p (k m) -> p k m", m=128), [], [r_sel])
            dma("gpsimd", tri[:, :], I["tri"], [], [r_tri])
            for b_ in range(2):
                ins("gpsimd", "memset", [], [r_rhl[b_]], ap=rhl[b_][:, :], constant=0.0)
                for j_ in range(2):
                    ins("gpsimd", "memset", [], [r_q[b_]], append=(j_ > 0), ap=qz[b_][j_][:, :], constant=0.0)

            def load_pair(i):
                b = i % 2
                dma("sync", kb2[b][:, :], KB[i], [], [r_k[b]])
                for j in range(2):
                    dma("sync", qz[b][j][64 * j:64 * j + 64, :], QB[i, 64 * j:64 * j + 64, :], [], [r_q[b]], append=(j > 0))
                dma("sync", vb2[b][:, :, :], VB[i].rearrange("p (t d) -> p t d", d=128), [], [r_v[b]])

            cnt3 = {"z": 0, "e": 0}

            def make_unit(i, hp, s):
                b = i % 2
                p0 = 64 * hp
                hh = 2 * i + hp
                nkt = 4 * s + 4
                a = (hh * NST + s) % 2
                lbuf = lbuf2[a]
                r_l = r_l2[a]
                qs = qz[b][hp][:, s * 512:(s + 1) * 512]
                steps1 = []
                steps2 = []
                for j in range(nkt // 2):
                    st1 = {}
                    st2 = {}

                    los = [max(0, 2 * j + h2 - 4 * s) * 128 for h2 in range(2)]

                    def A1(j=j, st=st1, los=los):
                        zi = cnt3["z"] % 3
                        cnt3["z"] += 1
                        st["zi"] = zi
                        for h2 in range(2):
                            kt = 2 * j + h2
                            diag = kt >= 4 * s
                            lo = los[h2]
                            base = h2 * 512
                            mm(zb[zi][:, base + lo:base + 512], kb2[b][:, kt * 128:(kt + 1) * 128], qs[:, lo:512], True, not diag,
                               [r_k[b], r_q[b]], r_zb[zi], h2 == 0)
                            if diag:
                                mm(zb[zi][:, base + lo:base + lo + 128], identb[:, :], sbneg[:, kt - 4 * s, lo:lo + 128], False, True,
                                   [r_id, r_neg], r_zb[zi], False)

                    def B1(j=j, st=st1, los=los):
                        zi = st["zi"]
                        ei = cnt3["e"] % 2
                        cnt3["e"] += 1
                        if los[0] == 0 and los[1] == 0:
                            ins("scalar", "activation", [r_zb[zi]], [r_e[ei]], out=ebuf[ei][:, :], in_=zb[zi][:, :], func=AF.Exp)
                            ins("scalar", "activation", [r_e[ei]], [r_l[j]], out=lbuf[j][:, :], in_=ebuf[ei][:, :], func=AF.Ln, bias=1.0, scale=1.0)
                        else:
                            for h2 in range(2):
                                c0_, c1_ = h2 * 512 + los[h2], (h2 + 1) * 512
                                ins("scalar", "activation", [r_zb[zi]], [r_e[ei]], append=(h2 > 0), out=ebuf[ei][:, c0_:c1_], in_=zb[zi][:, c0_:c1_],
                                    func=AF.Exp)
                                ins("scalar", "activation", [r_e[ei]], [r_l[j]], append=(h2 > 0), out=lbuf[j][:, c0_:c1_], in_=ebuf[ei][:, c0_:c1_],
                                    func=AF.Ln, bias=1.0, scale=1.0)

                    def C1(j=j, st=st1, los=los):
                        for h2 in range(2):
                            kt = 2 * j + h2
                            lo = los[h2]
                            mm(rps[a][:, lo:512], u2[:, kt, :], lbuf[j][:, h2 * 512 + lo:(h2 + 1) * 512], kt == 0, kt == nkt - 1, [r_u2, r_l[j]],
                               r_rps[a], kt == 0)

                    def A2(j=j, st=st2, los=los):
                        zi = cnt3["z"] % 3
                        cnt3["z"] += 1
                        st["zi"] = zi
                        for h2 in range(2):
                            kt = 2 * j + h2
                            diag = kt >= 4 * s
                            lo = los[h2]
                            base = h2 * 512
                            zo = zb[zi][:, base + lo:base + 512]
                            mm(zo, kb2[b][:, kt * 128:(kt + 1) * 128], qs[:, lo:512], True, False, [r_k[b], r_q[b]], r_zb[zi], h2 == 0)
                            if diag:
                                mm(zb[zi][:, base + lo:base + lo + 128], identb[:, :], sbneg[:, kt - 4 * s, lo:lo + 128], False, False,
                                   [r_id, r_neg], r_zb[zi], False)
                            mm(zo, tri[:, :], lbuf[j][:, base + lo:base + 512], False, False, [r_tri, r_l[j]], r_zb[zi], False)
                            mm(zo, sel[:, kt, :], rhl[a][:, lo:512], False, True, [r_sel, r_rhl[a]], r_zb[zi], False)

                    def B2(j=j, st=st2, los=los):
                        zi = st["zi"]
                        if los[0] == 0 and los[1] == 0:
                            ins("scalar", "activation", [r_zb[zi]], [r_at[zi]], out=at[zi][:, :], in_=zb[zi][:, :], func=AF.Exp)
                        else:
                            for h2 in range(2):
                                c0_, c1_ = h2 * 512 + los[h2], (h2 + 1) * 512
                                ins("scalar", "activation", [r_zb[zi]], [r_at[zi]], append=(h2 > 0), out=at[zi][:, c0_:c1_], in_=zb[zi][:, c0_:c1_],
                                    func=AF.Exp)

                    def C2(j=j, st=st2, los=los):
                        zi = st["zi"]
                        for h2 in range(2):
                            kt = 2 * j + h2
                            lo = los[h2]
                            mm(ops_[a][:, lo:512], vb2[b][:, kt, :], at[zi][:, h2 * 512 + lo:(h2 + 1) * 512], kt == 0, kt == nkt - 1,
                               [r_v[b], r_at[zi]], r_ops[a], kt == 0)

                    steps1.append((A1, B1, C1))
                    steps2.append((A2, B2, C2))

                def pre2():
                    ins("vector", "tensor_copy", [r_rps[a]], [r_rhl[a]], out=rhl[a][0:64, :], in_=rps[a][0:64, :])
                    ins("vector", "tensor_tensor", [r_rps[a], r_rhl[a]], [r_rhl[a]], out=rhl[a][32:64, :], in0=rps[a][32:64, :],
                        in1=rhl[a][32:64, :], op=ALU.subtract)

                def post2():
                    ins("vector", "tensor_copy", [r_ops[a]], [r_yo[a]], out=yo[a][p0:p0 + 64, :], in_=ops_[a][p0:p0 + 64, :])
                    dma("sync", YT[6 + hh, :, s * 512:(s + 1) * 512], yo[a][p0:p0 + 64, :], [r_yo[a]], [])
                    if hp == 1 and s == NST - 1 and i + 2 < 3:
                        load_pair(i + 2)

                return (steps1, steps2, pre2, post2)

            def interleave(x, y):
                out = []
                i = j = 0
                while i < len(x) or j < len(y):
                    if j >= len(y) or (i < len(x) and i * len(y) <= j * len(x)):
                        out.append(x[i]); i += 1
                    else:
                        out.append(y[j]); j += 1
                return out

            load_pair(0)
            load_pair(1)
            units = []
            for i in range(3):
                for hp in range(2):
                    for s in range(NST):
                        units.append(make_unit(i, hp, s))
            segs = [{"steps": units[0][0], "pre": None, "post": None, "hoist": False}]
            for u in range(len(units)):
                nxt1 = units[u + 1][0] if u + 1 < len(units) else []
                segs.append({"steps": nxt1[0:2] + interleave(units[u][1], nxt1[2:]), "pre": units[u][2], "post": units[u][3],
                             "hoist": len(nxt1) >= 2})
            run_segments(segs)
            K.emit()
        if stage <= 3:
            return nc

        with ExitStack() as es:
            sb = lambda n, s, d: es.enter_context(nc.sbuf_tensor(n, s, d))
            ps = lambda n: es.enter_context(nc.psum_tensor(n, [128, 512], F32))
            memT = sb("p4_mem", [128, 8, 256], BF16); r_mem = K.res()
            wkv = sb("p4_wkv", [128, 8, 512], BF16); r_wkv = K.res()
            km = sb("p4_km", [128, 2, 256], BF16); r_km = K.res()
            vm = sb("p4_vm", [128, 2, 256], BF16); r_vm = K.res()
            qmz = [sb("p4_qm%d" % i, [128, S], BF16) for i in range(4)]; r_qm = K.res()
            ones64 = sb("p4_ones", [128, 64], BF16); r_ones = K.res()
            pt4 = [sb("p4_pt%d" % i, [128, 1024], BF16) for i in range(2)]
            r_pt = [K.res() for _ in range(2)]
            rden = [sb("p4_rden%d" % i, [128, 512], F32) for i in range(2)]
            r_rden = [K.res() for _ in range(2)]
            yo = [sb("p4_yo%d" % i, [128, 512], BF16) for i in range(2)]
            r_yo = [K.res() for _ in range(2)]
            sps4 = [es.enter_context(nc.psum_tensor("p4_s%d" % i, [128, 1024], F32)) for i in range(2)]; r_sps = [K.res() for _ in range(2)]
            nps = [ps("p4_n%d" % i) for i in range(2)]; r_nps = [K.res() for _ in range(2)]
            dps = [ps("p4_d%d" % i) for i in range(2)]; r_dps = [K.res() for _ in range(2)]
            dma("gpsimd", memT[:, :, :], I["memT"].rearrange("(c p) m -> p c m", p=128), [], [r_mem])
            dma("gpsimd", wkv[:, :, :], I["w_kv"].rearrange("(c p) n -> p c n", p=128), [], [r_wkv])
            r_qmz = [K.res() for _ in range(4)]
            for hm_ in range(4):
                ins("gpsimd" if hm_ % 2 == 0 else "vector", "memset", [], [r_qmz[hm_]], ap=qmz[hm_][:, :], constant=0.0)
            for hm_ in range(4):
                p_ = 64 * (hm_ % 2)
                dma("sync" if hm_ % 2 == 0 else "scalar", qmz[hm_][p_:p_ + 64, :], QM[hm_ // 2, p_:p_ + 64, :], [], [r_qmz[hm_]])
            ins("gpsimd", "memset", [], [r_ones], ap=ones64[:, :], constant=1.0)
            for i in range(2):
                for c in range(8):
                    mm(sps4[i][:, 0:256], wkv[:, c, i * 128:(i + 1) * 128], memT[:, c, :], c == 0, c == 7, [r_wkv, r_mem], r_sps[i], c == 0)
                ins("vector", "tensor_copy", [r_sps[i]], [r_km], append=(i > 0), out=km[:, i, :], in_=sps4[i][:, 0:256])
            for j in range(2):
                for c in range(8):
                    mm(nps[j][:, 0:256], memT[:, c, j * 128:(j + 1) * 128], wkv[:, c, 256:512], c == 0, c == 7, [r_wkv, r_mem], r_nps[j], c == 0)
                ins("vector", "tensor_copy", [r_nps[j]], [r_vm], append=(j > 0), out=vm[:, j, :], in_=nps[j][:, 0:256])
            def make_seg4(hm, s):
                i = hm // 2
                a = (hm * NST + s) % 2
                st = {}

                def A():
                    si = (hm * NST + s) % 2
                    st["si"] = si
                    for j in range(2):
                        mm(sps4[si][:, j * 512:(j + 1) * 512], km[:, i, j * 128:(j + 1) * 128], qmz[hm][:, s * 512:(s + 1) * 512], True, True,
                           [r_km, r_qmz[hm]], r_sps[si], j == 0)

                def B():
                    si = st["si"]
                    ins("scalar", "activation", [r_sps[si]], [r_pt[si]], out=pt4[si][:, :], in_=sps4[si][:, :], func=AF.Exp)

                def C():
                    si = st["si"]
                    for j in range(2):
                        mm(nps[a][0:64, :], vm[:, j, hm * 64:(hm + 1) * 64], pt4[si][:, j * 512:(j + 1) * 512], j == 0, j == 1, [r_vm, r_pt[si]], r_nps[a], j == 0)
                        mm(dps[a][0:64, :], ones64[:, :], pt4[si][:, j * 512:(j + 1) * 512], j == 0, j == 1, [r_ones, r_pt[si]], r_dps[a], j == 0)

                def post():
                    ins("scalar", "activation", [r_dps[a]], [r_rden[a]], out=rden[a][0:64, :], in_=dps[a][0:64, :], func=AF.Ln)
                    ins("scalar", "activation", [r_rden[a]], [r_rden[a]], out=rden[a][0:64, :], in_=rden[a][0:64, :], func=AF.Exp, scale=-1.0)
                    ins("vector", "tensor_tensor", [r_nps[a], r_rden[a]], [r_yo[a]], out=yo[a][0:64, :], in0=nps[a][0:64, :],
                        in1=rden[a][0:64, :], op=ALU.mult)
                    dma("sync", YT[12 + hm, :, s * 512:(s + 1) * 512], yo[a][0:64, :], [r_yo[a]], [])
                return {"steps": [(A, B, C)], "pre": None, "post": post, "hoist": True}

            segs = []
            for hm in range(4):
                for s in range(NST):
                    segs.append(make_seg4(hm, s))
            A0, B0, C0 = segs[0]["steps"][0]
            A0(); B0()
            for u in range(len(segs)):
                if u + 1 < len(segs):
                    An, Bn, Cn = segs[u + 1]["steps"][0]
                    An(); Bn()
                segs[u]["steps"][0][2]()
                segs[u]["post"]()
            K.emit()
        if stage <= 4:
            return nc

        with ExitStack() as es:
            sb = lambda n, s, d: es.enter_context(nc.sbuf_tensor(n, s, d))
            ps = lambda n: es.enter_context(nc.psum_tensor(n, [128, 512], F32))
            wbr = sb("p5_wbr", [128, 8, D], BF16); r_wbr = K.res()
            wout = sb("p5_wout", [128, 8, D], BF16); r_wout = K.res()
            g1 = sb("p5_g1", [128, D], F32); r_g1 = K.res()
            b1 = sb("p5_b1", [128, D], F32); r_b1 = K.res()
            wr = sb("p5_wr", [128, 8, 36], F32); r_wr = K.res()
            brr = sb("p5_br", [128, 36], F32); r_br = K.res()
            identf = sb("p5_idf", [128, 128], F32); r_idf = K.res()
            tst = sb("p5_tst", [128, 128], BF16); r_tst = K.res()
            onesb = sb("p5_ones", [128, 128], BF16); r_onesb = K.res()
            cbe = sb("p5_cbe", [128, 32], F32); r_cbe = K.res()
            ytl = [sb("p5_y%d" % i, [128, 8, 512], BF16) for i in range(2)]; r_y = [K.res() for _ in range(2)]
            gl = sb("p5_gl", [128, 24, 512], BF16); r_gl = [K.res() for _ in range(8)]
            acc = [sb("p5_acc%d" % i, [128, 512], F32) for i in range(2)]; r_acc = [K.res() for _ in range(2)]
            a1 = [sb("p5_a1%d" % i, [128, 512], F32) for i in range(2)]; r_a1 = [K.res() for _ in range(2)]
            c2 = [sb("p5_c2%d" % i, [128, 512], F32) for i in range(2)]; r_c2 = [K.res() for _ in range(2)]
            mgb = [sb("p5_mg%d" % i, [128, 8, 512], BF16) for i in range(2)]; r_mg = [K.res() for _ in range(2)]
            xtok = [sb("p5_xt%d" % i, [128, D], F32) for i in range(4)]; r_x = [K.res() for _ in range(4)]
            x1 = sb("p5_xone", [128, 4, D], F32); r_x1 = [K.res() for _ in range(4)]
            x1a = [sb("p5_x1a%d" % i, [128, D], F32) for i in range(2)]; r_x1a = [K.res() for _ in range(2)]
            x1b = [sb("p5_x1b%d" % i, [128, 4, D], BF16) for i in range(2)]; r_x1b = [[K.res() for _ in range(4)] for _ in range(2)]
            x1T = sb("p5_x1T", [128, 8, 512], F32); r_x1T = [K.res() for _ in range(4)]
            st6 = sb("p5_st6", [128, 4, 12], F32); r_st6 = K.res()
            mv = sb("p5_mv", [128, 4, 2], F32); r_mv = K.res()
            sm = sb("p5_sm", [128, 3, 4], F32); r_sm = K.res()
            lg4 = sb("p5_lg4", [128, 4, 36], F32); r_lg = K.res()
            gmax = sb("p5_gmax", [128, 4], F32); r_gmax = K.res()
            goh = sb("p5_goh", [128, 16], F32); r_goh = K.res()
            dd = sb("p5_dd", [128, 16], F32); r_dd = K.res()
            gex = sb("p5_gex", [128, 16], F32); r_gex = K.res()
            gp = sb("p5_gp", [128, 4], F32); r_gp = K.res()
            lem = sb("p5_lem", [128, 128], F32); r_lem = K.res()
            e8 = sb("p5_e8", [128, 4, 8], F32); r_e8 = K.res()
            oh1 = sb("p5_oh1", [128, 128], F32); r_oh1 = K.res()
            oh2 = sb("p5_oh2", [128, 128], F32); r_oh2 = K.res()
            ohb = sb("p5_ohb", [128, 128], BF16); r_ohb = K.res()
            wt = sb("p5_wt", [128, 4], F32); r_wt = K.res()
            cnt = sb("p5_cnt", [128, 128], F32); r_cnt = K.res()
            t128 = sb("p5_t128", [128, 128], F32); r_t128 = K.res()
            slf = sb("p5_slf", [128, 8], F32); r_slf = K.res()
            r_slot = K.res()
            r_gate = K.res()
            bps = [ps("p5_pb%d" % i) for i in range(3)]; r_bps = [K.res() for _ in range(3)]
            mps = [ps("p5_pm%d" % i) for i in range(2)]; r_mps = [K.res() for _ in range(2)]
            tps2 = [ps("p5_pt%d" % i) for i in range(2)]; r_tps2 = [K.res() for _ in range(2)]
            sps5 = ps("p5_ps"); r_lgps = K.res(); r_cps = K.res()
            dma("gpsimd", wbr[:, :, :], I["wbr"].rearrange("(c p) n -> p c n", p=128), [], [r_wbr])
            dma("gpsimd", wout[:, :, :], I["w_out"].rearrange("(c p) n -> p c n", p=128), [], [r_wout])
            dma("sync", g1[:, :], I["ln1g"], [], [r_g1])
            dma("sync", b1[:, :], I["ln1b"], [], [r_b1])
            dma("sync", wr[:, :, :], I["w_r"].rearrange("(c p) n -> p c n", p=128), [], [r_wr])
            dma("sync", brr[:, :], I["b_r"], [], [r_br])
            dma("sync", identf[:, :], I["identf"], [], [r_idf])
            dma("gpsimd", tst[:, :], I["tst"], [], [r_tst])
            dma("sync", cbe[:, :], I["ebase"], [], [r_cbe])
            ins("gpsimd", "memset", [], [r_onesb], ap=onesb[:, :], constant=1.0)
            YT_v = YT.rearrange("(c two) p t -> c (two p) t", two=2).rearrange("c q t -> q c t")
            G_v = G.rearrange("(b m) p t -> p m b t", b=3)
            gl_v = gl[:, :, :].rearrange("p (m b) t -> p m b t", b=3)
            heads = ((0, 3), (3, 6), (6, 8))
            cnt5 = {"b": 0}

            def load_y(s):
                dma("sync", ytl[s % 2][:, :, :], YT_v[:, :, s * 512:(s + 1) * 512], [], [r_y[s % 2]])

            def load_g(s, m):
                dma("sync", gl_v[:, m, :, :], G_v[:, m, :, s * 512:(s + 1) * 512], [], [r_gl[m]])

            def M(s, pieces=None):
                b = s % 2
                for m in range(8):
                    if pieces is not None and m < len(pieces):
                        pieces[m]()
                    a = m % 2
                    pbs = []
                    for br in range(3):
                        pb = cnt5["b"] % 3
                        cnt5["b"] += 1
                        pbs.append(pb)
                        h0, h1 = heads[br]
                        for hh in range(h0, h1):
                            mm(bps[pb][:, :], wbr[:, hh, m * 128:(m + 1) * 128], ytl[b][:, hh, :], hh == h0, hh == h1 - 1,
                               [r_wbr, r_y[b]], r_bps[pb], hh == h0)
                    ins("vector", "tensor_tensor", [r_bps[pbs[0]], r_gl[m]], [r_acc[a]], out=acc[a][:, :], in0=bps[pbs[0]][:, :],
                        in1=gl[:, 3 * m + 0, :], op=ALU.mult)
                    ins("vector", "tensor_tensor", [r_bps[pbs[1]], r_gl[m]], [r_a1[a]], out=a1[a][:, :], in0=bps[pbs[1]][:, :],
                        in1=gl[:, 3 * m + 1, :], op=ALU.mult)
                    ins("vector", "tensor_tensor", [r_bps[pbs[2]], r_gl[m]], [r_c2[a]], out=c2[a][:, :], in0=bps[pbs[2]][:, :],
                        in1=gl[:, 3 * m + 2, :], op=ALU.mult)
                    ins("gpsimd", "tensor_tensor", [r_acc[a], r_a1[a]], [r_acc[a]], out=acc[a][:, :], in0=acc[a][:, :], in1=a1[a][:, :], op=ALU.add)
                    ins("gpsimd", "tensor_tensor", [r_acc[a], r_c2[a]], [r_mg[b]], append=(m > 0),
                        out=mgb[b][:, m, :], in0=acc[a][:, :], in1=c2[a][:, :], op=ALU.add)
                    if s + 1 < NST:
                        load_g(s + 1, m)
                if s + 2 < NST:
                    load_y(s + 2)

            def load_x(s):
                for tt in range(4):
                    t = 4 * s + tt
                    dma("scalar", xtok[tt][:, :], I["x"][t * 128:(t + 1) * 128, :], [], [r_x[tt]])

            def XA(s):
                b = s % 2
                for tt in range(4):
                    for half in range(2):
                        for c in range(8):
                            mm(mps[half][:, :], mgb[b][:, c, tt * 128:(tt + 1) * 128], wout[:, c, half * 512:(half + 1) * 512], c == 0, c == 7,
                               [r_mg[b], r_wout], r_mps[half], c == 0)
                        ins("vector", "scalar_tensor_tensor", [r_x[tt], r_mps[half]], [r_x[tt]],
                            out=xtok[tt][:, half * 512:(half + 1) * 512], in0=xtok[tt][:, half * 512:(half + 1) * 512], scalar=ALPHA,
                            in1=mps[half][:, :], op0=ALU.mult, op1=ALU.add)
                    for half in range(2):
                        ins("vector", "bn_stats", [r_x[tt]], [r_st6], append=(tt + half > 0), out=st6[:, tt, half * 6:(half + 1) * 6],
                            in_=xtok[tt][:, half * 512:(half + 1) * 512])
                for tt in range(4):
                    ins("vector", "bn_aggr", [r_st6], [r_mv], append=(tt > 0), out=mv[:, tt, :], in_=st6[:, tt, :])
                ins("scalar", "activation", [r_mv], [r_sm], out=sm[:, 0, :], in_=mv[:, :, 1], func=AF.Ln, bias=EPS, scale=1.0)
                ins("scalar", "activation", [r_sm], [r_sm], out=sm[:, 1, :], in_=sm[:, 0, :], func=AF.Exp, scale=-0.5)
                ins("vector", "scalar_tensor_tensor", [r_mv, r_sm], [r_sm], out=sm[:, 2, :], in0=mv[:, :, 0], scalar=-1.0,
                    in1=sm[:, 1, :], op0=ALU.mult, op1=ALU.mult)

            def XB(s):
                b = s % 2
                for tt in range(4):
                    ins("scalar", "activation", [r_x[tt], r_sm], [r_x1[tt]], out=x1[:, tt, :], in_=xtok[tt][:, :], func=AF.Identity,
                        bias=sm[:, 2, tt:tt + 1], scale=sm[:, 1, tt:tt + 1])
                for tt in range(4):
                    ins("vector", "tensor_tensor", [r_x1[tt], r_g1], [r_x1[tt]], out=x1[:, tt, :], in0=x1[:, tt, :], in1=g1[:, :], op=ALU.mult)
                    ins("vector", "tensor_tensor", [r_x1[tt], r_b1], [r_x1[tt]], out=x1[:, tt, :], in0=x1[:, tt, :], in1=b1[:, :], op=ALU.add)
                for tt in range(4):
                    t = 4 * s + tt
                    u = t % 2
                    ins("scalar", "mul", [r_x1[tt]], [r_x1a[u]], out=x1a[u][:, :], in_=x1[:, tt, :], mul=ALPHA)
                    ins("scalar", "copy", [r_x1[tt]], [r_x1b[b][tt]], out=x1b[b][:, tt, :], in_=x1[:, tt, :])
                    dma("sync", X1A[t * 128:(t + 1) * 128, :], x1a[u][:, :], [r_x1a[u]], [])

            def T(s):
                def TR(tt):
                    for c in range(8):
                        hf = c // 4
                        K.op("tensor", (lambda e, c=c, hf=hf, tt=tt: e.transpose(out=tps2[hf][:, (c % 4) * 128:(c % 4 + 1) * 128],
                                                                                 in_=x1[:, tt, c * 128:(c + 1) * 128], identity=identf[:, :])),
                             [r_x1[tt], r_idf], [r_tps2[hf]], append=(c % 4 > 0))

                def CP(tt):
                    for hf in range(2):
                        ins("scalar", "copy", [r_tps2[hf]], [r_x1T[tt]], append=(hf > 0), out=x1T[:, hf * 4:(hf + 1) * 4, tt * 128:(tt + 1) * 128],
                            in_=tps2[hf][:, :].rearrange("p (c t) -> p c t", t=128))

                def MMr(tt):
                    for c in range(8):
                        mm(sps5[:, tt * 36:(tt + 1) * 36], x1T[:, c, tt * 128:(tt + 1) * 128], wr[:, c, :], c == 0, c == 7, [r_x1T[tt], r_wr],
                           r_lgps, tt == 0 and c == 0)

                return [lambda: TR(0), lambda: (CP(0), TR(1)), lambda: MMr(0), lambda: (CP(1), TR(2)), lambda: MMr(1),
                        lambda: (CP(2), TR(3)), lambda: MMr(2), lambda: (CP(3), MMr(3))]

            v4 = lambda ap, n: ap.rearrange("p (t n) -> p t n", n=n)

            def Ra(s):
                ins("vector", "tensor_tensor", [r_lgps, r_br], [r_lg], out=lg4[:, :, :], in0=v4(sps5[:, 0:144], 36),
                    in1=brr[:, :].unsqueeze(1).to_broadcast([128, 4, 36]), op=ALU.add)
                ins("vector", "tensor_reduce", [r_lg], [r_gmax], out=gmax[:, :], in_=lg4[:, :, 0:4], axis=AX.X, op=ALU.max)
                ins("vector", "tensor_tensor", [r_lg, r_gmax], [r_goh], out=v4(goh[:, :], 4), in0=lg4[:, :, 0:4],
                    in1=gmax[:, :].unsqueeze(2).to_broadcast([128, 4, 4]), op=ALU.is_equal)
                ins("vector", "tensor_tensor", [r_lg, r_gmax], [r_dd], out=v4(dd[:, :], 4), in0=lg4[:, :, 0:4],
                    in1=gmax[:, :].unsqueeze(2).to_broadcast([128, 4, 4]), op=ALU.subtract)
                ins("scalar", "activation", [r_dd], [r_gex], out=gex[:, :], in_=dd[:, :], func=AF.Exp)
                ins("vector", "tensor_reduce", [r_gex], [r_gp], out=gp[:, :], in_=v4(gex[:, :], 4), axis=AX.X, op=ALU.add)
                ins("vector", "reciprocal", [r_gp], [r_gp], out=gp[:, :], in_=gp[:, :])
                ins("vector", "tensor_scalar", [r_goh], [r_goh], out=goh[:, :], in0=goh[:, :], scalar1=-1.0, scalar2=1e30,
                    op0=ALU.add, op1=ALU.mult)
                ins("vector", "tensor_tensor", [r_lg, r_goh], [r_lem], out=lem[:, :].rearrange("p (t g e) -> p t g e", g=4, e=8),
                    in0=lg4[:, :, 4:36].rearrange("p t (g e) -> p t g e", e=8),
                    in1=v4(goh[:, :], 4).unsqueeze(3).to_broadcast([128, 4, 4, 8]), op=ALU.add)

            def Rb(s):
                for tt in range(4):
                    ins("vector", "max", [r_lem], [r_e8], append=(tt > 0), out=e8[:, tt, :], in_=lem[:, tt * 32:(tt + 1) * 32])
                ins("vector", "tensor_tensor", [r_lem, r_e8], [r_oh1], out=v4(oh1[:, :], 32), in0=v4(lem[:, :], 32),
                    in1=e8[:, :, 0:1].to_broadcast([128, 4, 32]), op=ALU.is_equal)
                ins("vector", "tensor_tensor", [r_lem, r_e8], [r_oh2], out=v4(oh2[:, :], 32), in0=v4(lem[:, :], 32),
                    in1=e8[:, :, 1:2].to_broadcast([128, 4, 32]), op=ALU.is_equal)
                ins("vector", "tensor_tensor", [r_e8], [r_wt], out=wt[:, :], in0=e8[:, :, 1], in1=e8[:, :, 0], op=ALU.subtract)
                ins("scalar", "activation", [r_wt], [r_wt], out=wt[:, :], in_=wt[:, :], func=AF.Exp)
                ins("vector", "tensor_scalar", [r_wt], [r_wt], out=wt[:, :], in0=wt[:, :], scalar1=1.0, scalar2=None, op0=ALU.add)
                ins("vector", "reciprocal", [r_wt], [r_wt], out=wt[:, :], in_=wt[:, :])
                gs = gateS[:, 8 * s:8 * s + 8].rearrange("p (t k) -> p t k", k=2)
                ins("vector", "tensor_tensor", [r_wt, r_gp], [r_gate], append=True, out=gs[:, :, 0], in0=wt[:, :], in1=gp[:, :], op=ALU.mult)
                ins("vector", "tensor_tensor", [r_gp, r_gate], [r_gate], append=True, out=gs[:, :, 1], in0=gp[:, :], in1=gs[:, :, 0], op=ALU.subtract)
                ins("vector", "tensor_tensor", [r_oh1, r_oh2], [r_ohb], out=ohb[:, :], in0=oh1[:, :], in1=oh2[:, :], op=ALU.add)

            def Rc(s):
                b = s % 2
                for tt in range(4):
                    mm(sps5[:, 256 + tt * 32:256 + (tt + 1) * 32], tst[:, :], ohb[:, tt * 32:(tt + 1) * 32], True, tt == 0,
                       [r_tst, r_ohb], r_cps, tt == 0)
                    for t2 in range(tt):
                        mm(sps5[:, 256 + tt * 32:256 + (tt + 1) * 32], onesb[:, :], ohb[:, t2 * 32:(t2 + 1) * 32], False, t2 == tt - 1,
                           [r_onesb, r_ohb], r_cps, False)
                for tt in range(4):
                    mm(sps5[:, 384:416], onesb[:, :], ohb[:, tt * 32:(tt + 1) * 32], tt == 0, tt == 3, [r_onesb, r_ohb], r_cps, False)
                ins("vector", "tensor_tensor", [r_cps, r_cbe], [r_cnt], out=v4(cnt[:, :], 32), in0=v4(sps5[:, 256:384], 32),
                    in1=cbe[:, :].unsqueeze(1).to_broadcast([128, 4, 32]), op=ALU.add)
                ins("vector", "tensor_tensor", [r_cps, r_cbe], [r_cbe], out=cbe[:, :], in0=sps5[:, 384:416], in1=cbe[:, :], op=ALU.add)
                sl = slf[:, :].rearrange("p (t k) -> p t k", k=2)
                ins("vector", "tensor_tensor", [r_cnt, r_oh1], [r_t128], out=t128[:, :], in0=cnt[:, :], in1=oh1[:, :], op=ALU.mult)
                ins("vector", "tensor_reduce", [r_t128], [r_slf], out=sl[:, :, 0], in_=v4(t128[:, :], 32), axis=AX.X, op=ALU.add)
                ins("vector", "tensor_tensor", [r_cnt, r_oh2], [r_t128], out=t128[:, :], in0=cnt[:, :], in1=oh2[:, :], op=ALU.mult)
                ins("vector", "tensor_reduce", [r_t128, r_slf], [r_slf], out=sl[:, :, 1], in_=v4(t128[:, :], 32), axis=AX.X, op=ALU.add)
                ins("vector", "tensor_copy", [r_slf], [r_slot], append=True, out=slotS[:, 8 * s:8 * s + 8], in_=slf[:, :])
                for tt in range(4):
                    for k in range(2):
                        col = 8 * s + 2 * tt + k
                        K.op("gpsimd", (lambda e, col=col, tt=tt, b=b: e.indirect_dma_start(
                            out=XP, out_offset=bass.IndirectOffsetOnAxis(ap=slotS[:, col:col + 1], axis=0),
                            in_=x1b[b][:, tt, :], in_offset=None)), [r_x1b[b][tt], r_slot], [], dma=True)

            load_y(0)
            load_y(1)
            for m in range(8):
                load_g(0, m)

            load_x(0)
            M(0)
            XA(0)
            XB(0)
            for s in range(NST):
                tp_ = T(s)
                if s + 1 < NST:
                    load_x(s + 1)
                    M(s + 1, tp_)
                else:
                    for f in tp_:
                        f()
                Ra(s)
                if s + 1 < NST:
                    XA(s + 1)
                Rb(s)
                if s + 1 < NST:
                    XB(s + 1)
                Rc(s)
            if debug:
                dma("sync", SLOTD, slotS[:, :], [r_slot], [])
                dma("sync", GATED, gateS[:, :], [r_gate], [])
            K.emit()
        if stage <= 5:
            return nc

        with ExitStack() as es:
            sb = lambda n, s, d: es.enter_context(nc.sbuf_tensor(n, s, d))
            ps = lambda n, dt=F32, w=512: es.enter_context(nc.psum_tensor(n, [128, w], dt))
            wg = [sb("p6_wg%d" % i, [128, 8, 512], BF16) for i in range(2)]; r_wg = [K.res() for _ in range(2)]
            wu = [sb("p6_wu%d" % i, [128, 8, 512], BF16) for i in range(2)]; r_wu = [K.res() for _ in range(2)]
            wd = [sb("p6_wd%d" % i, [128, 4, D], BF16) for i in range(2)]; r_wd = [K.res() for _ in range(2)]
            xp = [sb("p6_xp%d" % i, [128, NT, D], BF16) for i in range(2)]; r_xp = [K.res() for _ in range(2)]
            xpT = [sb("p6_xpT%d" % i, [128, 8, CAP], BF16) for i in range(2)]; r_xpT = [K.res() for _ in range(2)]
            identb = sb("p6_id", [128, 128], BF16); r_id = K.res()
            sg = [sb("p6_sg%d" % i, [128, 512], F32) for i in range(2)]; r_sg = [K.res() for _ in range(2)]
            hT = [sb("p6_hT%d" % i, [128, 4, CAP], BF16) for i in range(2)]; r_hT = [K.res() for _ in range(2)]
            yb = [sb("p6_y%d" % i, [128, D], BF16) for i in range(4)]; r_yb = [K.res() for _ in range(4)]
            tp = [ps("p6_pt%d" % i, BF16, 1024) for i in range(2)]; r_tp = [K.res() for _ in range(2)]
            gp = [ps("p6_pg%d" % i) for i in range(2)]; r_gp = [K.res() for _ in range(2)]
            up = [ps("p6_pu%d" % i) for i in range(2)]; r_up = [K.res() for _ in range(2)]
            yp = [ps("p6_py%d" % i) for i in range(2)]; r_yp = [K.res() for _ in range(2)]
            dma("gpsimd", identb[:, :], I["identf"], [], [r_id])
            XP_v = XP.rearrange("(e i p) d -> e p i d", p=128, i=NT)
            YP_v = YP.rearrange("(e i p) d -> e i p d", p=128, i=NT)

            def load_e(e):
                b = e % 2
                dma("gpsimd", wg[b][:, :, :], I["w_gate"][e].rearrange("(c p) n -> p c n", p=128), [], [r_wg[b]])
                dma("gpsimd", wu[b][:, :, :], I["w_up"][e].rearrange("(c p) n -> p c n", p=128), [], [r_wu[b]])
                for hf in range(2):
                    dma("gpsimd", wd[b][:, :, hf * 512:(hf + 1) * 512], I["w_down"][e].rearrange("(c p) n -> p c n", p=128)[:, :, hf * 512:(hf + 1) * 512],
                        [], [r_wd[b]], append=(hf > 0))
                dma("sync", xp[b][:, :, :], XP_v[e], [], [r_xp[b]])

            cnt6 = {"t": 0, "j": 0, "y": 0}

            def TRP(e):
                b = e % 2
                for i in range(NT):
                    tb = cnt6["t"] % 2
                    cnt6["t"] += 1
                    for c in range(8):
                        K.op("tensor", (lambda en, i=i, c=c, b=b, tb=tb: en.transpose(out=tp[tb][:, c * 128:(c + 1) * 128],
                                                                                      in_=xp[b][:, i, c * 128:(c + 1) * 128], identity=identb[:, :])),
                             [r_xp[b], r_id], [r_tp[tb]], append=(c > 0))
                    ins("vector" if i % 2 == 0 else "scalar", "tensor_copy" if i % 2 == 0 else "copy", [r_tp[tb]], [r_xpT[b]], append=(i > 0),
                        out=xpT[b][:, :, i * 128:(i + 1) * 128], in_=tp[tb][:, :].rearrange("p (c t) -> p c t", t=128))

            def GU(e):
                b = e % 2
                for j in range(4):
                    jb = cnt6["j"] % 2
                    cnt6["j"] += 1
                    for c in range(8):
                        mm(gp[jb][:, 0:CAP], wg[b][:, c, j * 128:(j + 1) * 128], xpT[b][:, c, :], c == 0, c == 7, [r_wg[b], r_xpT[b]], r_gp[jb], c == 0)
                    for c in range(8):
                        mm(up[jb][:, 0:CAP], wu[b][:, c, j * 128:(j + 1) * 128], xpT[b][:, c, :], c == 0, c == 7, [r_wu[b], r_xpT[b]], r_up[jb], c == 0)
                    ins("scalar", "activation", [r_gp[jb]], [r_sg[jb]], out=sg[jb][:, 0:CAP], in_=gp[jb][:, 0:CAP], func=AF.Silu)
                    ins("vector", "tensor_tensor", [r_sg[jb], r_up[jb]], [r_hT[b]], append=(j > 0), out=hT[b][:, j, :], in0=sg[jb][:, 0:CAP],
                        in1=up[jb][:, 0:CAP], op=ALU.mult)

            def DN(e):
                b = e % 2
                for i in range(NT):
                    ob = cnt6["y"] % 4
                    cnt6["y"] += 1
                    for hf in range(2):
                        for j in range(4):
                            mm(yp[hf][:, :], hT[b][:, j, i * 128:(i + 1) * 128], wd[b][:, j, hf * 512:(hf + 1) * 512], j == 0, j == 3,
                               [r_hT[b], r_wd[b]], r_yp[hf], j == 0)
                        if hf == 0:
                            ins("scalar", "copy", [r_yp[hf]], [r_yb[ob]], out=yb[ob][:, hf * 512:(hf + 1) * 512], in_=yp[hf][:, :])
                        else:
                            ins("vector", "tensor_copy", [r_yp[hf]], [r_yb[ob]], append=True, out=yb[ob][:, hf * 512:(hf + 1) * 512], in_=yp[hf][:, :])
                    dma("sync", YP_v[e, i], yb[ob][:, :], [r_yb[ob]], [])

            load_e(0)
            TRP(0)
            for e in range(32):
                if e + 1 < 32:
                    load_e(e + 1)
                GU(e)
                if e + 1 < 32:
                    TRP(e + 1)
                DN(e)
            K.emit()
        if stage <= 6:
            return nc

        with ExitStack() as es:
            sb = lambda n, s, d: es.enter_context(nc.sbuf_tensor(n, s, d))
            NB = 3
            g2 = sb("p7_g2", [128, D], F32); r_g2 = K.res()
            b2 = sb("p7_b2", [128, D], F32); r_b2 = K.res()
            xa = [sb("p7_xa%d" % i, [128, D], F32) for i in range(NB)]; r_xa = [K.res() for _ in range(NB)]
            y1 = [sb("p7_y1%d" % i, [128, D], BF16) for i in range(NB)]; r_y1 = [K.res() for _ in range(NB)]
            y2 = [sb("p7_y2%d" % i, [128, D], BF16) for i in range(NB)]; r_y2 = [K.res() for _ in range(NB)]
            h2 = [sb("p7_h%d" % i, [128, D], F32) for i in range(NB)]; r_h2 = [K.res() for _ in range(NB)]
            t7 = [sb("p7_t7_%d" % i, [128, D], F32) for i in range(2)]; r_t7 = [K.res() for _ in range(2)]
            o2 = [sb("p7_o%d" % i, [128, D], F32) for i in range(2)]; r_o2 = [K.res() for _ in range(2)]
            junk = sb("p7_junk", [128, D], BF16); r_junk = K.res()
            st6 = [sb("p7_st6_%d" % i, [128, 4], F32) for i in range(NB)]; r_st6 = [K.res() for _ in range(NB)]
            mv = [sb("p7_mv_%d" % i, [128, 2], F32) for i in range(NB)]; r_mv = [K.res() for _ in range(NB)]
            sm = [sb("p7_sm_%d" % i, [128, 4], F32) for i in range(NB)]; r_sm = [K.res() for _ in range(NB)]
            dma("sync", g2[:, :], I["ln2g"], [], [r_g2])
            dma("sync", b2[:, :], I["ln2b"], [], [r_b2])

            def load_t(t):
                u = t % NB
                dma("sync", xa[u][:, :], X1A[t * 128:(t + 1) * 128, :], [], [r_xa[u]])
                K.op("gpsimd", (lambda e, t=t, u=u: e.indirect_dma_start(
                    out=y1[u][:, :], out_offset=None, in_=YP,
                    in_offset=bass.IndirectOffsetOnAxis(ap=slotS[:, 2 * t:2 * t + 1], axis=0))), [], [r_y1[u]], dma=True)
                K.op("gpsimd", (lambda e, t=t, u=u: e.indirect_dma_start(
                    out=y2[u][:, :], out_offset=None, in_=YP,
                    in_offset=bass.IndirectOffsetOnAxis(ap=slotS[:, 2 * t + 1:2 * t + 2], axis=0))), [], [r_y2[u]], dma=True)

            def Y1a(t):
                u = t % NB
                w = t % 2
                ins("vector", "scalar_tensor_tensor", [r_y1[u], r_xa[u]], [r_t7[w]], out=t7[w][:, :], in0=y1[u][:, :],
                    scalar=gateS[:, 2 * t:2 * t + 1], in1=xa[u][:, :], op0=ALU.mult, op1=ALU.add)
                ins("vector", "scalar_tensor_tensor", [r_y2[u], r_t7[w]], [r_h2[u]], out=h2[u][:, :], in0=y2[u][:, :],
                    scalar=gateS[:, 2 * t + 1:2 * t + 2], in1=t7[w][:, :], op0=ALU.mult, op1=ALU.add)
                if t + NB < 32:
                    load_t(t + NB)

            def YS(t):
                u = t % NB
                ins("scalar", "activation", [r_h2[u]], [r_junk, r_st6[u]], out=junk[:, :], in_=h2[u][:, :], func=AF.Identity,
                    accum_out=st6[u][:, 0:1])
                ins("scalar", "activation", [r_h2[u]], [r_junk, r_st6[u]], out=junk[:, :], in_=h2[u][:, :], func=AF.Square,
                    accum_out=st6[u][:, 1:2])

            def Y1b(t):
                u = t % NB
                ins("vector", "tensor_scalar", [r_st6[u]], [r_mv[u]], out=mv[u][:, 0:1], in0=st6[u][:, 0:1], scalar1=1.0 / D, scalar2=None,
                    op0=ALU.mult)
                ins("vector", "tensor_tensor", [r_mv[u]], [r_st6[u]], out=st6[u][:, 2:3], in0=mv[u][:, 0:1], in1=mv[u][:, 0:1], op=ALU.mult)
                ins("vector", "scalar_tensor_tensor", [r_st6[u], r_mv[u]], [r_mv[u]], out=mv[u][:, 1:2], in0=st6[u][:, 1:2], scalar=1.0 / D,
                    in1=st6[u][:, 2:3], op0=ALU.mult, op1=ALU.subtract)
                ins("scalar", "activation", [r_mv[u]], [r_sm[u]], out=sm[u][:, 0:1], in_=mv[u][:, 1:2], func=AF.Ln, bias=EPS, scale=1.0)
                ins("scalar", "activation", [r_sm[u]], [r_sm[u]], out=sm[u][:, 1:2], in_=sm[u][:, 0:1], func=AF.Exp, scale=-0.5)
                ins("vector", "scalar_tensor_tensor", [r_mv[u], r_sm[u]], [r_sm[u]], out=sm[u][:, 2:3], in0=mv[u][:, 0:1], scalar=-1.0,
                    in1=sm[u][:, 1:2], op0=ALU.mult, op1=ALU.mult)

            def Y2a(t):
                u = t % NB
                w = t % 2
                ins("scalar", "activation", [r_h2[u], r_sm[u]], [r_o2[w]], out=o2[w][:, :], in_=h2[u][:, :], func=AF.Identity,
                    bias=sm[u][:, 2:3], scale=sm[u][:, 1:2])

            def Y2b(t):
                w = t % 2
                ins("vector", "tensor_tensor", [r_o2[w], r_g2], [r_o2[w]], out=o2[w][:, :], in0=o2[w][:, :], in1=g2[:, :], op=ALU.mult)
                ins("vector", "tensor_tensor", [r_o2[w], r_b2], [r_o2[w]], out=o2[w][:, :], in0=o2[w][:, :], in1=b2[:, :], op=ALU.add)
                dma("sync", out_d[t * 128:(t + 1) * 128, :], o2[w][:, :], [r_o2[w]], [])

            for t in range(NB):
                load_t(t)
            Y1a(0); YS(0); Y1b(0)
            Y1a(1); YS(1)
            for t in range(32):
                Y2a(t)
                if t + 2 < 32:
                    Y1a(t + 2)
                    YS(t + 2)
                Y2b(t)
                if t + 1 < 32:
                    Y1b(t + 1)
            K.emit()
    return nc


_NC_CACHE = {}


def kernel(**inputs):
    maps = _prep(inputs)
    if "nc" not in _NC_CACHE:
        _NC_CACHE["nc"] = build()
    nc = _NC_CACHE["nc"]
    res = run_bass_kernel_spmd(nc, maps, core_ids=list(range(len(maps))))
    out = np.stack([np.asarray(r["out"], dtype=np.float32) for r in res.results], axis=0)
    return out
```
